# Optimizing a Trainium2 kernel written in Bass

```python
import math
import jax, jax.numpy as jnp
from jax import lax
import numpy as np


D_MODEL = 1024
BATCH = 8
SEQ = 4096
DEPTH = 4

N_MIXERS = 4
HEAD_DIM = 64
RMS_EPS = 1e-6
D_FF = 4 * D_MODEL
NEG_INF = -1e30

MOBA_HEADS = D_MODEL // HEAD_DIM
MOBA_BLOCK = 256
MOBA_TOPK = 3
MOBA_Q_CHUNK = 16

DIL_PAIRS = ((128, 1), (512, 4), (2048, 16))
DIL_HEADS_PER_GROUP = 8
DIL_BAND = 128

MLA_HEADS = 16
MLA_Q_RANK = 768
MLA_KV_RANK = 256
MLA_NOPE = 64
MLA_ROPE = 32
MLA_V = 64
ROPE_THETA = 10000.0
MLA_Q_BLOCK = 128

SWA_Q_HEADS = 16
SWA_KV_HEADS = 2
SWA_WINDOW = 128
SWA_BLOCK = 128

kernel_name = 'hybrid_moba_dilated_mla_swa_trunk'


def rmsnorm(x, g):
    xf = x.astype(jnp.float32)
    y = xf * lax.rsqrt(jnp.mean(xf * xf, axis=-1, keepdims=True) + RMS_EPS)
    return (y * g.astype(jnp.float32)).astype(x.dtype)


def alibi_slopes(n_heads):
    return 2.0 ** (-8.0 * jnp.arange(1, n_heads + 1, dtype=jnp.float32) / n_heads)


def heads_first(t, n_heads):
    B, S, _ = t.shape
    return t.reshape(B, S, n_heads, -1).transpose(0, 2, 1, 3)


def rope(x, pos):
    R = x.shape[-1]
    inv = ROPE_THETA ** (-jnp.arange(0, R, 2, dtype=jnp.float32) / R)
    ang = pos.astype(jnp.float32)[:, None] * inv[None, :]
    cos, sin = jnp.cos(ang)[:, None, :], jnp.sin(ang)[:, None, :]
    x1, x2 = jnp.split(x.astype(jnp.float32), 2, axis=-1)
    return jnp.concatenate([x1 * cos - x2 * sin, x1 * sin + x2 * cos], axis=-1).astype(x.dtype)


def moba_attention(h, w_qkv, w_o):
    B, S, _ = h.shape
    H, Dh, BLK, CQ = MOBA_HEADS, HEAD_DIM, MOBA_BLOCK, MOBA_Q_CHUNK
    nb = -(-S // BLK)
    Sp = nb * BLK
    pad = ((0, 0), (0, 0), (0, Sp - S), (0, 0))
    q, k, v = (jnp.pad(heads_first(t, H), pad) for t in jnp.split(h @ w_qkv, 3, axis=-1))
    scale = Dh ** -0.5
    slopes = alibi_slopes(H)[:, None, None]
    k_mean = jnp.mean(k.reshape(B, H, nb, BLK, Dh).astype(jnp.float32), axis=3)
    gate = jnp.einsum('bhsd,bhnd->bhsn', q.astype(jnp.float32), k_mean)
    q_block = jnp.arange(Sp) // BLK
    fully_past = jnp.arange(nb)[None, :] < q_block[:, None]
    gate = jnp.where(fully_past, gate, NEG_INF)
    n_sel = min(MOBA_TOPK, nb)
    _, sel = lax.top_k(gate, n_sel)
    sel_valid = sel < q_block[:, None]
    k_blocks = k.reshape(B, H, nb, BLK, Dh)
    v_blocks = v.reshape(B, H, nb, BLK, Dh)
    b_ix = jnp.arange(B)[:, None, None, None]
    h_ix = jnp.arange(H)[None, :, None, None]
    offs = jnp.arange(BLK)
    nc = Sp // CQ

    def to_chunks(t):
        return jnp.moveaxis(t.reshape(B, H, nc, CQ, *t.shape[3:]), 2, 0)

    def chunk(args):
        c, q_c, sel_c, valid_c = args
        t = c * CQ + jnp.arange(CQ)
        own = (c * CQ) // BLK
        kg = k_blocks[b_ix, h_ix, sel_c].reshape(B, H, CQ, n_sel * BLK, Dh)
        vg = v_blocks[b_ix, h_ix, sel_c].reshape(B, H, CQ, n_sel * BLK, Dh)
        s_g = (sel_c[..., None] * BLK + offs).reshape(B, H, CQ, n_sel * BLK)
        m_g = jnp.broadcast_to(valid_c[..., None], (B, H, CQ, n_sel, BLK)).reshape(B, H, CQ, n_sel * BLK)
        l_g = (jnp.einsum('bhqd,bhqkd->bhqk', q_c, kg).astype(jnp.float32) * scale
               - slopes * (t[:, None] - s_g).astype(jnp.float32))
        l_g = jnp.where(m_g, l_g, NEG_INF)
        ko = lax.dynamic_slice_in_dim(k, own * BLK, BLK, axis=2)
        vo = lax.dynamic_slice_in_dim(v, own * BLK, BLK, axis=2)
        d_o = t[:, None] - (own * BLK + offs)[None, :]
        l_o = (jnp.einsum('bhqd,bhkd->bhqk', q_c, ko).astype(jnp.float32) * scale
               - slopes * d_o.astype(jnp.float32))
        l_o = jnp.where(d_o >= 0, l_o, NEG_INF)
        p = jax.nn.softmax(jnp.concatenate([l_g, l_o], axis=-1), axis=-1).astype(v.dtype)
        return (jnp.einsum('bhqk,bhqkd->bhqd', p[..., :n_sel * BLK], vg)
                + jnp.einsum('bhqk,bhkd->bhqd', p[..., n_sel * BLK:], vo))

    o = lax.map(chunk, (jnp.arange(nc), to_chunks(q), to_chunks(sel), to_chunks(sel_valid)))
    o = jnp.moveaxis(o, 0, 2).reshape(B, H, Sp, Dh)[:, :, :S]
    return o.transpose(0, 2, 1, 3).reshape(B, S, H * Dh) @ w_o


def dilated_group(q, k, v, window, dil, slopes):
    B, S, Hg, Dh = q.shape
    n_pts = window // dil
    Sd = S // dil
    nblk = -(-Sd // DIL_BAND)
    Sdp = nblk * DIL_BAND
    scale = Dh ** -0.5

    def to_sub(t):
        t = t.reshape(B, Sd, dil, Hg, Dh).transpose(0, 2, 3, 1, 4)
        t = jnp.pad(t, ((0, 0), (0, 0), (0, 0), (0, Sdp - Sd), (0, 0)))
        return t.reshape(B, dil, Hg, nblk, DIL_BAND, Dh)

    def band(t):
        prev = jnp.pad(t[:, :, :, :-1], ((0, 0), (0, 0), (0, 0), (1, 0), (0, 0), (0, 0)))
        return jnp.concatenate([prev, t], axis=4)

    qs = to_sub(q)
    kb, vb = band(to_sub(k)), band(to_sub(v))
    logits = jnp.einsum('brhnqd,brhnkd->brhnqk', qs, kb).astype(jnp.float32) * scale
    qi = jnp.arange(DIL_BAND)[:, None]
    ki = jnp.arange(2 * DIL_BAND)[None, :]
    diff = qi + DIL_BAND - ki
    key_idx = jnp.arange(nblk)[:, None, None] * DIL_BAND - DIL_BAND + ki
    mask = (diff >= 0) & (diff <= n_pts) & (key_idx >= 0)
    logits = logits - slopes[:, None, None, None] * (dil * diff).astype(jnp.float32)
    logits = jnp.where(mask, logits, NEG_INF)
    m = jnp.max(logits, axis=-1, keepdims=True)
    e = jnp.exp(logits - m)
    den = jnp.sum(e, axis=-1, keepdims=True)
    o = jnp.einsum('brhnqk,brhnkd->brhnqd', e.astype(v.dtype), vb).astype(jnp.float32) / den
    lse = (m + jnp.log(den))[..., 0]
    o = o.reshape(B, dil, Hg, Sdp, Dh)[:, :, :, :Sd].transpose(0, 3, 1, 2, 4).reshape(B, S, Hg, Dh)
    lse = lse.reshape(B, dil, Hg, Sdp)[..., :Sd].transpose(0, 3, 1, 2).reshape(B, S, Hg)
    return o, lse


def dilated_attention(h, w_qkv, w_o):
    B, S, _ = h.shape
    G, Hg, Dh = len(DIL_PAIRS), DIL_HEADS_PER_GROUP, HEAD_DIM
    qkv = (h @ w_qkv).reshape(B, S, G, 3, Hg, Dh)
    slopes = alibi_slopes(G * Hg).reshape(G, Hg)
    outs, lses = [], []
    for g, (window, dil) in enumerate(DIL_PAIRS):
        o_g, lse_g = dilated_group(qkv[:, :, g, 0], qkv[:, :, g, 1], qkv[:, :, g, 2], window, dil, slopes[g])
        outs.append(o_g)
        lses.append(lse_g)
    w = jax.nn.softmax(jnp.stack(lses, axis=0), axis=0)[..., None]
    merged = jnp.sum(w * jnp.stack(outs, axis=0), axis=0).astype(h.dtype)
    return merged.reshape(B, S, Hg * Dh) @ w_o


def causal_block_attention(q, k, v, scale):
    B, H, S, Dk = q.shape
    QB = MLA_Q_BLOCK
    nqb = S // QB
    qb = jnp.moveaxis(q.reshape(B, H, nqb, QB, Dk), 2, 0)
    s_pos = jnp.arange(S)

    def blk(args):
        n, q_n = args
        t = n * QB + jnp.arange(QB)
        lg = jnp.einsum('bhqd,bhkd->bhqk', q_n, k).astype(jnp.float32) * scale
        lg = jnp.where(s_pos[None, :] <= t[:, None], lg, NEG_INF)
        p = jax.nn.softmax(lg, axis=-1).astype(v.dtype)
        return jnp.einsum('bhqk,bhkd->bhqd', p, v)

    o = lax.map(blk, (jnp.arange(nqb), qb))
    return jnp.moveaxis(o, 0, 2).reshape(B, H, S, v.shape[-1])


def mla_attention(h, w_dkv, q_norm, w_uq, kv_norm, w_ukv, w_o):
    B, S, _ = h.shape
    H = MLA_HEADS
    c_q, c_kv, k_rope = jnp.split(h @ w_dkv, [MLA_Q_RANK, MLA_Q_RANK + MLA_KV_RANK], axis=-1)
    q = (rmsnorm(c_q, q_norm) @ w_uq).reshape(B, S, H, MLA_NOPE + MLA_ROPE)
    kv = (rmsnorm(c_kv, kv_norm) @ w_ukv).reshape(B, S, H, MLA_NOPE + MLA_V)
    pos = jnp.arange(S)
    q = jnp.concatenate([q[..., :MLA_NOPE], rope(q[..., MLA_NOPE:], pos)], axis=-1)
    k_r = jnp.broadcast_to(rope(k_rope[:, :, None, :], pos), (B, S, H, MLA_ROPE))
    k = jnp.concatenate([kv[..., :MLA_NOPE], k_r], axis=-1)
    v = kv[..., MLA_NOPE:]
    o = causal_block_attention(q.transpose(0, 2, 1, 3), k.transpose(0, 2, 1, 3), v.transpose(0, 2, 1, 3),
                               (MLA_NOPE + MLA_ROPE) ** -0.5)
    return o.transpose(0, 2, 1, 3).reshape(B, S, H * MLA_V) @ w_o


def swa_sink_attention(h, w_qkv, sinks, w_o):
    B, S, _ = h.shape
    Hq, Hkv, Dh, BLK = SWA_Q_HEADS, SWA_KV_HEADS, HEAD_DIM, SWA_BLOCK
    G = Hq // Hkv
    nb = S // BLK
    q, k, v = jnp.split(h @ w_qkv, [Hq * Dh, (Hq + Hkv) * Dh], axis=-1)
    q = q.reshape(B, nb, BLK, Hkv, G, Dh).transpose(0, 3, 4, 1, 2, 5)

    def band(t):
        t = t.reshape(B, nb, BLK, Hkv, Dh).transpose(0, 3, 1, 2, 4)
        prev = jnp.pad(t[:, :, :-1], ((0, 0), (0, 0), (1, 0), (0, 0), (0, 0)))
        return jnp.concatenate([prev, t], axis=3)

    kb, vb = band(k), band(v)
    logits = jnp.einsum('bkgnqd,bkntd->bkgnqt', q, kb).astype(jnp.float32) * Dh ** -0.5
    qi = jnp.arange(BLK)[:, None]
    ki = jnp.arange(2 * BLK)[None, :]
    diff = qi + BLK - ki
    key_idx = jnp.arange(nb)[:, None, None] * BLK - BLK + ki
    mask = (diff >= 0) & (diff < SWA_WINDOW) & (key_idx >= 0)
    slopes = alibi_slopes(Hq).reshape(Hkv, G)[:, :, None, None, None]
    logits = jnp.where(mask, logits - slopes * diff.astype(jnp.float32), NEG_INF)
    sink = jnp.broadcast_to(sinks.astype(jnp.float32).reshape(Hkv, G, 1, 1, 1), logits.shape[:-1] + (1,))
    p = jax.nn.softmax(jnp.concatenate([logits, sink], axis=-1), axis=-1)[..., :-1].astype(v.dtype)
    o = jnp.einsum('bkgnqt,bkntd->bkgnqd', p, vb)
    o = o.transpose(0, 3, 4, 1, 2, 5).reshape(B, S, Hq * Dh)
    return o @ w_o


def squared_relu_mlp(h, w_up, w_down):
    return jnp.square(jax.nn.relu(h @ w_up)) @ w_down


def setup_inputs(seed: int = 0) -> dict:
    key = jax.random.key(seed)
    keys = jax.random.split(key, 64)
    counter = [0]

    def nk():
        counter[0] += 1
        return keys[counter[0] - 1]

    def w(shape):
        return jax.random.normal(nk(), shape, jnp.float32) * (shape[0] ** -0.5)

    def gain(n):
        return 1.0 + 0.02 * jax.random.normal(nk(), (n,), jnp.float32)

    p = {}
    p['x'] = jax.random.normal(nk(), (BATCH, SEQ, D_MODEL), jnp.float32)
    p['l0_attn_norm'] = gain(D_MODEL)
    p['l0_w_qkv'] = w((D_MODEL, 3 * MOBA_HEADS * HEAD_DIM))
    p['l0_w_o'] = w((MOBA_HEADS * HEAD_DIM, D_MODEL))
    p['l0_mlp_norm'] = gain(D_MODEL)
    p['l0_w_up'] = w((D_MODEL, D_FF))
    p['l0_w_down'] = w((D_FF, D_MODEL))
    p['l1_attn_norm'] = gain(D_MODEL)
    p['l1_w_qkv'] = w((D_MODEL, len(DIL_PAIRS) * 3 * DIL_HEADS_PER_GROUP * HEAD_DIM))
    p['l1_w_o'] = w((DIL_HEADS_PER_GROUP * HEAD_DIM, D_MODEL))
    p['l1_mlp_norm'] = gain(D_MODEL)
    p['l1_w_up'] = w((D_MODEL, D_FF))
    p['l1_w_down'] = w((D_FF, D_MODEL))
    p['l2_attn_norm'] = gain(D_MODEL)
    p['l2_w_dkv'] = w((D_MODEL, MLA_Q_RANK + MLA_KV_RANK + MLA_ROPE))
    p['l2_q_norm'] = gain(MLA_Q_RANK)
    p['l2_w_uq'] = w((MLA_Q_RANK, MLA_HEADS * (MLA_NOPE + MLA_ROPE)))
    p['l2_kv_norm'] = gain(MLA_KV_RANK)
    p['l2_w_ukv'] = w((MLA_KV_RANK, MLA_HEADS * (MLA_NOPE + MLA_V)))
    p['l2_w_o'] = w((MLA_HEADS * MLA_V, D_MODEL))
    p['l2_mlp_norm'] = gain(D_MODEL)
    p['l2_w_up'] = w((D_MODEL, D_FF))
    p['l2_w_down'] = w((D_FF, D_MODEL))
    p['l3_attn_norm'] = gain(D_MODEL)
    p['l3_w_qkv'] = w((D_MODEL, (SWA_Q_HEADS + 2 * SWA_KV_HEADS) * HEAD_DIM))
    p['l3_sinks'] = 0.5 * jax.random.normal(nk(), (SWA_Q_HEADS,), jnp.float32)
    p['l3_w_o'] = w((SWA_Q_HEADS * HEAD_DIM, D_MODEL))
    p['l3_mlp_norm'] = gain(D_MODEL)
    p['l3_w_up'] = w((D_MODEL, D_FF))
    p['l3_w_down'] = w((D_FF, D_MODEL))
    p['final_norm'] = gain(D_MODEL)
    return p


def reference(x,
              l0_attn_norm, l0_w_qkv, l0_w_o, l0_mlp_norm, l0_w_up, l0_w_down,
              l1_attn_norm, l1_w_qkv, l1_w_o, l1_mlp_norm, l1_w_up, l1_w_down,
              l2_attn_norm, l2_w_dkv, l2_q_norm, l2_w_uq, l2_kv_norm, l2_w_ukv, l2_w_o,
              l2_mlp_norm, l2_w_up, l2_w_down,
              l3_attn_norm, l3_w_qkv, l3_sinks, l3_w_o, l3_mlp_norm, l3_w_up, l3_w_down,
              final_norm):
    mixers = (moba_attention, dilated_attention, mla_attention, swa_sink_attention)
    mixer_params = ((l0_w_qkv, l0_w_o),
                    (l1_w_qkv, l1_w_o),
                    (l2_w_dkv, l2_q_norm, l2_w_uq, l2_kv_norm, l2_w_ukv, l2_w_o),
                    (l3_w_qkv, l3_sinks, l3_w_o))
    attn_norms = (l0_attn_norm, l1_attn_norm, l2_attn_norm, l3_attn_norm)
    mlp_norms = (l0_mlp_norm, l1_mlp_norm, l2_mlp_norm, l3_mlp_norm)
    w_ups = (l0_w_up, l1_w_up, l2_w_up, l3_w_up)
    w_downs = (l0_w_down, l1_w_down, l2_w_down, l3_w_down)
    h = x
    for i in range(DEPTH):
        mix = mixers[i % N_MIXERS]
        h = h + mix(rmsnorm(h, attn_norms[i]), *mixer_params[i])
        h = h + squared_relu_mlp(rmsnorm(h, mlp_norms[i]), w_ups[i], w_downs[i])
    return rmsnorm(h, final_norm)
```

```python
import contextlib
import numpy as np
import ml_dtypes
import concourse.bass as bass
import concourse.mybir as mybir
from concourse.bass_utils import run_bass_kernel_spmd

F32 = mybir.dt.float32
BF16 = mybir.dt.bfloat16
AF = mybir.ActivationFunctionType
ALU = mybir.AluOpType
AX = mybir.AxisListType
NPBF = ml_dtypes.bfloat16

SEQ = 4096
DM = 1024
NT = SEQ // 128
DFF = 4096
EPS = 1e-6
NEG = -30000.0

ENGS = ['pe', 'act', 'dve', 'pool', 'sp']
NDMASEM = 8
DBG = {}


class Op:
    __slots__ = ('fn', 'waits', 'key', 'idx', 'signal', 'isdma')

    def __init__(self, fn, key, idx, isdma):
        self.fn = fn
        self.waits = []
        self.key = key
        self.idx = idx
        self.signal = False
        self.isdma = isdma


class Sched:
    def __init__(self, nc):
        self.nc = nc
        self.streams = {e: [] for e in ENGS}
        self.keyops = {}
        self.seen = {e: {} for e in ENGS}
        self.res = {}
        self.dma_rr = {e: 0 for e in ENGS}

    def _need(self, eng, op, tok):
        key, idx = tok
        if self.seen[eng].get(key, -1) >= idx:
            return
        self.seen[eng][key] = idx
        op.waits.append(tok)

    def _deps(self, eng, op, reads, writes, mykey, same_ok):
        for r in reads:
            st = self.res.get(r)
            if st is None:
                continue
            w = st[0]
            if w is not None and not (same_ok and w[0] == mykey):
                self._need(eng, op, w)
        for r in writes:
            st = self.res.get(r)
            if st is None:
                continue
            w = st[0]
            if w is not None and w[0] != mykey:
                self._need(eng, op, w)
            for k, i in st[1].items():
                if k != mykey:
                    self._need(eng, op, (k, i))

    def _commit(self, tok, reads, writes):
        for r in reads:
            st = self.res.get(r)
            if st is None:
                st = self.res[r] = [None, {}]
            st[1][tok[0]] = tok[1]
        for r in writes:
            self.res[r] = [tok, {}]

    def op(self, eng, fn, reads=(), writes=()):
        key = eng
        lst = self.keyops.setdefault(key, [])
        o = Op(fn, key, len(lst), False)
        self._deps(eng, o, reads, writes, key, same_ok=(eng == 'pe'))
        lst.append(o)
        self.streams[eng].append(o)
        self._commit((key, o.idx), reads, writes)
        return o

    def dma(self, q, fn, reads=(), writes=()):
        j = self.dma_rr[q]
        self.dma_rr[q] = (j + 1) % NDMASEM
        key = ('dma', q, j)
        lst = self.keyops.setdefault(key, [])
        o = Op(fn, key, len(lst), True)
        if lst:
            self._need(q, o, (key, len(lst) - 1))
        self._deps(q, o, reads, writes, key, same_ok=False)
        lst.append(o)
        self.streams[q].append(o)
        self._commit((key, o.idx), reads, writes)
        return o

    def barrier(self):
        toks = [(key, len(lst) - 1) for key, lst in self.keyops.items() if lst]
        for e in ENGS:
            o = Op(None, None, None, False)
            for t in toks:
                self._need(e, o, t)
            if o.waits:
                self.streams[e].append(o)

    def emit(self):
        nc = self.nc
        for e in ENGS:
            for o in self.streams[e]:
                for (key, idx) in o.waits:
                    self.keyops[key][idx].signal = True
        semval = {}
        for key, lst in self.keyops.items():
            c = 0
            for o in lst:
                if o.isdma:
                    c += 16
                    semval[(key, o.idx)] = c
                    o.signal = True
                elif o.signal:
                    c += 1
                    semval[(key, o.idx)] = c
            assert c < 60000, (key, c)
        keys = [k for k, l in self.keyops.items() if l]
        with contextlib.ExitStack() as es:
            sems = {}
            for k in keys:
                nm = 's_' + ('_'.join(str(x) for x in k) if isinstance(k, tuple) else k)
                sems[k] = es.enter_context(nc.semaphore(nm))
            block = es.enter_context(nc.Block())

            def run(e):
                def body(eng):
                    for o in self.streams[e]:
                        for tok in o.waits:
                            eng.wait_ge(sems[tok[0]], semval[tok])
                        if o.fn is None:
                            continue
                        ins = o.fn(eng)
                        if o.signal:
                            ins.then_inc(sems[o.key], 16 if o.isdma else 1)
                return body
            if self.streams['pe']:
                block.tensor(run('pe'))
            if self.streams['act']:
                block.scalar(run('act'))
            if self.streams['dve']:
                block.vector(run('dve'))
            if self.streams['pool']:
                block.gpsimd(run('pool'))
            if self.streams['sp']:
                block.sync(run('sp'))


class Rot:
    def __init__(self, name, tiles):
        self.name = name
        self.tiles = tiles
        self.i = 0

    def next(self):
        j = self.i % len(self.tiles)
        self.i += 1
        return self.tiles[j], (self.name, j)


def alibi(n):
    return (2.0 ** (-8.0 * np.arange(1, n + 1, dtype=np.float64) / n))


def split_hi_lo(v):
    v = v.astype(np.float32)
    hi = v.astype(NPBF)
    lo = (v - hi.astype(np.float32)).astype(NPBF)
    return hi, lo


def band_bias(slope_eff, W, width=256):
    k = np.arange(128)[:, None].astype(np.float64)
    col = np.arange(width)[None, :].astype(np.float64)
    diff = col - k
    val = -slope_eff * diff * 8.0
    val = np.where((diff >= 0) & (diff < W), val, NEG)
    return val.astype(np.float32)


def host_consts():
    c = {}
    c['ident'] = np.eye(128, dtype=np.float32).astype(NPBF)
    sl = alibi(16)
    t = np.zeros((128, 16, 2, 256), NPBF)
    for h in range(16):
        hi, lo = split_hi_lo(band_bias(sl[h], 128))
        t[:, h, 0], t[:, h, 1] = hi, lo
    c['bsw'] = t
    sl24 = alibi(24)
    dils = (1, 4, 16)
    t = np.zeros((128, 24, 2, 256), NPBF)
    for g in range(3):
        for hs in range(8):
            hi, lo = split_hi_lo(band_bias(sl24[g * 8 + hs] * dils[g], 129))
            t[:, g * 8 + hs, 0], t[:, g * 8 + hs, 1] = hi, lo
    c['bdl'] = t
    t = np.zeros((128, 16, 2, 256), NPBF)
    for h in range(16):
        hi, lo = split_hi_lo(band_bias(sl[h], 10 ** 9))
        t[:, h, 0], t[:, h, 1] = hi, lo
    c['bmo'] = t
    p = np.arange(128)[:, None, None].astype(np.float64)
    j = np.arange(2)[None, :, None].astype(np.float64)
    c['tqm'] = (-sl[None, None, :] * 8.0 * (j * 128 + p) - 30000.0).astype(np.float32)
    idx = np.arange(31)[None, None, :].astype(np.float64)
    c['dtab'] = np.broadcast_to((-sl[None, :, None] * 2048.0 * (15 - idx)), (128, 16, 31)).astype(np.float32).copy()
    hi, lo = split_hi_lo(np.broadcast_to((sl * 8.0)[None, :], (128, 16)).copy())
    c['slp'] = np.stack([hi, lo], axis=-1)
    X = np.zeros((128, 16, 2, 128), np.float32)
    for n in range(16):
        X[n, n] = 1.0
        X[16 + n, n] = 1.0
    for half in range(2):
        X[32, :, half, :] = np.arange(128)[None, :] + 128 * half
        X[33, :, half, :] = np.arange(128)[None, :] + 128 * half
    c['xsel'] = X.astype(NPBF)
    k = np.arange(128)[:, None]
    col = np.arange(512)[None, :]
    c['mtri'] = np.where(col >= k, 0.0, NEG).astype(np.float32).astype(NPBF)
    inv = 10000.0 ** (-np.arange(0, 32, 2, dtype=np.float64) / 32)
    pos = (np.arange(32)[None, :] * 128 + np.arange(128)[:, None]).astype(np.float64)
    ang = pos[:, :, None] * inv[None, None, :]
    ang = ang.astype(np.float32).astype(np.float64)
    cos, sin = np.cos(ang), np.sin(ang)
    c['ropecs'] = np.concatenate([cos, cos], axis=-1).astype(np.float32)
    c['ropesn'] = np.concatenate([-sin, sin], axis=-1).astype(np.float32)
    return c


CONST_SPECS = {
    'ident': ([128, 128], BF16), 'bsw': ([128, 16, 2, 256], BF16), 'bdl': ([128, 24, 2, 256], BF16),
    'bmo': ([128, 16, 2, 256], BF16), 'tqm': ([128, 2, 16], F32), 'dtab': ([128, 16, 31], F32),
    'slp': ([128, 16, 2], BF16), 'xsel': ([128, 16, 2, 128], BF16), 'mtri': ([128, 512], BF16),
    'ropecs': ([128, 32, 32], F32), 'ropesn': ([128, 32, 32], F32),
}

WEIGHT_SPECS = [
    ('l0_attn_norm', [1024]), ('l0_w_qkv', [1024, 3072]), ('l0_w_o', [1024, 1024]), ('l0_mlp_norm', [1024]),
    ('l0_w_up', [1024, 4096]), ('l0_w_down', [4096, 1024]),
    ('l1_attn_norm', [1024]), ('l1_w_qkv', [1024, 4608]), ('l1_w_o', [512, 1024]), ('l1_mlp_norm', [1024]),
    ('l1_w_up', [1024, 4096]), ('l1_w_down', [4096, 1024]),
    ('l2_attn_norm', [1024]), ('l2_w_dkv', [1024, 1056]), ('l2_q_norm', [768]), ('l2_w_uq', [768, 1536]),
    ('l2_kv_norm', [256]), ('l2_w_ukv', [256, 2048]), ('l2_w_o', [1024, 1024]), ('l2_mlp_norm', [1024]),
    ('l2_w_up', [1024, 4096]), ('l2_w_down', [4096, 1024]),
    ('l3_attn_norm', [1024]), ('l3_w_qkv', [1024, 1280]), ('l3_sinks', [16]), ('l3_w_o', [1024, 1024]),
    ('l3_mlp_norm', [1024]), ('l3_w_up', [1024, 4096]), ('l3_w_down', [4096, 1024]),
    ('final_norm', [1024]),
]


class Builder:
    def __init__(self, layers=(0, 1, 2, 3), final=True):
        self.layers = tuple(layers)
        self.final = final
        nc = self.nc = bass.Bass("TRN2", target_bir_lowering=False)
        self.S = Sched(nc)
        self.x = nc.dram_tensor("x", [SEQ, DM], F32, kind="ExternalInput").ap()
        self.W = {}
        for name, shape in WEIGHT_SPECS:
            if name == 'final_norm' or int(name[1]) in self.layers:
                self.W[name] = nc.dram_tensor(name, shape, F32, kind="ExternalInput").ap()
        self.C = {}
        for name, (shape, dt) in CONST_SPECS.items():
            self.C[name] = nc.dram_tensor("c_" + name, shape, dt, kind="ExternalInput").ap()
        self.y = nc.dram_tensor("y", [SEQ, DM], F32, kind="ExternalOutput").ap()
        self.hbuf = nc.dram_tensor("hbuf", [SEQ, DM], F32, kind="Internal").ap()
        self.oTs = nc.dram_tensor("oTs", [8, 128, SEQ], BF16, kind="Internal").ap()
        self.qTs = nc.dram_tensor("qTs", [16, 96, SEQ], BF16, kind="Internal").ap()
        self.cast_rr = 0

    def used_inputs(self):
        return list(self.W.keys())

    def sb(self, es, name, shape, dt):
        self.uid = getattr(self, 'uid', 0) + 1
        return es.enter_context(self.nc.sbuf_tensor(f"{name}_{self.uid}", shape, dt))

    def mm(self, out, lhsT, rhs, start, stop, reads, writes, skip=False):
        self.S.op('pe', lambda e: e.matmul(out, lhsT=lhsT, rhs=rhs, start=start, stop=stop,
                                           skip_group_check=skip), reads, writes)

    def tr(self, out, in_, reads, writes):
        ident = self.ident
        self.S.op('pe', lambda e: e.transpose(out, in_, ident[:]), list(reads) + ['ident'], writes)

    def act(self, out, in_, func, reads, writes, scale=1.0, bias=None, accum=None):
        kw = {}
        if bias is not None:
            kw['bias'] = bias
        if accum is not None:
            kw['accum_out'] = accum
        self.S.op('act', lambda e: e.activation(out=out, in_=in_, func=func, scale=scale, **kw), reads, writes)

    def dma(self, out, in_, reads, writes, q='sp'):
        self.S.dma(q, lambda e: e.dma_start(out=out, in_=in_), reads, writes)

    def load_cast(self, dst, src, n, reads_src=(), wres=None, shape3=None):
        stg, sres = self.stg.next()
        sv = stg[:, 0:n]
        if shape3 is not None:
            sv = sv.rearrange("p (a b) -> p a b", a=shape3[0])
        self.dma(sv, src, list(reads_src), [sres])
        eng = ('dve', 'pool')[self.cast_rr % 2]
        self.cast_rr += 1
        self.S.op(eng, lambda e: e.tensor_copy(dst, sv), [sres], [wres])

    def rms_stats(self, src_ap, ss_ap, reads, ssres):
        junk, jres = self.junk.next()
        self.act(junk[:], src_ap, AF.Square, reads, [jres, ssres], accum=ss_ap)

    def rms_rstd(self, ss_ap, rstd_ap, ssres, rres, n_feat):
        self.act(rstd_ap, ss_ap, AF.Ln, [ssres, 'epsc'], [rres], scale=1.0 / n_feat, bias=self.epsc[:, 0:1])
        self.act(rstd_ap, rstd_ap, AF.Exp, [rres], [rres], scale=-0.5)

    def phase_norm(self, es, src_dram, gname, xnT):
        S = self.S
        nc = self.nc
        gbc = self.sb(es, "gbc", [128, DM], F32)
        self.dma(gbc[:], self.W[gname].partition_broadcast(128), [], ['gbc'])
        hts = Rot('ht', [self.sb(es, f"ht{i}", [128, DM], F32) for i in range(3)])
        xns = Rot('xn', [self.sb(es, f"xn{i}", [128, DM], BF16) for i in range(2)])
        ss = self.sb(es, "ssn", [128, NT], F32)
        rs = self.sb(es, "rsn", [128, NT], F32)
        for t in range(NT):
            ht, hres = hts.next()
            self.dma(ht[:], src_dram[t * 128:(t + 1) * 128, :], [('h', t // 4)], [hres])
            self.rms_stats(ht[:], ss[:, t:t + 1], [hres], ('ssn', t))
            self.rms_rstd(ss[:, t:t + 1], rs[:, t:t + 1], ('ssn', t), ('rsn', t), DM)
            xn, xres = xns.next()
            S.op('dve', lambda e, xn=xn, ht=ht, t=t: e.scalar_tensor_tensor(
                out=xn[:], in0=ht[:], scalar=rs[:, t:t + 1], in1=gbc[:], op0=ALU.mult, op1=ALU.mult),
                [hres, ('rsn', t), 'gbc'], [xres])
            tp, tres = self.tps.next()
            for k in range(8):
                self.tr(tp[:, k * 128:(k + 1) * 128], xn[:, k * 128:(k + 1) * 128], [xres], [tres])
            self.act(xnT[:, :, t * 128:(t + 1) * 128], tp[:].rearrange("p (k n) -> p k n", k=8), AF.Copy,
                     [tres], [('xnT', t // 4)])

    def phase_oproj(self, li, npair, hsrc):
        S = self.S
        W = self.W
        with contextlib.ExitStack() as es:
            wo = self.sb(es, "wo", [128, npair, DM], BF16)
            self.stg = Rot('stg', [self.sb(es, f"stgo{i}", [128, 1024], F32) for i in range(3)])
            wov = W[f'l{li}_w_o'].rearrange("(k p) n -> p k n", p=128)
            for k in range(npair):
                self.load_cast(wo[:, k, :], wov[:, k, :], DM, wres=('wo', k))
            oTg = Rot('oTg', [self.sb(es, f"oTg{i}", [128, npair, 512], BF16) for i in range(2)])
            hgs = Rot('hgo', [self.sb(es, f"hgo{i}", [128, 4, DM], F32) for i in range(2)])
            for g in range(8):
                og, ores = oTg.next()
                hg, hres = hgs.next()
                self.dma(og[:], self.oTs[0:npair, :, g * 512:(g + 1) * 512].rearrange("a p n -> p a n"),
                         [('oTs', g)], [ores])
                self.dma(hg[:], hsrc[g * 512:(g + 1) * 512, :].rearrange("(t p) n -> p t n", p=128),
                         [('h', g)], [hres])
                for t in range(4):
                    for hf in range(2):
                        pj, pres = self.pjs.next()
                        for k in range(npair):
                            self.mm(pj[:], og[:, k, t * 128:(t + 1) * 128], wo[:, k, hf * 512:(hf + 1) * 512],
                                    k == 0, k == npair - 1, [ores, ('wo', k)], [pres])
                        S.op('dve', lambda e, hg=hg, pj=pj, t=t, hf=hf: e.tensor_tensor(
                            hg[:, t, hf * 512:(hf + 1) * 512], pj[:], hg[:, t, hf * 512:(hf + 1) * 512], ALU.add),
                            [pres, hres], [hres])
                self.dma(self.hbuf[g * 512:(g + 1) * 512, :].rearrange("(t p) n -> p t n", p=128), hg[:],
                         [hres], [('h', g)])
        S.barrier()

    def phase_ffn(self, li, last):
        S = self.S
        W = self.W
        with contextlib.ExitStack() as es:
            wup = self.sb(es, "wup", [128, 8, DFF], BF16)
            wdn = self.sb(es, "wdn", [128, 32, DM], BF16)
            gbc = self.sb(es, "gbcf", [128, DM], F32)
            gfin = self.sb(es, "gfin", [128, DM], F32)
            self.stg = Rot('stg', [self.sb(es, f"stgf{i}", [128, 2048], F32) for i in range(2)])
            self.dma(gbc[:], W[f'l{li}_mlp_norm'].partition_broadcast(128), [], ['gbcf'])
            if last and self.final:
                self.dma(gfin[:], W['final_norm'].partition_broadcast(128), [], ['gfin'])
            wupv = W[f'l{li}_w_up'].rearrange("(k p) n -> p k n", p=128)
            for hf in range(2):
                for k in range(8):
                    self.load_cast(wup[:, k, hf * 2048:(hf + 1) * 2048], wupv[:, k, hf * 2048:(hf + 1) * 2048], 2048,
                                   wres=('wup', k, hf))
            wdnv = W[f'l{li}_w_down'].rearrange("(k p) n -> p k n", p=128)
            for k2 in range(16):
                self.load_cast(wdn[:, 2 * k2:2 * k2 + 2, :], wdnv[:, 2 * k2:2 * k2 + 2, :], 2048,
                               wres=('wdn', k2), shape3=(2, DM))
            hgs = Rot('hg', [self.sb(es, f"hg{i}", [128, 2, DM], F32) for i in range(2)])
            xns = Rot('xnf', [self.sb(es, f"xnf{i}", [128, DM], BF16) for i in range(2)])
            xT = Rot('xT', [self.sb(es, f"xT{i}", [128, 8, 256], BF16) for i in range(1)])
            uTs = Rot('uT', [self.sb(es, f"uT{i}", [128, 8, 256], BF16) for i in range(2)])
            rl = Rot('rl', [self.sb(es, f"rl{i}", [128, 256], F32) for i in range(3)])
            ss = self.sb(es, "ssf", [128, NT], F32)
            rs = self.sb(es, "rsf", [128, NT], F32)
            ssl = self.sb(es, "ssl", [128, NT], F32)
            rsl = self.sb(es, "rsl", [128, NT], F32)
            for g in range(16):
                hg, hres = hgs.next()
                self.dma(hg[:], self.hbuf[g * 256:(g + 1) * 256, :].rearrange("(t p) n -> p t n", p=128),
                         [('h', g // 2)], [hres])
                x2, x2res = xT.next()
                for t in range(2):
                    i = g * 2 + t
                    self.rms_stats(hg[:, t, :], ss[:, i:i + 1], [hres], ('ssf', i))
                    self.rms_rstd(ss[:, i:i + 1], rs[:, i:i + 1], ('ssf', i), ('rsf', i), DM)
                    xn, xres = xns.next()
                    S.op('dve', lambda e, xn=xn, hg=hg, t=t, i=i: e.scalar_tensor_tensor(
                        out=xn[:], in0=hg[:, t, :], scalar=rs[:, i:i + 1], in1=gbc[:], op0=ALU.mult, op1=ALU.mult),
                        [hres, ('rsf', i), 'gbcf'], [xres])
                    tp, tres = self.tps.next()
                    for k in range(8):
                        self.tr(tp[:, k * 128:(k + 1) * 128], xn[:, k * 128:(k + 1) * 128], [xres], [tres])
                    self.act(x2[:, :, t * 128:(t + 1) * 128], tp[:].rearrange("p (k n) -> p k n", k=8), AF.Copy,
                             [tres], [x2res])
                for q in range(4):
                    uT, ures = uTs.next()
                    for cc in range(8):
                        c = q * 8 + cc
                        sc, sres = self.scs.next()
                        for k in range(8):
                            self.mm(sc[:, 0:256], wup[:, k, c * 128:(c + 1) * 128], x2[:, k, :], k == 0, k == 7,
                                    [x2res, ('wup', k, c // 16)], [sres])
                        r, rres = rl.next()
                        self.act(r[:], sc[:, 0:256], AF.Relu, [sres], [rres])
                        S.op('dve', lambda e, r=r, cc=cc, uT=uT: e.tensor_tensor(uT[:, cc, :], r[:], r[:], ALU.mult),
                             [rres], [(ures, cc)])
                    for t in range(2):
                        for hf in range(2):
                            pj, pres = self.pjs.next()
                            for cc in range(8):
                                c = q * 8 + cc
                                self.mm(pj[:], uT[:, cc, t * 128:(t + 1) * 128], wdn[:, c, hf * 512:(hf + 1) * 512],
                                        cc == 0, cc == 7, [(ures, cc), ('wdn', c // 2)], [pres])
                            S.op('dve', lambda e, hg=hg, pj=pj, t=t, hf=hf: e.tensor_tensor(
                                hg[:, t, hf * 512:(hf + 1) * 512], pj[:], hg[:, t, hf * 512:(hf + 1) * 512], ALU.add),
                                [pres, hres], [hres])
                rows = slice(g * 256, (g + 1) * 256)
                if last and self.final:
                    for t in range(2):
                        i = g * 2 + t
                        self.rms_stats(hg[:, t, :], ssl[:, i:i + 1], [hres], ('ssl', i))
                        self.rms_rstd(ssl[:, i:i + 1], rsl[:, i:i + 1], ('ssl', i), ('rsl', i), DM)
                        S.op('dve', lambda e, hg=hg, i=i, t=t: e.scalar_tensor_tensor(
                            out=hg[:, t, :], in0=hg[:, t, :], scalar=rsl[:, i:i + 1], in1=gfin[:],
                            op0=ALU.mult, op1=ALU.mult), [hres, ('rsl', i), 'gfin'], [hres])
                dst = self.y if last else self.hbuf
                self.dma(dst[rows, :].rearrange("(t p) n -> p t n", p=128), hg[:], [hres],
                         ['y'] if last else [('h', g // 2)])
        S.barrier()

    def normalize_out(self, src, sres, ncols, slot, pair, col0, extra=None):
        S = self.S
        orow = slice(0, 64) if slot == 0 else slice(64, 128)
        drow = slice(64, 128) if slot == 0 else slice(0, 64)
        rc, rres = self.rcs.next()
        S.op('dve', lambda e: e.tensor_copy(rc[orow, 0:ncols], src(drow)), [sres], [rres])
        if extra is not None:
            sc_ap, sc_res = extra
            S.op('dve', lambda e: e.tensor_scalar(rc[orow, 0:ncols], rc[orow, 0:ncols], sc_ap(orow), None, ALU.add),
                 [rres, sc_res], [rres])
        S.op('dve', lambda e: e.reciprocal(rc[orow, 0:ncols], rc[orow, 0:ncols]), [rres], [rres])
        on, onres = self.ons.next()
        S.op('dve', lambda e: e.tensor_tensor(on[orow, 0:ncols], src(orow), rc[orow, 0:ncols], ALU.mult),
             [sres, rres], [onres])
        self.dma(self.oTs[pair, orow, col0:col0 + ncols], on[orow, 0:ncols], [onres], [('oTs', col0 // 512)])

    def alloc_attn_common(self, es):
        self.pts = Rot('pt', [self.sb(es, f"pt{i}", [128, 512], BF16) for i in range(4)])
        self.rcs = Rot('rc', [self.sb(es, f"rc{i}", [128, 512], F32) for i in range(2)])
        self.ons = Rot('on', [self.sb(es, f"on{i}", [128, 512], BF16) for i in range(2)])
        self.stg = Rot('stg', [self.sb(es, f"stga{i}", [128, 1024], F32) for i in range(3)])

    def phase_swa(self, li, xnT):
        S = self.S
        W = self.W
        with contextlib.ExitStack() as es:
            self.alloc_attn_common(es)
            bsw = self.sb(es, "bsw", [128, 16, 2, 256], BF16)
            self.dma(bsw[:], self.C['bsw'], [], ['bsw'])
            esink = self.sb(es, "esink", [128, 16], F32)
            self.dma(esink[:], W[f'l{li}_sinks'].partition_broadcast(128), [], ['esink'])
            self.act(esink[:], esink[:], AF.Exp, ['esink'], ['esink'])
            wkv = self.sb(es, "wkv", [128, 8, 256], BF16)
            wqs = Rot('wq', [self.sb(es, f"wq{i}", [128, 8, 128], BF16) for i in range(2)])
            kTv = self.sb(es, "kTv", [128, 2, 2, SEQ], BF16)
            vaug = self.sb(es, "vaug", [128, NT, 2, 2, 128], BF16)
            qTs = Rot('qT', [self.sb(es, f"qT{i}", [128, SEQ], BF16) for i in range(2)])
            wv = W[f'l{li}_w_qkv'].rearrange("(k p) n -> p k n", p=128)
            self.load_cast(wkv[:, :, 0:128], wv[:, :, 1024:1152], 1024, wres='wkv', shape3=(8, 128))
            self.load_cast(wkv[:, :, 128:256], wv[:, :, 1152:1280], 1024, wres='wkv2', shape3=(8, 128))
            S.op('pool', lambda e: e.memset(vaug[:, :, :, 0, 64:128], 1.0), [], ['vaug1'])
            S.op('pool', lambda e: e.memset(vaug[:, :, :, 1, 0:64], 1.0), [], ['vaug1'])
            for kvh in range(2):
                S.op('pool', lambda e, kvh=kvh: e.memset(kTv[64:128, kvh, 0, :], 0.0), [], ['kTz'])
                S.op('pool', lambda e, kvh=kvh: e.memset(kTv[0:64, kvh, 1, :], 0.0), [], ['kTz'])
            for c in range(8):
                pj, pres = self.pjs.next()
                cs = slice(c * 512, (c + 1) * 512)
                for k in range(8):
                    self.mm(pj[:], wkv[:, k, 0:128], xnT[:, k, cs], k == 0, k == 7, ['wkv', ('xnT', c)], [pres])
                self.act(kTv[0:64, 0, 0, cs], pj[0:64, :], AF.Copy, [pres, 'kTz'], [('kT', c)])
                self.act(kTv[64:128, 1, 1, cs], pj[64:128, :], AF.Copy, [pres, 'kTz'], [('kT', c)])
                S.op('dve', lambda e, pj=pj, cs=cs: e.tensor_copy(kTv[64:128, 0, 1, cs], pj[0:64, :]),
                     [pres, 'kTz'], [('kT', c)])
                S.op('dve', lambda e, pj=pj, cs=cs: e.tensor_copy(kTv[0:64, 1, 0, cs], pj[64:128, :]),
                     [pres, 'kTz'], [('kT', c)])
            for t in range(NT):
                pj, pres = self.pjs.next()
                for k in range(8):
                    self.mm(pj[:, 0:128], xnT[:, k, t * 128:(t + 1) * 128], wkv[:, k, 128:256], k == 0, k == 7,
                            ['wkv2', ('xnT', t // 4)], [pres])
                for kvh in range(2):
                    self.act(vaug[:, t, kvh, 0, 0:64], pj[:, kvh * 64:(kvh + 1) * 64], AF.Copy, [pres, 'vaug1'],
                             [('vaug', t)])
                    S.op('dve', lambda e, pj=pj, t=t, kvh=kvh: e.tensor_copy(
                        vaug[:, t, kvh, 1, 64:128], pj[:, kvh * 64:(kvh + 1) * 64]), [pres, 'vaug1'], [('vaug', t)])
            for p in range(8):
                wq, wqres = wqs.next()
                self.load_cast(wq[:], wv[:, :, p * 128:(p + 1) * 128], 1024, wres=wqres, shape3=(8, 128))
                qT, qres = qTs.next()
                for c in range(8):
                    pj, pres = self.pjs.next()
                    for k in range(8):
                        self.mm(pj[:], wq[:, k, :], xnT[:, k, c * 512:(c + 1) * 512], k == 0, k == 7,
                                [wqres, ('xnT', c)], [pres])
                    self.act(qT[:, c * 512:(c + 1) * 512], pj[:], AF.Copy, [pres], [(qres, c)])
                for slot in range(2):
                    h = 2 * p + slot
                    kvh = h // 8
                    for c in range(8):
                        oa, ores = self.oas.next()
                        kts = [kt for kt in range(4 * c - 1, 4 * c + 4) if kt >= 0]
                        for si, kt in enumerate(kts):
                            qts = [qt for qt in (kt, kt + 1) if 4 * c <= qt <= 4 * c + 3]
                            col0 = (qts[0] - 4 * c) * 128
                            n = 128 * len(qts)
                            boff = 0 if qts[0] == kt else 128
                            sc, sres = self.scs.next()
                            self.mm(sc[:, 0:n], kTv[:, kvh, slot, kt * 128:(kt + 1) * 128],
                                    qT[:, c * 512 + col0:c * 512 + col0 + n], True, False,
                                    [('kT', kt // 4), 'kTz', (qres, c)], [sres])
                            self.mm(sc[:, 0:n], self.ident[:], bsw[:, h, 0, boff:boff + n], False, False,
                                    ['ident', 'bsw'], [sres])
                            self.mm(sc[:, 0:n], self.ident[:], bsw[:, h, 1, boff:boff + n], False, True,
                                    ['ident', 'bsw'], [sres])
                            pt, ptres = self.pts.next()
                            self.act(pt[:, 0:n], sc[:, 0:n], AF.Exp, [sres], [ptres], scale=0.125)
                            self.mm(oa[:, col0:col0 + n], vaug[:, kt, kvh, slot, :], pt[:, 0:n], si == 0,
                                    si == len(kts) - 1, [('vaug', kt), 'vaug1', ptres], [ores], skip=True)
                        self.normalize_out(lambda rows, oa=oa: oa[rows, 0:512], ores, 512, slot, p, c * 512,
                                           extra=(lambda rows, h=h: esink[rows, h:h + 1], 'esink'))
        S.barrier()

    def alloc_pair(self, es):
        S = self.S
        self.wqkv = Rot('wqkv', [self.sb(es, f"wqkv{i}", [128, 8, 384], BF16) for i in range(2)])
        self.qT = self.sb(es, "qTp", [128, SEQ], BF16)
        self.kTz = self.sb(es, "kTz", [128, 2, SEQ], BF16)
        self.vaug = self.sb(es, "vaugp", [128, NT, 2, 128], BF16)
        self.vTs = Rot('vT', [self.sb(es, f"vT{i}", [128, 512], BF16) for i in range(2)])
        S.op('pool', lambda e: e.memset(self.kTz[64:128, 0, :], 0.0), [], ['kTzz'])
        S.op('pool', lambda e: e.memset(self.kTz[0:64, 1, :], 0.0), [], ['kTzz'])
        S.op('pool', lambda e: e.memset(self.vaug[:, :, 0, 64:128], 1.0), [], ['vaug1'])
        S.op('pool', lambda e: e.memset(self.vaug[:, :, 1, 0:64], 1.0), [], ['vaug1'])

    def pair_proj(self, wv, qc, kc, vc, xnT, colmap=None):
        S = self.S
        w, wres = self.wqkv.next()
        for j, c0 in enumerate((qc, kc, vc)):
            self.load_cast(w[:, :, j * 128:(j + 1) * 128], wv[:, :, c0:c0 + 128], 1024, wres=(wres, j), shape3=(8, 128))
        qT, kTz, vaug = self.qT, self.kTz, self.vaug
        for c in range(8):
            cs = slice(c * 512, (c + 1) * 512)
            xres = ('xnT', c) if colmap is None else 'xnTall'
            outs = []
            for j in range(3):
                pj, pres = self.pjs.next()
                for k in range(8):
                    rhs = xnT[:, k, cs] if colmap is None else colmap(k, c)
                    o = pj[:] if len(rhs.shape) == 2 else pj[:].rearrange("p (a b) -> p a b", a=rhs.shape[1])
                    self.mm(o, w[:, k, j * 128:(j + 1) * 128], rhs, k == 0, k == 7,
                            [(wres, j)] + ([xres] if colmap is None else [('xnT', cc) for cc in range(8)]), [pres])
                if j == 0:
                    self.act(qT[:, cs], pj[:], AF.Copy, [pres], [('qT', c)])
                elif j == 1:
                    self.act(kTz[0:64, 0, cs], pj[0:64, :], AF.Copy, [pres, 'kTzz'], [('kT', c)])
                    S.op('dve', lambda e, pj=pj, cs=cs: e.tensor_copy(kTz[64:128, 1, cs], pj[64:128, :]),
                         [pres, 'kTzz'], [('kT', c)])
                else:
                    vT, vres = self.vTs.next()
                    self.act(vT[:], pj[:], AF.Copy, [pres], [vres])
                    tp, tres = self.tps.next()
                    for jj in range(4):
                        self.tr(tp[:, jj * 128:(jj + 1) * 128], vT[:, jj * 128:(jj + 1) * 128], [vres], [tres])
                    tv = tp[:, 0:512].rearrange("p (a b) -> p a b", a=4)
                    self.act(vaug[:, 4 * c:4 * c + 4, 0, 0:64], tv[:, :, 0:64], AF.Copy, [tres, 'vaug1'],
                             [('vaug', c)])
                    S.op('dve', lambda e, tv=tv, c=c: e.tensor_copy(vaug[:, 4 * c:4 * c + 4, 1, 64:128],
                                                                  tv[:, :, 64:128]), [tres, 'vaug1'], [('vaug', c)])

    def phase_dilated(self, li, xnT):
        S = self.S
        W = self.W
        dils = (1, 4, 16)
        with contextlib.ExitStack() as es:
            self.alloc_attn_common(es)
            self.alloc_pair(es)
            bdl = self.sb(es, "bdl", [128, 24, 2, 256], BF16)
            self.dma(bdl[:], self.C['bdl'], [], ['bdl'])
            acc = [self.sb(es, f"acc{i}", [128, SEQ], F32) for i in range(2)]
            wv = W[f'l{li}_w_qkv'].rearrange("(k p) n -> p k n", p=128)
            qT, kTz, vaug = self.qT, self.kTz, self.vaug
            for pp in range(4):
                for g in range(3):
                    d = dils[g]
                    Sd = SEQ // d
                    nseg = Sd // 128

                    def colmap(k, c, d=d, Sd=Sd):
                        if d == 1:
                            return xnT[:, k, c * 512:(c + 1) * 512]
                        if Sd >= 512:
                            r, i0 = divmod(c * 512, Sd)
                            st = r + d * i0
                            return xnT[:, k, st:st + d * 511 + 1:d]
                        v = xnT[:, k, :].rearrange("p (j d) -> p d j", d=d)
                        nr = 512 // Sd
                        return v[:, c * nr:(c + 1) * nr, :]

                    base = (g * 3) * 512
                    self.pair_proj(wv, base + pp * 128, base + 512 + pp * 128, base + 1024 + pp * 128, xnT,
                                   colmap=(None if d == 1 else colmap))
                    for slot in range(2):
                        hs = 2 * pp + slot
                        hb = g * 8 + hs
                        for c in range(8):
                            oa, ores = self.oas.next()
                            steps = []
                            for kt in range(4 * c - 1, 4 * c + 4):
                                if kt < 0:
                                    continue
                                qts = [kt] + ([kt + 1] if (kt + 1) % nseg != 0 else [])
                                qts = [qt for qt in qts if 4 * c <= qt <= 4 * c + 3]
                                if qts:
                                    steps.append((kt, qts))
                            for si, (kt, qts) in enumerate(steps):
                                col0 = (qts[0] - 4 * c) * 128
                                n = 128 * len(qts)
                                boff = 0 if qts[0] == kt else 128
                                sc, sres = self.scs.next()
                                self.mm(sc[:, 0:n], kTz[:, slot, kt * 128:(kt + 1) * 128],
                                        qT[:, c * 512 + col0:c * 512 + col0 + n], True, False,
                                        [('kT', kt // 4), 'kTzz', ('qT', c)], [sres])
                                self.mm(sc[:, 0:n], self.ident[:], bdl[:, hb, 0, boff:boff + n], False, False,
                                        ['ident', 'bdl'], [sres])
                                self.mm(sc[:, 0:n], self.ident[:], bdl[:, hb, 1, boff:boff + n], False, True,
                                        ['ident', 'bdl'], [sres])
                                pt, ptres = self.pts.next()
                                self.act(pt[:, 0:n], sc[:, 0:n], AF.Exp, [sres], [ptres], scale=0.125)
                                self.mm(oa[:, col0:col0 + n], vaug[:, kt, slot, :], pt[:, 0:n], si == 0,
                                        si == len(steps) - 1, [('vaug', kt // 4), 'vaug1', ptres], [ores], skip=True)
                            A = acc[slot]
                            ares = ('acc', slot, c) if d == 1 else ('accall', slot)
                            if d == 1:
                                self.act(A[:, c * 512:(c + 1) * 512], oa[:], AF.Copy, [ores, ('accall', slot)],
                                         [('acc', slot, c)])
                            else:
                                if Sd >= 512:
                                    r, i0 = divmod(c * 512, Sd)
                                    st = r + d * i0
                                    pieces = [(A[:, st:st + d * 511 + 1:d], oa[:, 0:512])]
                                else:
                                    nr = 512 // Sd
                                    pieces = []
                                    for rr in range(nr):
                                        r = c * nr + rr
                                        pieces.append((A[:, r:r + d * (Sd - 1) + 1:d], oa[:, rr * Sd:(rr + 1) * Sd]))
                                for (av, ov) in pieces:
                                    S.op('dve', lambda e, av=av, ov=ov: e.tensor_tensor(av, ov, av, ALU.add),
                                         [ores] + [('acc', slot, cc) for cc in range(8)], [('accall', slot)])
                for slot in range(2):
                    A = acc[slot]
                    for c in range(8):
                        self.normalize_out(lambda rows, A=A, c=c: A[rows, c * 512:(c + 1) * 512], ('accall', slot),
                                           512, slot, pp, c * 512)
        S.barrier()

    def phase_moba(self, li, xnT):
        S = self.S
        W = self.W
        C = self.C
        with contextlib.ExitStack() as es:
            self.alloc_attn_common(es)
            self.alloc_pair(es)
            bmo = self.sb(es, "bmo", [128, 16, 2, 256], BF16)
            self.dma(bmo[:], C['bmo'], [], ['bmo'])
            xsel = self.sb(es, "xsel", [128, 16, 2, 128], BF16)
            self.dma(xsel[:], C['xsel'], [], ['xsel'])
            tqm = self.sb(es, "tqm", [128, 2, 16], F32)
            self.dma(tqm[:], C['tqm'], [], ['tqm'])
            dtab = self.sb(es, "dtab", [128, 16, 31], F32)
            self.dma(dtab[:], C['dtab'], [], ['dtab'])
            slp = self.sb(es, "slp", [128, 16, 2], BF16)
            self.dma(slp[:], C['slp'], [], ['slp'])
            c30 = self.sb(es, "c30", [128, 16], F32)
            S.op('dve', lambda e: e.memset(c30[:], 30000.0), [], ['c30'])
            kmf = self.sb(es, "kmf", [128, 16], F32)
            kmT = self.sb(es, "kmT", [128, 2, 16], BF16)
            gss = Rot('gs', [self.sb(es, f"gs{i}", [128, 16], F32) for i in range(2)])
            t8s = Rot('t8', [self.sb(es, f"t8{i}", [128, 8], F32) for i in range(2)])
            s3s = Rot('s3', [self.sb(es, f"s3{i}", [128, 16], F32) for i in range(2)])
            vvs = Rot('vv', [self.sb(es, f"vv{i}", [128, 16], F32) for i in range(2)])
            yps = Rot('yp', [self.sb(es, f"yp{i}", [128, 128], BF16) for i in range(2)])
            for i in range(2):
                S.op('pool', lambda e, i=i: e.memset(yps.tiles[i][:], 0.0), [], [('yp', i)])
            Ys = Rot('Y', [self.sb(es, f"Y{i}", [128, 256], BF16) for i in range(2)])
            wv = W[f'l{li}_w_qkv'].rearrange("(k p) n -> p k n", p=128)
            qT, kTz, vaug = self.qT, self.kTz, self.vaug
            allk = [('kT', c) for c in range(8)]
            for p in range(8):
                self.pair_proj(wv, p * 128, 1024 + p * 128, 2048 + p * 128, xnT)
                for slot in range(2):
                    h = 2 * p + slot
                    S.op('dve', lambda e, slot=slot: e.tensor_reduce(
                        out=kmf[:], in_=kTz[:, slot, :].rearrange("p (n k) -> p n k", k=256), axis=AX.X, op=ALU.add),
                        allk + ['kTzz'], ['kmf'])
                    S.op('dve', lambda e, slot=slot: e.tensor_scalar(kmT[:, slot, :], kmf[:], 1.0 / 256, None, ALU.mult),
                         ['kmf'], [('kmT', slot)])
                    for b in range(16):
                        Y, Yres = (None, None)
                        if b >= 1:
                            Y, Yres = Ys.next()
                            for j in range(2):
                                tcols = slice(b * 256 + j * 128, b * 256 + (j + 1) * 128)
                                if b > 3:
                                    pj, pres = self.pjs.next()
                                    self.mm(pj[:, 0:16], qT[:, tcols], kmT[:, slot, :], True, True,
                                            [('qT', b // 2), ('kmT', slot)], [pres])
                                    gs, gres = gss.next()
                                    S.op('dve', lambda e, gs=gs, pj=pj: e.tensor_copy(gs[:], pj[:, 0:16]), [pres], [gres])
                                    S.op('dve', lambda e, gs=gs, b=b: e.memset(gs[:, b:16], -1e30), [], [gres])
                                    t8, t8res = t8s.next()
                                    S.op('dve', lambda e, gs=gs, t8=t8: e.max(t8[:], gs[:]), [gres], [t8res])
                                    s3, s3res = s3s.next()
                                    S.op('dve', lambda e, gs=gs, t8=t8, s3=s3: e.tensor_scalar(
                                        s3[:], gs[:], t8[:, 2:3], 30000.0, ALU.is_ge, ALU.mult), [gres, t8res], [s3res])
                                else:
                                    s3, s3res = c30, 'c30'
                                vv, vres = vvs.next()
                                S.op('dve', lambda e, s3=s3, vv=vv, j=j, h=h, b=b: e.scalar_tensor_tensor(
                                    out=vv[:], in0=s3[:], scalar=tqm[:, j, h:h + 1], in1=dtab[:, h, 15 - b:31 - b],
                                    op0=ALU.add, op1=ALU.add), [s3res, 'tqm', 'dtab'], [vres])
                                yp, ypres = yps.next()
                                S.op('dve', lambda e, yp=yp, vv=vv: e.tensor_copy(yp[:, 0:16], vv[:]), [vres], [ypres])
                                S.op('dve', lambda e, yp=yp, vv=vv: e.tensor_tensor(yp[:, 16:32], vv[:], yp[:, 0:16],
                                                                                  ALU.subtract), [vres, ypres], [ypres])
                                S.op('dve', lambda e, yp=yp, h=h: e.tensor_copy(yp[:, 32:34], slp[:, h, :]),
                                     ['slp'], [ypres])
                                tp, tres = self.tps.next()
                                self.tr(tp[:, 0:128], yp[:], [ypres], [tres])
                                S.op('dve', lambda e, Y=Y, tp=tp, j=j: e.tensor_copy(Y[:, j * 128:(j + 1) * 128],
                                                                                 tp[:, 0:128]), [tres], [Yres])
                        oa, ores = self.oas.next()
                        qcols = slice(b * 256, (b + 1) * 256)
                        first = True
                        for n in range(b):
                            for half in range(2):
                                kt = 2 * n + half
                                sc, sres = self.scs.next()
                                self.mm(sc[:, 0:256], kTz[:, slot, kt * 128:(kt + 1) * 128], qT[:, qcols], True, False,
                                        [('kT', kt // 4), 'kTzz', ('qT', b // 2)], [sres])
                                self.mm(sc[:, 0:256], xsel[:, n, half, :], Y[:, :], False, True, ['xsel', Yres], [sres])
                                pt, ptres = self.pts.next()
                                self.act(pt[:, 0:256], sc[:, 0:256], AF.Exp, [sres], [ptres], scale=0.125)
                                self.mm(oa[:, 0:256], vaug[:, kt, slot, :], pt[:, 0:256], first, False,
                                        [('vaug', kt // 4), 'vaug1', ptres], [ores], skip=True)
                                first = False
                        for half in range(2):
                            kt = 2 * b + half
                            n = 256 - 128 * half
                            qc = slice(b * 256 + 128 * half, (b + 1) * 256)
                            sc, sres = self.scs.next()
                            self.mm(sc[:, 0:n], kTz[:, slot, kt * 128:(kt + 1) * 128], qT[:, qc], True, False,
                                    [('kT', kt // 4), 'kTzz', ('qT', b // 2)], [sres])
                            self.mm(sc[:, 0:n], self.ident[:], bmo[:, h, 0, 0:n], False, False, ['ident', 'bmo'], [sres])
                            self.mm(sc[:, 0:n], self.ident[:], bmo[:, h, 1, 0:n], False, True, ['ident', 'bmo'], [sres])
                            pt, ptres = self.pts.next()
                            self.act(pt[:, 0:n], sc[:, 0:n], AF.Exp, [sres], [ptres], scale=0.125)
                            self.mm(oa[:, 128 * half:256], vaug[:, kt, slot, :], pt[:, 0:n], first, half == 1,
                                    [('vaug', kt // 4), 'vaug1', ptres], [ores], skip=True)
                            first = False
                        self.normalize_out(lambda rows, oa=oa: oa[rows, 0:256], ores, 256, slot, p, b * 256)
        S.barrier()

    def phase_mla(self, li, xnT):
        S = self.S
        W = self.W
        C = self.C
        with contextlib.ExitStack() as es:
            ckvT = self.sb(es, "ckvT", [128, 2, SEQ], BF16)
            krT = self.sb(es, "krT", [32, SEQ], BF16)
            wukv = self.sb(es, "wukv", [128, 2, 2048], BF16)
            mtri = self.sb(es, "mtri", [128, 512], BF16)
            self.dma(mtri[:], C['mtri'], [], ['mtri'])
            A, Ares = self.pjs.tiles[0], ('pj', 0)
            B, Bres = self.pjs.tiles[1], ('pj', 1)
            Cb, Cres = self.scs.tiles[0], ('sc', 0)
            Q = [(self.oas.tiles[0], ('oa', 0)), (self.oas.tiles[1], ('oa', 1)), (self.scs.tiles[1], ('sc', 1))]
            tpA, tAres = self.tps.tiles[0], ('tp', 0)
            tpB, tBres = self.tps.tiles[1], ('tp', 1)
            with contextlib.ExitStack() as es1:
                self.stg = Rot('stg', [self.sb(es1, f"stgm{i}", [128, 1024], F32) for i in range(2)])
                wdkv = self.sb(es1, "wdkv", [128, 8, 1056], BF16)
                wuq = self.sb(es1, "wuq", [128, 6, 1536], BF16)
                wd = W[f'l{li}_w_dkv'].rearrange("(k p) n -> p k n", p=128)
                for k in range(8):
                    self.load_cast(wdkv[:, k, 0:1024], wd[:, k, 0:1024], 1024, wres=('wdkv', k))
                if not DBG.get('skip_wdkvr'):
                    self.load_cast(wdkv[:, :, 1024:1056], wd[:, :, 1024:1056], 256, wres='wdkvr', shape3=(8, 32))
                wq = W[f'l{li}_w_uq'].rearrange("(k p) n -> p k n", p=128)
                for k in range(6):
                    self.load_cast(wuq[:, k, 0:1024], wq[:, k, 0:1024], 1024, wres=('wuq', k))
                    self.load_cast(wuq[:, k, 1024:1536], wq[:, k, 1024:1536], 512, wres=('wuq2', k))
                wk = W[f'l{li}_w_ukv'].rearrange("(k p) n -> p k n", p=128)
                for k in range(2):
                    for hf in range(2):
                        self.load_cast(wukv[:, k, hf * 1024:(hf + 1) * 1024], wk[:, k, hf * 1024:(hf + 1) * 1024], 1024,
                                       wres=('wukv', k, hf))
                qg = self.sb(es1, "qg", [128, 768], F32)
                kvg = self.sb(es1, "kvg", [128, 256], F32)
                if not DBG.get('skip_g'):
                    self.dma(qg[:], W[f'l{li}_q_norm'].partition_broadcast(128), [], ['qg'])
                    self.dma(kvg[:], W[f'l{li}_kv_norm'].partition_broadcast(128), [], ['kvg'])
                cs = self.sb(es1, "ropecs", [128, 32, 32], F32)
                sn = self.sb(es1, "ropesn", [128, 32, 32], F32)
                if not DBG.get('skip_rope'):
                    self.dma(cs[:], C['ropecs'], [], ['ropecs'])
                    self.dma(sn[:], C['ropesn'], [], ['ropesn'])
                cqns = Rot('cqn', [self.sb(es1, f"cqn{i}", [128, 768], BF16) for i in range(2)])
                ckvns = Rot('ckvn', [self.sb(es1, f"ckvn{i}", [128, 256], BF16) for i in range(2)])
                krrs = Rot('krr', [self.sb(es1, f"krr{i}", [128, 128], BF16) for i in range(2)])
                for i in range(2):
                    S.op('pool', lambda e, i=i: e.memset(krrs.tiles[i][:], 0.0), [], [('krr', i)])
                ra = self.sb(es1, "ra", [128, 32], F32)
                rb = self.sb(es1, "rb", [128, 32], F32)
                cqTs = Rot('cqT', [self.sb(es1, f"cqT{i}", [128, 6, 128], BF16) for i in range(2)])
                qf = self.sb(es1, "qf", [128, 1536], F32)
                qb = self.sb(es1, "qb", [128, 1536], BF16)
                ta = self.sb(es1, "ta", [128, 16, 32], F32)
                tb = self.sb(es1, "tb", [128, 16, 32], F32)
                qst = self.sb(es1, "qst", [96, 16, 512], BF16)
                ssa = self.sb(es1, "ssa", [128, NT], F32)
                ssb = self.sb(es1, "ssb", [128, NT], F32)
                ssk = self.sb(es1, "ssk", [128, NT], F32)
                rsq = self.sb(es1, "rsq", [128, NT], F32)
                rsk = self.sb(es1, "rsk", [128, NT], F32)
                for t in range(DBG.get('mla_nt', NT)):
                    ts = slice(t * 128, (t + 1) * 128)
                    t1 = slice(t, t + 1)
                    for (dst, dres, c0, c1) in ((A, Ares, 0, 512), (B, Bres, 512, 768), (Cb, Cres, 768, 1056)):
                        for k in range(8):
                            self.mm(dst[:, 0:c1 - c0], xnT[:, k, ts], wdkv[:, k, c0:c1], k == 0, k == 7,
                                    [('xnT', t // 4), ('wdkv', k), 'wdkvr'], [dres])
                    if DBG.get('mla_step', 99) <= 1:
                        continue
                    junk, jres = self.junk.next()
                    self.act(junk[:, 0:512], A[:], AF.Square, [Ares], [jres, ('ssa', t)], accum=ssa[:, t1])
                    junk, jres = self.junk.next()
                    self.act(junk[:, 0:256], B[:, 0:256], AF.Square, [Bres], [jres, ('ssb', t)], accum=ssb[:, t1])
                    junk, jres = self.junk.next()
                    self.act(junk[:, 0:256], Cb[:, 0:256], AF.Square, [Cres], [jres, ('ssk', t)], accum=ssk[:, t1])
                    S.op('dve', lambda e, t1=t1: e.tensor_tensor(ssa[:, t1], ssa[:, t1], ssb[:, t1], ALU.add),
                         [('ssa', t), ('ssb', t)], [('ssa', t)])
                    self.rms_rstd(ssa[:, t1], rsq[:, t1], ('ssa', t), ('rsq', t), 768)
                    self.rms_rstd(ssk[:, t1], rsk[:, t1], ('ssk', t), ('rsk', t), 256)
                    if DBG.get('mla_step', 99) <= 2:
                        continue
                    cqn, cqres = cqns.next()
                    ckvn, ckres = ckvns.next()
                    S.op('dve', lambda e, cqn=cqn, t1=t1: e.scalar_tensor_tensor(
                        out=cqn[:, 0:512], in0=A[:], scalar=rsq[:, t1], in1=qg[:, 0:512], op0=ALU.mult, op1=ALU.mult),
                        [Ares, ('rsq', t), 'qg'], [cqres])
                    S.op('dve', lambda e, cqn=cqn, t1=t1: e.scalar_tensor_tensor(
                        out=cqn[:, 512:768], in0=B[:, 0:256], scalar=rsq[:, t1], in1=qg[:, 512:768],
                        op0=ALU.mult, op1=ALU.mult), [Bres, ('rsq', t), 'qg'], [cqres])
                    S.op('dve', lambda e, ckvn=ckvn, t1=t1: e.scalar_tensor_tensor(
                        out=ckvn[:], in0=Cb[:, 0:256], scalar=rsk[:, t1], in1=kvg[:], op0=ALU.mult, op1=ALU.mult),
                        [Cres, ('rsk', t), 'kvg'], [ckres])
                    if DBG.get('mla_step', 99) <= 3:
                        continue
                    krr, krres = krrs.next()
                    S.op('dve', lambda e, t=t: e.tensor_tensor(ra[:], Cb[:, 256:288], cs[:, t, :], ALU.mult),
                         [Cres, 'ropecs'], ['ra'])
                    S.op('dve', lambda e, t=t: e.tensor_tensor(rb[:, 0:16], Cb[:, 272:288], sn[:, t, 0:16], ALU.mult),
                         [Cres, 'ropesn'], ['rb'])
                    S.op('dve', lambda e, t=t: e.tensor_tensor(rb[:, 16:32], Cb[:, 256:272], sn[:, t, 16:32], ALU.mult),
                         [Cres, 'ropesn'], ['rb'])
                    S.op('dve', lambda e, krr=krr: e.tensor_tensor(krr[:, 0:32], ra[:], rb[:], ALU.add),
                         ['ra', 'rb'], [krres])
                    if DBG.get('mla_step', 99) <= 4:
                        continue
                    m5 = DBG.get('mla5', 7)
                    cqT, cqTres = cqTs.next()
                    if m5 & 1:
                        for k in range(6):
                            self.tr(tpA[:, k * 128:(k + 1) * 128], cqn[:, k * 128:(k + 1) * 128], [cqres], [tAres])
                    if m5 & 2:
                        for k in range(2):
                            self.tr(tpA[:, 768 + k * 128:768 + (k + 1) * 128], ckvn[:, k * 128:(k + 1) * 128], [ckres],
                                    [tAres])
                    if m5 & 4:
                        self.tr(tpB[:, 0:128], krr[:], [krres], [tBres])
                    if m5 & 1:
                        self.act(cqT[:], tpA[:, 0:768].rearrange("p (k n) -> p k n", k=6), AF.Copy, [tAres], [cqTres])
                    if m5 & 2:
                        self.act(ckvT[:, :, ts], tpA[:, 768:1024].rearrange("p (k n) -> p k n", k=2), AF.Copy,
                                 [tAres], [('ckvT', t // 4)])
                    if m5 & 4:
                        S.op('dve', lambda e, ts=ts: e.tensor_copy(krT[0:32, ts], tpB[0:32, 0:128]), [tBres],
                             [('krT', t // 4)])
                    if DBG.get('mla_step', 99) <= 5:
                        continue
                    for j in range(3):
                        Qj, Qres = Q[j]
                        for k in range(6):
                            self.mm(Qj[:], cqT[:, k, :], wuq[:, k, j * 512:(j + 1) * 512], k == 0, k == 5,
                                    [cqTres, ('wuq', k), ('wuq2', k)], [Qres])
                        self.act(qf[:, j * 512:(j + 1) * 512], Qj[:], AF.Copy, [Qres], ['qf'])
                    if DBG.get('mla_step', 99) <= 6:
                        continue
                    qv = qf[:].rearrange("p (h d) -> p h d", h=16)
                    qbv = qb[:].rearrange("p (h d) -> p h d", h=16)
                    csb = cs[:, t:t + 1, :].broadcast_to([128, 16, 32])
                    snb = sn[:, t:t + 1, :].broadcast_to([128, 16, 32])
                    S.op('dve', lambda e, qv=qv, csb=csb: e.tensor_tensor(ta[:], qv[:, :, 64:96], csb, ALU.mult),
                         ['qf', 'ropecs'], ['ta'])
                    S.op('dve', lambda e, qv=qv, snb=snb: e.tensor_tensor(tb[:, :, 0:16], qv[:, :, 80:96],
                                                                      snb[:, :, 0:16], ALU.mult),
                         ['qf', 'ropesn'], ['tb'])
                    S.op('dve', lambda e, qv=qv, snb=snb: e.tensor_tensor(tb[:, :, 16:32], qv[:, :, 64:80],
                                                                      snb[:, :, 16:32], ALU.mult),
                         ['qf', 'ropesn'], ['tb'])
                    S.op('pool', lambda e, qv=qv, qbv=qbv: e.tensor_copy(qbv[:, :, 0:64], qv[:, :, 0:64]), ['qf'], ['qb'])
                    S.op('dve', lambda e, qbv=qbv: e.tensor_tensor(qbv[:, :, 64:96], ta[:], tb[:], ALU.add),
                         ['ta', 'tb'], ['qb'])
                    if DBG.get('mla_step', 99) <= 7:
                        continue
                    for hh in range(16):
                        tp_, tr_ = (tpA, tAres) if hh < 8 else (tpB, tBres)
                        self.tr(tp_[0:96, (hh % 8) * 128:(hh % 8 + 1) * 128], qb[:, hh * 96:(hh + 1) * 96], ['qb'], [tr_])
                    tsub = t % 4
                    self.act(qst[:, 0:8, tsub * 128:(tsub + 1) * 128], tpA[0:96, :].rearrange("p (h n) -> p h n", h=8),
                             AF.Copy, [tAres], ['qst'])
                    S.op('dve', lambda e, tsub=tsub: e.tensor_copy(qst[:, 8:16, tsub * 128:(tsub + 1) * 128],
                                                                 tpB[0:96, :].rearrange("p (h n) -> p h n", h=8)),
                         [tBres], ['qst'])
                    if DBG.get('mla_step', 99) <= 8:
                        continue
                    if tsub == 3:
                        g = t // 4
                        self.dma(self.qTs[:, :, g * 512:(g + 1) * 512].rearrange("h r n -> r h n"), qst[:], ['qst'],
                                 [('qTs', g)])
            S.barrier()
            with contextlib.ExitStack() as es2:
                self.alloc_attn_common(es2)
                if DBG.get('mla_stage1_only'):
                    return
                qThs = Rot('qTh', [self.sb(es2, f"qTh{i}", [96, SEQ], BF16) for i in range(2)])
                kThs = Rot('kTh', [self.sb(es2, f"kTh{i}", [96, SEQ], BF16) for i in range(2)])
                vaugs = [self.sb(es2, f"vaugm{i}", [128, NT, 128], BF16) for i in range(2)]
                S.op('pool', lambda e: e.memset(vaugs[0][:, :, 64:128], 1.0), [], [('vaugm1', 0)])
                S.op('pool', lambda e: e.memset(vaugs[1][:, :, 0:64], 1.0), [], [('vaugm1', 1)])
                vTz = [Rot(f'vTz{sl}', [self.sb(es2, f"vTz{sl}_{i}", [128, 512], BF16) for i in range(2)])
                       for sl in range(2)]
                for sl in range(2):
                    for i in range(2):
                        zr = slice(64, 128) if sl == 0 else slice(0, 64)
                        S.op('pool', lambda e, sl=sl, i=i, zr=zr: e.memset(vTz[sl].tiles[i][zr, :], 0.0), [],
                             [(f'vTz{sl}z', i)])
                scale = 96.0 ** -0.5
                for h in range(16):
                    slot = h % 2
                    qTh, qres = qThs.next()
                    self.dma(qTh[:], self.qTs[h], [('qTs', g) for g in range(8)], [qres])
                    kTh, kres = kThs.next()
                    va = vaugs[slot]
                    S.op('pool', lambda e, kTh=kTh: e.tensor_copy(kTh[64:96, :], krT[0:32, :]),
                         [('krT', g) for g in range(8)], [(kres, 'r')])
                    for c in range(8):
                        cs_ = slice(c * 512, (c + 1) * 512)
                        pj, pres = self.pjs.next()
                        for k in range(2):
                            self.mm(pj[:], wukv[:, k, h * 128:(h + 1) * 128], ckvT[:, k, cs_], k == 0, k == 1,
                                    [('wukv', k, h // 8), ('ckvT', c)], [pres])
                        self.act(kTh[0:64, cs_], pj[0:64, :], AF.Copy, [pres], [(kres, c)])
                        vt, vtres = vTz[slot].next()
                        zres = (f'vTz{slot}z', vtres[1])
                        if slot == 0:
                            S.op('dve', lambda e, vt=vt, pj=pj: e.tensor_copy(vt[0:64, :], pj[64:128, :]),
                                 [pres], [vtres])
                        else:
                            S.op('dve', lambda e, vt=vt, pj=pj: e.tensor_copy(vt[64:128, :], pj[64:128, :]),
                                 [pres], [vtres])
                        tp, tres = self.tps.next()
                        for jj in range(4):
                            self.tr(tp[:, jj * 128:(jj + 1) * 128], vt[:, jj * 128:(jj + 1) * 128], [vtres, zres], [tres])
                        tv = tp[:, 0:512].rearrange("p (a b) -> p a b", a=4)
                        vs = slice(0, 64) if slot == 0 else slice(64, 128)
                        S.op('dve', lambda e, va=va, tv=tv, c=c, vs=vs: e.tensor_copy(va[:, 4 * c:4 * c + 4, vs],
                                                                                  tv[:, :, vs]),
                             [tres, ('vaugm1', slot)], [('vaugm', slot, c)])
                    for c in range(8):
                        oa, ores = self.oas.next()
                        nk = 4 * c + 4
                        for kt in range(nk):
                            sc, sres = self.scs.next()
                            ks = slice(kt * 128, (kt + 1) * 128)
                            if kt < 4 * c:
                                col0, n = 0, 512
                                self.mm(sc[:, 0:n], kTh[:, ks], qTh[:, c * 512:(c + 1) * 512], True, True,
                                        [(kres, kt // 4), (kres, 'r'), qres], [sres])
                            else:
                                col0 = 128 * (kt - 4 * c)
                                n = 512 - col0
                                self.mm(sc[:, 0:n], kTh[:, ks], qTh[:, c * 512 + col0:(c + 1) * 512], True, False,
                                        [(kres, kt // 4), (kres, 'r'), qres], [sres])
                                self.mm(sc[:, 0:n], self.ident[:], mtri[:, 0:n], False, True, ['ident', 'mtri'], [sres])
                            pt, ptres = self.pts.next()
                            self.act(pt[:, 0:n], sc[:, 0:n], AF.Exp, [sres], [ptres], scale=scale)
                            self.mm(oa[:, col0:512], va[:, kt, :], pt[:, 0:n], kt == 0, kt == nk - 1,
                                    [('vaugm', slot, kt // 4), ('vaugm1', slot), ptres], [ores], skip=True)
                        self.normalize_out(lambda rows, oa=oa: oa[rows, 0:512], ores, 512, slot, h // 2, c * 512)
        S.barrier()

    def build(self):
        nc = self.nc
        S = self.S
        with contextlib.ExitStack() as es:
            self.ident = self.sb(es, "ident", [128, 128], BF16)
            self.dma(self.ident[:], self.C['ident'], [], ['ident'])
            self.epsc = self.sb(es, "epsc", [128, 1], F32)
            S.op('dve', lambda e: e.memset(self.epsc[:], EPS), [], ['epsc'])
            self.junk = Rot('junk', [self.sb(es, f"junk{i}", [128, DM], BF16) for i in range(2)])
            ps = lambda name, shape, dt: es.enter_context(nc.psum_tensor(name, shape, dt))
            self.tps = Rot('tp', [ps(f"tp{i}", [128, 1024], BF16) for i in range(2)])
            self.pjs = Rot('pj', [ps(f"pj{i}", [128, 512], F32) for i in range(2)])
            self.scs = Rot('sc', [ps(f"sc{i}", [128, 512], F32) for i in range(2)])
            self.oas = Rot('oa', [ps(f"oa{i}", [128, 512], F32) for i in range(2)])
            hsrc = self.x
            for n, li in enumerate(self.layers):
                last = (n == len(self.layers) - 1)
                with contextlib.ExitStack() as es2:
                    xnT = self.sb(es2, "xnT", [128, 8, SEQ], BF16)
                    with contextlib.ExitStack() as es3:
                        self.phase_norm(es3, hsrc, f'l{li}_attn_norm', xnT)
                    S.barrier()
                    npair = 8
                    if li == 3:
                        self.phase_swa(li, xnT)
                    elif li == 1:
                        npair = 4
                        self.phase_dilated(li, xnT)
                    elif li == 0:
                        self.phase_moba(li, xnT)
                    elif li == 2:
                        self.phase_mla(li, xnT)
                    else:
                        raise NotImplementedError
                self.phase_oproj(li, npair, hsrc)
                self.phase_ffn(li, last)
                hsrc = self.hbuf
            S.barrier()
            S.emit()
        return nc


_CONSTS = None


def run(inputs, layers=(0, 1, 2, 3), final=True, cores=8, x_override=None):
    global _CONSTS
    if _CONSTS is None:
        _CONSTS = host_consts()
    b = Builder(layers, final)
    nc = b.build()
    x = np.ascontiguousarray(inputs['x'], dtype=np.float32) if x_override is None else x_override
    in_maps = []
    for c in range(cores):
        m = {'x': np.ascontiguousarray(x[c])}
        for name in b.used_inputs():
            m[name] = np.ascontiguousarray(inputs[name], dtype=np.float32)
        for name in CONST_SPECS:
            m['c_' + name] = _CONSTS[name]
        in_maps.append(m)
    res = run_bass_kernel_spmd(nc, in_maps, core_ids=list(range(cores)))
    return np.stack([np.asarray(r['y']) for r in res.results], axis=0)


def kernel(**inputs):
    out = run(inputs)
    return out.astype(np.float32)
```

```python
import contextlib
import numpy as np
import ml_dtypes
import concourse.bass as bass
import concourse.mybir as mybir
from concourse.bass_utils import run_bass_kernel_spmd

F32 = mybir.dt.float32
BF16 = mybir.dt.bfloat16
AF = mybir.ActivationFunctionType
ALU = mybir.AluOpType
AX = mybir.AxisListType
NPBF = ml_dtypes.bfloat16

SEQ = 4096
DM = 1024
NT = SEQ // 128
DFF = 4096
EPS = 1e-6
NEG = -30000.0

ENGS = ['pe', 'act', 'dve', 'pool', 'sp']
NDMASEM = 8
DBG = {}


class Op:
    __slots__ = ('fn', 'waits', 'key', 'idx', 'signal', 'isdma')

    def __init__(self, fn, key, idx, isdma):
        self.fn = fn
        self.waits = []
        self.key = key
        self.idx = idx
        self.signal = False
        self.isdma = isdma


class Sched:
    def __init__(self, nc):
        self.nc = nc
        self.streams = {e: [] for e in ENGS}
        self.keyops = {}
        self.seen = {e: {} for e in ENGS}
        self.res = {}
        self.dma_rr = {e: 0 for e in ENGS}

    def _need(self, eng, op, tok):
        key, idx = tok
        if self.seen[eng].get(key, -1) >= idx:
            return
        self.seen[eng][key] = idx
        op.waits.append(tok)

    def _deps(self, eng, op, reads, writes, mykey, same_ok):
        for r in reads:
            st = self.res.get(r)
            if st is None:
                continue
            w = st[0]
            if w is not None and not (same_ok and w[0] == mykey):
                self._need(eng, op, w)
        for r in writes:
            st = self.res.get(r)
            if st is None:
                continue
            w = st[0]
            if w is not None and w[0] != mykey:
                self._need(eng, op, w)
            for k, i in st[1].items():
                if k != mykey:
                    self._need(eng, op, (k, i))

    def _commit(self, tok, reads, writes):
        for r in reads:
            st = self.res.get(r)
            if st is None:
                st = self.res[r] = [None, {}]
            st[1][tok[0]] = tok[1]
        for r in writes:
            self.res[r] = [tok, {}]

    def op(self, eng, fn, reads=(), writes=()):
        key = eng
        lst = self.keyops.setdefault(key, [])
        o = Op(fn, key, len(lst), False)
        self._deps(eng, o, reads, writes, key, same_ok=(eng == 'pe'))
        lst.append(o)
        self.streams[eng].append(o)
        self._commit((key, o.idx), reads, writes)
        return o

    def dma(self, q, fn, reads=(), writes=()):
        j = self.dma_rr[q]
        self.dma_rr[q] = (j + 1) % NDMASEM
        key = ('dma', q, j)
        lst = self.keyops.setdefault(key, [])
        o = Op(fn, key, len(lst), True)
        if lst:
            self._need(q, o, (key, len(lst) - 1))
        self._deps(q, o, reads, writes, key, same_ok=False)
        lst.append(o)
        self.streams[q].append(o)
        self._commit((key, o.idx), reads, writes)
        return o

    def barrier(self):
        toks = [(key, len(lst) - 1) for key, lst in self.keyops.items() if lst]
        for e in ENGS:
            o = Op(None, None, None, False)
            for t in toks:
                self._need(e, o, t)
            if o.waits:
                self.streams[e].append(o)

    def emit(self):
        nc = self.nc
        for e in ENGS:
            for o in self.streams[e]:
                for (key, idx) in o.waits:
                    self.keyops[key][idx].signal = True
        semval = {}
        for key, lst in self.keyops.items():
            c = 0
            for o in lst:
                if o.isdma:
                    c += 16
                    semval[(key, o.idx)] = c
                    o.signal = True
                elif o.signal:
                    c += 1
                    semval[(key, o.idx)] = c
            assert c < 60000, (key, c)
        keys = [k for k, l in self.keyops.items() if l]
        with contextlib.ExitStack() as es:
            sems = {}
            for k in keys:
                nm = 's_' + ('_'.join(str(x) for x in k) if isinstance(k, tuple) else k)
                sems[k] = es.enter_context(nc.semaphore(nm))
            block = es.enter_context(nc.Block())

            def run(e):
                def body(eng):
                    for o in self.streams[e]:
                        for tok in o.waits:
                            eng.wait_ge(sems[tok[0]], semval[tok])
                        if o.fn is None:
                            continue
                        ins = o.fn(eng)
                        if o.signal:
                            ins.then_inc(sems[o.key], 16 if o.isdma else 1)
                return body
            if self.streams['pe']:
                block.tensor(run('pe'))
            if self.streams['act']:
                block.scalar(run('act'))
            if self.streams['dve']:
                block.vector(run('dve'))
            if self.streams['pool']:
                block.gpsimd(run('pool'))
            if self.streams['sp']:
                block.sync(run('sp'))


class Rot:
    def __init__(self, name, tiles):
        self.name = name
        self.tiles = tiles
        self.i = 0

    def next(self):
        j = self.i % len(self.tiles)
        self.i += 1
        return self.tiles[j], (self.name, j)


def alibi(n):
    return (2.0 ** (-8.0 * np.arange(1, n + 1, dtype=np.float64) / n))


def split_hi_lo(v):
    v = v.astype(np.float32)
    hi = v.astype(NPBF)
    lo = (v - hi.astype(np.float32)).astype(NPBF)
    return hi, lo


def band_bias(slope_eff, W, width=256):
    k = np.arange(128)[:, None].astype(np.float64)
    col = np.arange(width)[None, :].astype(np.float64)
    diff = col - k
    val = -slope_eff * diff * 8.0
    val = np.where((diff >= 0) & (diff < W), val, NEG)
    return val.astype(np.float32)


def host_consts():
    c = {}
    c['ident'] = np.eye(128, dtype=np.float32).astype(NPBF)
    sl = alibi(16)
    t = np.zeros((128, 16, 2, 256), NPBF)
    for h in range(16):
        hi, lo = split_hi_lo(band_bias(sl[h], 128))
        t[:, h, 0], t[:, h, 1] = hi, lo
    c['bsw'] = t
    sl24 = alibi(24)
    dils = (1, 4, 16)
    t = np.zeros((128, 24, 2, 256), NPBF)
    for g in range(3):
        for hs in range(8):
            hi, lo = split_hi_lo(band_bias(sl24[g * 8 + hs] * dils[g], 129))
            t[:, g * 8 + hs, 0], t[:, g * 8 + hs, 1] = hi, lo
    c['bdl'] = t
    t = np.zeros((128, 16, 2, 256), NPBF)
    for h in range(16):
        hi, lo = split_hi_lo(band_bias(sl[h], 10 ** 9))
        t[:, h, 0], t[:, h, 1] = hi, lo
    c['bmo'] = t
    p = np.arange(128)[:, None, None].astype(np.float64)
    j = np.arange(2)[None, :, None].astype(np.float64)
    c['tqm'] = (-sl[None, None, :] * 8.0 * (j * 128 + p) - 30000.0).astype(np.float32)
    idx = np.arange(31)[None, None, :].astype(np.float64)
    c['dtab'] = np.broadcast_to((-sl[None, :, None] * 2048.0 * (15 - idx)), (128, 16, 31)).astype(np.float32).copy()
    hi, lo = split_hi_lo(np.broadcast_to((sl * 8.0)[None, :], (128, 16)).copy())
    c['slp'] = np.stack([hi, lo], axis=-1)
    X = np.zeros((128, 16, 2, 128), np.float32)
    for n in range(16):
        X[n, n] = 1.0
        X[16 + n, n] = 1.0
    for half in range(2):
        X[32, :, half, :] = np.arange(128)[None, :] + 128 * half
        X[33, :, half, :] = np.arange(128)[None, :] + 128 * half
    c['xsel'] = X.astype(NPBF)
    k = np.arange(128)[:, None]
    col = np.arange(512)[None, :]
    c['mtri'] = np.where(col >= k, 0.0, NEG).astype(np.float32).astype(NPBF)
    inv = 10000.0 ** (-np.arange(0, 32, 2, dtype=np.float64) / 32)
    pos = (np.arange(32)[None, :] * 128 + np.arange(128)[:, None]).astype(np.float64)
    ang = pos[:, :, None] * inv[None, None, :]
    ang = ang.astype(np.float32).astype(np.float64)
    cos, sin = np.cos(ang), np.sin(ang)
    c['ropecs'] = np.concatenate([cos, cos], axis=-1).astype(np.float32)
    c['ropesn'] = np.concatenate([-sin, sin], axis=-1).astype(np.float32)
    return c


CONST_SPECS = {
    'ident': ([128, 128], BF16), 'bsw': ([128, 16, 2, 256], BF16), 'bdl': ([128, 24, 2, 256], BF16),
    'bmo': ([128, 16, 2, 256], BF16), 'tqm': ([128, 2, 16], F32), 'dtab': ([128, 16, 31], F32),
    'slp': ([128, 16, 2], BF16), 'xsel': ([128, 16, 2, 128], BF16), 'mtri': ([128, 512], BF16),
    'ropecs': ([128, 32, 32], F32), 'ropesn': ([128, 32, 32], F32),
}

WEIGHT_SPECS = [
    ('l0_attn_norm', [1024]), ('l0_w_qkv', [1024, 3072]), ('l0_w_o', [1024, 1024]), ('l0_mlp_norm', [1024]),
    ('l0_w_up', [1024, 4096]), ('l0_w_down', [4096, 1024]),
    ('l1_attn_norm', [1024]), ('l1_w_qkv', [1024, 4608]), ('l1_w_o', [512, 1024]), ('l1_mlp_norm', [1024]),
    ('l1_w_up', [1024, 4096]), ('l1_w_down', [4096, 1024]),
    ('l2_attn_norm', [1024]), ('l2_w_dkv', [1024, 1056]), ('l2_q_norm', [768]), ('l2_w_uq', [768, 1536]),
    ('l2_kv_norm', [256]), ('l2_w_ukv', [256, 2048]), ('l2_w_o', [1024, 1024]), ('l2_mlp_norm', [1024]),
    ('l2_w_up', [1024, 4096]), ('l2_w_down', [4096, 1024]),
    ('l3_attn_norm', [1024]), ('l3_w_qkv', [1024, 1280]), ('l3_sinks', [16]), ('l3_w_o', [1024, 1024]),
    ('l3_mlp_norm', [1024]), ('l3_w_up', [1024, 4096]), ('l3_w_down', [4096, 1024]),
    ('final_norm', [1024]),
]


class Builder:
    def __init__(self, layers=(0, 1, 2, 3), final=True):
        self.layers = tuple(layers)
        self.final = final
        nc = self.nc = bass.Bass("TRN2", target_bir_lowering=False)
        self.S = Sched(nc)
        self.x = nc.dram_tensor("x", [SEQ, DM], F32, kind="ExternalInput").ap()
        self.W = {}
        for name, shape in WEIGHT_SPECS:
            if name == 'final_norm' or int(name[1]) in self.layers:
                self.W[name] = nc.dram_tensor(name, shape, F32, kind="ExternalInput").ap()
        self.C = {}
        for name, (shape, dt) in CONST_SPECS.items():
            self.C[name] = nc.dram_tensor("c_" + name, shape, dt, kind="ExternalInput").ap()
        self.y = nc.dram_tensor("y", [SEQ, DM], F32, kind="ExternalOutput").ap()
        self.hbuf = nc.dram_tensor("hbuf", [SEQ, DM], F32, kind="Internal").ap()
        self.oTs = nc.dram_tensor("oTs", [8, 128, SEQ], BF16, kind="Internal").ap()
        self.qTs = nc.dram_tensor("qTs", [16, 96, SEQ], BF16, kind="Internal").ap()
        self.cast_rr = 0

    def used_inputs(self):
        return list(self.W.keys())

    def sb(self, es, name, shape, dt):
        self.uid = getattr(self, 'uid', 0) + 1
        return es.enter_context(self.nc.sbuf_tensor(f"{name}_{self.uid}", shape, dt))

    def mm(self, out, lhsT, rhs, start, stop, reads, writes, skip=False):
        self.S.op('pe', lambda e: e.matmul(out, lhsT=lhsT, rhs=rhs, start=start, stop=stop,
                                           skip_group_check=skip), reads, writes)

    def tr(self, out, in_, reads, writes):
        ident = self.ident
        self.S.op('pe', lambda e: e.transpose(out, in_, ident[:]), list(reads) + ['ident'], writes)

    def act(self, out, in_, func, reads, writes, scale=1.0, bias=None, accum=None):
        kw = {}
        if bias is not None:
            kw['bias'] = bias
        if accum is not None:
            kw['accum_out'] = accum
        self.S.op('act', lambda e: e.activation(out=out, in_=in_, func=func, scale=scale, **kw), reads, writes)

    def dma(self, out, in_, reads, writes, q='sp'):
        self.S.dma(q, lambda e: e.dma_start(out=out, in_=in_), reads, writes)

    def load_cast(self, dst, src, n, reads_src=(), wres=None, shape3=None):
        stg, sres = self.stg.next()
        sv = stg[:, 0:n]
        if shape3 is not None:
            sv = sv.rearrange("p (a b) -> p a b", a=shape3[0])
        self.dma(sv, src, list(reads_src), [sres])
        eng = ('dve', 'pool')[self.cast_rr % 2]
        self.cast_rr += 1
        self.S.op(eng, lambda e: e.tensor_copy(dst, sv), [sres], [wres])

    def rms_stats(self, src_ap, ss_ap, reads, ssres):
        junk, jres = self.junk.next()
        self.act(junk[:], src_ap, AF.Square, reads, [jres, ssres], accum=ss_ap)

    def rms_rstd(self, ss_ap, rstd_ap, ssres, rres, n_feat):
        self.act(rstd_ap, ss_ap, AF.Ln, [ssres, 'epsc'], [rres], scale=1.0 / n_feat, bias=self.epsc[:, 0:1])
        self.act(rstd_ap, rstd_ap, AF.Exp, [rres], [rres], scale=-0.5)

    def phase_norm(self, es, src_dram, gname, xnT):
        S = self.S
        nc = self.nc
        gbc = self.sb(es, "gbc", [128, DM], F32)
        self.dma(gbc[:], self.W[gname].partition_broadcast(128), [], ['gbc'])
        hts = Rot('ht', [self.sb(es, f"ht{i}", [128, DM], F32) for i in range(3)])
        xns = Rot('xn', [self.sb(es, f"xn{i}", [128, DM], BF16) for i in range(2)])
        ss = self.sb(es, "ssn", [128, NT], F32)
        rs = self.sb(es, "rsn", [128, NT], F32)
        for t in range(NT):
            ht, hres = hts.next()
            self.dma(ht[:], src_dram[t * 128:(t + 1) * 128, :], [('h', t // 4)], [hres])
            self.rms_stats(ht[:], ss[:, t:t + 1], [hres], ('ssn', t))
            self.rms_rstd(ss[:, t:t + 1], rs[:, t:t + 1], ('ssn', t), ('rsn', t), DM)
            xn, xres = xns.next()
            S.op('dve', lambda e, xn=xn, ht=ht, t=t: e.scalar_tensor_tensor(
                out=xn[:], in0=ht[:], scalar=rs[:, t:t + 1], in1=gbc[:], op0=ALU.mult, op1=ALU.mult),
                [hres, ('rsn', t), 'gbc'], [xres])
            tp, tres = self.tps.next()
            for k in range(8):
                self.tr(tp[:, k * 128:(k + 1) * 128], xn[:, k * 128:(k + 1) * 128], [xres], [tres])
            self.act(xnT[:, :, t * 128:(t + 1) * 128], tp[:].rearrange("p (k n) -> p k n", k=8), AF.Copy,
                     [tres], [('xnT', t // 4)])

    def phase_oproj(self, li, npair, hsrc):
        S = self.S
        W = self.W
        with contextlib.ExitStack() as es:
            wo = self.sb(es, "wo", [128, npair, DM], BF16)
            self.stg = Rot('stg', [self.sb(es, f"stgo{i}", [128, 1024], F32) for i in range(3)])
            wov = W[f'l{li}_w_o'].rearrange("(k p) n -> p k n", p=128)
            for k in range(npair):
                self.load_cast(wo[:, k, :], wov[:, k, :], DM, wres=('wo', k))
            oTg = Rot('oTg', [self.sb(es, f"oTg{i}", [128, npair, 512], BF16) for i in range(2)])
            hgs = Rot('hgo', [self.sb(es, f"hgo{i}", [128, 4, DM], F32) for i in range(2)])
            for g in range(8):
                og, ores = oTg.next()
                hg, hres = hgs.next()
                self.dma(og[:], self.oTs[0:npair, :, g * 512:(g + 1) * 512].rearrange("a p n -> p a n"),
                         [('oTs', g)], [ores])
                self.dma(hg[:], hsrc[g * 512:(g + 1) * 512, :].rearrange("(t p) n -> p t n", p=128),
                         [('h', g)], [hres])
                for t in range(4):
                    for hf in range(2):
                        pj, pres = self.pjs.next()
                        for k in range(npair):
                            self.mm(pj[:], og[:, k, t * 128:(t + 1) * 128], wo[:, k, hf * 512:(hf + 1) * 512],
                                    k == 0, k == npair - 1, [ores, ('wo', k)], [pres])
                        S.op('dve', lambda e, hg=hg, pj=pj, t=t, hf=hf: e.tensor_tensor(
                            hg[:, t, hf * 512:(hf + 1) * 512], pj[:], hg[:, t, hf * 512:(hf + 1) * 512], ALU.add),
                            [pres, hres], [hres])
                self.dma(self.hbuf[g * 512:(g + 1) * 512, :].rearrange("(t p) n -> p t n", p=128), hg[:],
                         [hres], [('h', g)])
        S.barrier()

    def phase_ffn(self, li, last):
        S = self.S
        W = self.W
        with contextlib.ExitStack() as es:
            wup = self.sb(es, "wup", [128, 8, DFF], BF16)
            wdn = self.sb(es, "wdn", [128, 32, DM], BF16)
            gbc = self.sb(es, "gbcf", [128, DM], F32)
            gfin = self.sb(es, "gfin", [128, DM], F32)
            self.stg = Rot('stg', [self.sb(es, f"stgf{i}", [128, 2048], F32) for i in range(2)])
            self.dma(gbc[:], W[f'l{li}_mlp_norm'].partition_broadcast(128), [], ['gbcf'])
            if last and self.final:
                self.dma(gfin[:], W['final_norm'].partition_broadcast(128), [], ['gfin'])
            wupv = W[f'l{li}_w_up'].rearrange("(k p) n -> p k n", p=128)
            for hf in range(2):
                for k in range(8):
                    self.load_cast(wup[:, k, hf * 2048:(hf + 1) * 2048], wupv[:, k, hf * 2048:(hf + 1) * 2048], 2048,
                                   wres=('wup', k, hf))
            wdnv = W[f'l{li}_w_down'].rearrange("(k p) n -> p k n", p=128)
            for k2 in range(16):
                self.load_cast(wdn[:, 2 * k2:2 * k2 + 2, :], wdnv[:, 2 * k2:2 * k2 + 2, :], 2048,
                               wres=('wdn', k2), shape3=(2, DM))
            hgs = Rot('hg', [self.sb(es, f"hg{i}", [128, 2, DM], F32) for i in range(2)])
            xns = Rot('xnf', [self.sb(es, f"xnf{i}", [128, DM], BF16) for i in range(2)])
            xT = Rot('xT', [self.sb(es, f"xT{i}", [128, 8, 256], BF16) for i in range(1)])
            uTs = Rot('uT', [self.sb(es, f"uT{i}", [128, 8, 256], BF16) for i in range(2)])
            rl = Rot('rl', [self.sb(es, f"rl{i}", [128, 256], F32) for i in range(3)])
            ss = self.sb(es, "ssf", [128, NT], F32)
            rs = self.sb(es, "rsf", [128, NT], F32)
            ssl = self.sb(es, "ssl", [128, NT], F32)
            rsl = self.sb(es, "rsl", [128, NT], F32)
            for g in range(16):
                hg, hres = hgs.next()
                self.dma(hg[:], self.hbuf[g * 256:(g + 1) * 256, :].rearrange("(t p) n -> p t n", p=128),
                         [('h', g // 2)], [hres])
                x2, x2res = xT.next()
                for t in range(2):
                    i = g * 2 + t
                    self.rms_stats(hg[:, t, :], ss[:, i:i + 1], [hres], ('ssf', i))
                    self.rms_rstd(ss[:, i:i + 1], rs[:, i:i + 1], ('ssf', i), ('rsf', i), DM)
                    xn, xres = xns.next()
                    S.op('dve', lambda e, xn=xn, hg=hg, t=t, i=i: e.scalar_tensor_tensor(
                        out=xn[:], in0=hg[:, t, :], scalar=rs[:, i:i + 1], in1=gbc[:], op0=ALU.mult, op1=ALU.mult),
                        [hres, ('rsf', i), 'gbcf'], [xres])
                    tp, tres = self.tps.next()
                    for k in range(8):
                        self.tr(tp[:, k * 128:(k + 1) * 128], xn[:, k * 128:(k + 1) * 128], [xres], [tres])
                    self.act(x2[:, :, t * 128:(t + 1) * 128], tp[:].rearrange("p (k n) -> p k n", k=8), AF.Copy,
                             [tres], [x2res])
                for q in range(4):
                    uT, ures = uTs.next()
                    for cc in range(8):
                        c = q * 8 + cc
                        sc, sres = self.scs.next()
                        for k in range(8):
                            self.mm(sc[:, 0:256], wup[:, k, c * 128:(c + 1) * 128], x2[:, k, :], k == 0, k == 7,
                                    [x2res, ('wup', k, c // 16)], [sres])
                        r, rres = rl.next()
                        self.act(r[:], sc[:, 0:256], AF.Relu, [sres], [rres])
                        S.op('dve', lambda e, r=r, cc=cc, uT=uT: e.tensor_tensor(uT[:, cc, :], r[:], r[:], ALU.mult),
                             [rres], [(ures, cc)])
                    for t in range(2):
                        for hf in range(2):
                            pj, pres = self.pjs.next()
                            for cc in range(8):
                                c = q * 8 + cc
                                self.mm(pj[:], uT[:, cc, t * 128:(t + 1) * 128], wdn[:, c, hf * 512:(hf + 1) * 512],
                                        cc == 0, cc == 7, [(ures, cc), ('wdn', c // 2)], [pres])
                            S.op('dve', lambda e, hg=hg, pj=pj, t=t, hf=hf: e.tensor_tensor(
                                hg[:, t, hf * 512:(hf + 1) * 512], pj[:], hg[:, t, hf * 512:(hf + 1) * 512], ALU.add),
                                [pres, hres], [hres])
                rows = slice(g * 256, (g + 1) * 256)
                if last and self.final:
                    for t in range(2):
                        i = g * 2 + t
                        self.rms_stats(hg[:, t, :], ssl[:, i:i + 1], [hres], ('ssl', i))
                        self.rms_rstd(ssl[:, i:i + 1], rsl[:, i:i + 1], ('ssl', i), ('rsl', i), DM)
                        S.op('dve', lambda e, hg=hg, i=i, t=t: e.scalar_tensor_tensor(
                            out=hg[:, t, :], in0=hg[:, t, :], scalar=rsl[:, i:i + 1], in1=gfin[:],
                            op0=ALU.mult, op1=ALU.mult), [hres, ('rsl', i), 'gfin'], [hres])
                dst = self.y if last else self.hbuf
                self.dma(dst[rows, :].rearrange("(t p) n -> p t n", p=128), hg[:], [hres],
                         ['y'] if last else [('h', g // 2)])
        S.barrier()

    def normalize_out(self, src, sres, ncols, slot, pair, col0, extra=None):
        S = self.S
        orow = slice(0, 64) if slot == 0 else slice(64, 128)
        drow = slice(64, 128) if slot == 0 else slice(0, 64)
        rc, rres = self.rcs.next()
        S.op('dve', lambda e: e.tensor_copy(rc[orow, 0:ncols], src(drow)), [sres], [rres])
        if extra is not None:
            sc_ap, sc_res = extra
            S.op('dve', lambda e: e.tensor_scalar(rc[orow, 0:ncols], rc[orow, 0:ncols], sc_ap(orow), None, ALU.add),
                 [rres, sc_res], [rres])
        S.op('dve', lambda e: e.reciprocal(rc[orow, 0:ncols], rc[orow, 0:ncols]), [rres], [rres])
        on, onres = self.ons.next()
        S.op('dve', lambda e: e.tensor_tensor(on[orow, 0:ncols], src(orow), rc[orow, 0:ncols], ALU.mult),
             [sres, rres], [onres])
        self.dma(self.oTs[pair, orow, col0:col0 + ncols], on[orow, 0:ncols], [onres], [('oTs', col0 // 512)])

    def alloc_attn_common(self, es, nslots=4):
        self.pts = Rot('pt', [self.sb(es, f"pt{i}", [128, 512], BF16) for i in range(6)])
        self.rcs = Rot('rc', [self.sb(es, f"rc{i}", [128, 512], F32) for i in range(2)])
        self.ons = Rot('on', [self.sb(es, f"on{i}", [128, 512], BF16) for i in range(2)])
        self.stg = Rot('stg', [self.sb(es, f"stga{i}", [128, 1024], F32) for i in range(3)])
        self.pending = []
        t0, t1 = self.scs.tiles
        p0, p1 = self.pjs.tiles
        if nslots == 4:
            self.scslots = [(t0, 0, ('sc', 0)), (t1, 0, ('sc', 1)), (p0, 0, ('pj', 0)), (p1, 0, ('pj', 1))]
        else:
            self.scslots = [(t0, 0, ('sc', 0)), (t1, 0, ('sc', 1)), (p0, 0, ('pj', 0))]
        self.skew = 2
        self.sci = 0

    def attn_step(self, n, qk_list, scale, pv, after=None):
        tile, off, sres = self.scslots[self.sci % len(self.scslots)]
        self.sci += 1
        sc = tile[:, off:off + n]
        for i, (lhsT, rhs, reads) in enumerate(qk_list):
            self.mm(sc, lhsT, rhs, i == 0, i == len(qk_list) - 1, reads, [sres])
        pt, ptres = self.pts.next()
        self.act(pt[:, 0:n], sc, AF.Exp, [sres], [ptres], scale=scale)
        self.pending.append((pv, pt, ptres, n, after))
        while len(self.pending) > self.skew:
            self._attn_pop()

    def _attn_pop(self):
        pv, pt, ptres, n, after = self.pending.pop(0)
        out_ap, lhsT, reads, ores, first, last = pv
        self.mm(out_ap, lhsT, pt[:, 0:n], first, last, list(reads) + [ptres], [ores], skip=True)
        if after is not None:
            after()

    def attn_flush(self):
        while self.pending:
            self._attn_pop()

    def phase_swa(self, li, xnT):
        S = self.S
        W = self.W
        with contextlib.ExitStack() as es:
            self.alloc_attn_common(es)
            bsw = self.sb(es, "bsw", [128, 16, 2, 256], BF16)
            self.dma(bsw[:], self.C['bsw'], [], ['bsw'])
            esink = self.sb(es, "esink", [128, 16], F32)
            self.dma(esink[:], W[f'l{li}_sinks'].partition_broadcast(128), [], ['esink'])
            self.act(esink[:], esink[:], AF.Exp, ['esink'], ['esink'])
            wkv = self.sb(es, "wkv", [128, 8, 256], BF16)
            wqs = Rot('wq', [self.sb(es, f"wq{i}", [128, 8, 128], BF16) for i in range(2)])
            kTv = self.sb(es, "kTv", [128, 2, 2, SEQ], BF16)
            vaug = self.sb(es, "vaug", [128, NT, 2, 2, 128], BF16)
            qTs = Rot('qT', [self.sb(es, f"qT{i}", [128, SEQ], BF16) for i in range(2)])
            wv = W[f'l{li}_w_qkv'].rearrange("(k p) n -> p k n", p=128)
            self.load_cast(wkv[:, :, 0:128], wv[:, :, 1024:1152], 1024, wres='wkv', shape3=(8, 128))
            self.load_cast(wkv[:, :, 128:256], wv[:, :, 1152:1280], 1024, wres='wkv2', shape3=(8, 128))
            S.op('pool', lambda e: e.memset(vaug[:, :, :, 0, 64:128], 1.0), [], ['vaug1'])
            S.op('pool', lambda e: e.memset(vaug[:, :, :, 1, 0:64], 1.0), [], ['vaug1'])
            for kvh in range(2):
                S.op('pool', lambda e, kvh=kvh: e.memset(kTv[64:128, kvh, 0, :], 0.0), [], ['kTz'])
                S.op('pool', lambda e, kvh=kvh: e.memset(kTv[0:64, kvh, 1, :], 0.0), [], ['kTz'])
            for c in range(8):
                pj, pres = self.pjs.next()
                cs = slice(c * 512, (c + 1) * 512)
                for k in range(8):
                    self.mm(pj[:], wkv[:, k, 0:128], xnT[:, k, cs], k == 0, k == 7, ['wkv', ('xnT', c)], [pres])
                self.act(kTv[0:64, 0, 0, cs], pj[0:64, :], AF.Copy, [pres, 'kTz'], [('kT', c)])
                self.act(kTv[64:128, 0, 1, cs], pj[0:64, :], AF.Copy, [pres, 'kTz'], [('kT', c)])
                S.op('dve', lambda e, pj=pj, cs=cs: e.tensor_copy(kTv[64:128, 1, 1, cs], pj[64:128, :]),
                     [pres, 'kTz'], [('kT', c)])
                S.op('dve', lambda e, pj=pj, cs=cs: e.tensor_copy(kTv[0:64, 1, 0, cs], pj[64:128, :]),
                     [pres, 'kTz'], [('kT', c)])
            for t in range(NT):
                pj, pres = self.pjs.next()
                for k in range(8):
                    self.mm(pj[:, 0:128], xnT[:, k, t * 128:(t + 1) * 128], wkv[:, k, 128:256], k == 0, k == 7,
                            ['wkv2', ('xnT', t // 4)], [pres])
                for kvh in range(2):
                    S.op('dve', lambda e, pj=pj, t=t, kvh=kvh: e.tensor_copy(
                        vaug[:, t, kvh, 0, 0:64], pj[:, kvh * 64:(kvh + 1) * 64]), [pres, 'vaug1'], [('vaug', t)])
                    S.op('dve', lambda e, pj=pj, t=t, kvh=kvh: e.tensor_copy(
                        vaug[:, t, kvh, 1, 64:128], pj[:, kvh * 64:(kvh + 1) * 64]), [pres, 'vaug1'], [('vaug', t)])
            for p in range(8):
                wq, wqres = wqs.next()
                self.load_cast(wq[:], wv[:, :, p * 128:(p + 1) * 128], 1024, wres=wqres, shape3=(8, 128))
                qT, qres = qTs.next()
                for c in range(8):
                    pj, pres = self.pjs.next()
                    for k in range(8):
                        self.mm(pj[:], wq[:, k, :], xnT[:, k, c * 512:(c + 1) * 512], k == 0, k == 7,
                                [wqres, ('xnT', c)], [pres])
                    self.act(qT[:, c * 512:(c + 1) * 512], pj[:], AF.Copy, [pres], [(qres, c)])
                for slot in range(2):
                    h = 2 * p + slot
                    kvh = h // 8
                    for c in range(8):
                        oa, ores = self.oas.next()
                        kts = [kt for kt in range(4 * c - 1, 4 * c + 4) if kt >= 0]
                        for si, kt in enumerate(kts):
                            qts = [qt for qt in (kt, kt + 1) if 4 * c <= qt <= 4 * c + 3]
                            col0 = (qts[0] - 4 * c) * 128
                            n = 128 * len(qts)
                            boff = 0 if qts[0] == kt else 128
                            qk = [(kTv[:, kvh, slot, kt * 128:(kt + 1) * 128], qT[:, c * 512 + col0:c * 512 + col0 + n],
                                   [('kT', kt // 4), 'kTz', (qres, c)]),
                                  (self.ident[:], bsw[:, h, 0, boff:boff + n], ['ident', 'bsw']),
                                  (self.ident[:], bsw[:, h, 1, boff:boff + n], ['ident', 'bsw'])]
                            last = si == len(kts) - 1
                            after = None
                            if last:
                                after = (lambda oa=oa, ores=ores, slot=slot, p=p, c=c, h=h: self.normalize_out(
                                    lambda rows: oa[rows, 0:512], ores, 512, slot, p, c * 512,
                                    extra=(lambda rows: esink[rows, h:h + 1], 'esink')))
                            self.attn_step(n, qk, 0.125, (oa[:, col0:col0 + n], vaug[:, kt, kvh, slot, :],
                                                          [('vaug', kt), 'vaug1'], ores, si == 0, last), after)
                self.attn_flush()
        S.barrier()

    def alloc_pair(self, es):
        S = self.S
        self.wqkv = Rot('wqkv', [self.sb(es, f"wqkv{i}", [128, 8, 384], BF16) for i in range(2)])
        self.qT = self.sb(es, "qTp", [128, SEQ], BF16)
        self.kTz = self.sb(es, "kTz", [128, 2, SEQ], BF16)
        self.vaug = self.sb(es, "vaugp", [128, NT, 2, 128], BF16)
        self.vTs = Rot('vT', [self.sb(es, f"vT{i}", [128, 512], BF16) for i in range(2)])
        S.op('pool', lambda e: e.memset(self.kTz[64:128, 0, :], 0.0), [], ['kTzz'])
        S.op('pool', lambda e: e.memset(self.kTz[0:64, 1, :], 0.0), [], ['kTzz'])
        S.op('pool', lambda e: e.memset(self.vaug[:, :, 0, 64:128], 1.0), [], ['vaug1'])
        S.op('pool', lambda e: e.memset(self.vaug[:, :, 1, 0:64], 1.0), [], ['vaug1'])

    def pair_proj(self, wv, qc, kc, vc, xnT, colmap=None):
        S = self.S
        w, wres = self.wqkv.next()
        for j, c0 in enumerate((qc, kc, vc)):
            self.load_cast(w[:, :, j * 128:(j + 1) * 128], wv[:, :, c0:c0 + 128], 1024, wres=(wres, j), shape3=(8, 128))
        qT, kTz, vaug = self.qT, self.kTz, self.vaug
        for c in range(8):
            cs = slice(c * 512, (c + 1) * 512)
            xres = ('xnT', c) if colmap is None else 'xnTall'
            outs = []
            for j in range(3):
                pj, pres = self.pjs.next()
                for k in range(8):
                    rhs = xnT[:, k, cs] if colmap is None else colmap(k, c)
                    o = pj[:] if len(rhs.shape) == 2 else pj[:].rearrange("p (a b) -> p a b", a=rhs.shape[1])
                    self.mm(o, w[:, k, j * 128:(j + 1) * 128], rhs, k == 0, k == 7,
                            [(wres, j)] + ([xres] if colmap is None else [('xnT', cc) for cc in range(8)]), [pres])
                if j == 0:
                    self.act(qT[:, cs], pj[:], AF.Copy, [pres], [('qT', c)])
                elif j == 1:
                    self.act(kTz[0:64, 0, cs], pj[0:64, :], AF.Copy, [pres, 'kTzz'], [('kT', c)])
                    S.op('dve', lambda e, pj=pj, cs=cs: e.tensor_copy(kTz[64:128, 1, cs], pj[64:128, :]),
                         [pres, 'kTzz'], [('kT', c)])
                else:
                    vT, vres = self.vTs.next()
                    self.act(vT[:], pj[:], AF.Copy, [pres], [vres])
                    tp, tres = self.tps.next()
                    for jj in range(4):
                        self.tr(tp[:, jj * 128:(jj + 1) * 128], vT[:, jj * 128:(jj + 1) * 128], [vres], [tres])
                    tv = tp[:, 0:512].rearrange("p (a b) -> p a b", a=4)
                    S.op('dve', lambda e, tv=tv, c=c: e.tensor_copy(vaug[:, 4 * c:4 * c + 4, 0, 0:64], tv[:, :, 0:64]),
                         [tres, 'vaug1'], [('vaug', c)])
                    S.op('dve', lambda e, tv=tv, c=c: e.tensor_copy(vaug[:, 4 * c:4 * c + 4, 1, 64:128],
                                                                  tv[:, :, 64:128]), [tres, 'vaug1'], [('vaug', c)])

    def phase_dilated(self, li, xnT):
        S = self.S
        W = self.W
        dils = (1, 4, 16)
        with contextlib.ExitStack() as es:
            self.alloc_attn_common(es)
            self.alloc_pair(es)
            bdl = self.sb(es, "bdl", [128, 24, 2, 256], BF16)
            self.dma(bdl[:], self.C['bdl'], [], ['bdl'])
            acc = [self.sb(es, f"acc{i}", [128, SEQ], F32) for i in range(2)]
            wv = W[f'l{li}_w_qkv'].rearrange("(k p) n -> p k n", p=128)
            qT, kTz, vaug = self.qT, self.kTz, self.vaug
            for pp in range(4):
                for g in range(3):
                    d = dils[g]
                    Sd = SEQ // d
                    nseg = Sd // 128

                    def colmap(k, c, d=d, Sd=Sd):
                        if d == 1:
                            return xnT[:, k, c * 512:(c + 1) * 512]
                        if Sd >= 512:
                            r, i0 = divmod(c * 512, Sd)
                            st = r + d * i0
                            return xnT[:, k, st:st + d * 511 + 1:d]
                        v = xnT[:, k, :].rearrange("p (j d) -> p d j", d=d)
                        nr = 512 // Sd
                        return v[:, c * nr:(c + 1) * nr, :]

                    base = (g * 3) * 512
                    self.pair_proj(wv, base + pp * 128, base + 512 + pp * 128, base + 1024 + pp * 128, xnT,
                                   colmap=(None if d == 1 else colmap))
                    for slot in range(2):
                        hs = 2 * pp + slot
                        hb = g * 8 + hs
                        for c in range(8):
                            oa, ores = self.oas.next()
                            steps = []
                            for kt in range(4 * c - 1, 4 * c + 4):
                                if kt < 0:
                                    continue
                                qts = [kt] + ([kt + 1] if (kt + 1) % nseg != 0 else [])
                                qts = [qt for qt in qts if 4 * c <= qt <= 4 * c + 3]
                                if qts:
                                    steps.append((kt, qts))
                            A = acc[slot]
                            if d == 1:
                                def after(oa=oa, ores=ores, A=A, c=c, slot=slot):
                                    self.act(A[:, c * 512:(c + 1) * 512], oa[:], AF.Copy, [ores, ('accall', slot)],
                                             [('acc', slot, c)])
                            else:
                                if Sd >= 512:
                                    r, i0 = divmod(c * 512, Sd)
                                    st = r + d * i0
                                    pieces = [(A[:, st:st + d * 511 + 1:d], oa[:, 0:512])]
                                else:
                                    nr = 512 // Sd
                                    pieces = []
                                    for rr in range(nr):
                                        r = c * nr + rr
                                        pieces.append((A[:, r:r + d * (Sd - 1) + 1:d], oa[:, rr * Sd:(rr + 1) * Sd]))

                                def after(pieces=pieces, ores=ores, slot=slot):
                                    for (av, ov) in pieces:
                                        S.op('dve', lambda e, av=av, ov=ov: e.tensor_tensor(av, ov, av, ALU.add),
                                             [ores] + [('acc', slot, cc) for cc in range(8)], [('accall', slot)])
                            for si, (kt, qts) in enumerate(steps):
                                col0 = (qts[0] - 4 * c) * 128
                                n = 128 * len(qts)
                                boff = 0 if qts[0] == kt else 128
                                qk = [(kTz[:, slot, kt * 128:(kt + 1) * 128], qT[:, c * 512 + col0:c * 512 + col0 + n],
                                       [('kT', kt // 4), 'kTzz', ('qT', c)]),
                                      (self.ident[:], bdl[:, hb, 0, boff:boff + n], ['ident', 'bdl']),
                                      (self.ident[:], bdl[:, hb, 1, boff:boff + n], ['ident', 'bdl'])]
                                last = si == len(steps) - 1
                                self.attn_step(n, qk, 0.125, (oa[:, col0:col0 + n], vaug[:, kt, slot, :],
                                                              [('vaug', kt // 4), 'vaug1'], ores, si == 0, last),
                                               after if last else None)
                    self.attn_flush()
                for slot in range(2):
                    A = acc[slot]
                    for c in range(8):
                        self.normalize_out(lambda rows, A=A, c=c: A[rows, c * 512:(c + 1) * 512], ('accall', slot),
                                           512, slot, pp, c * 512)
        S.barrier()

    def phase_moba(self, li, xnT):
        S = self.S
        W = self.W
        C = self.C
        with contextlib.ExitStack() as es:
            self.alloc_attn_common(es, nslots=3)
            self.alloc_pair(es)
            bmo = self.sb(es, "bmo", [128, 16, 2, 256], BF16)
            self.dma(bmo[:], C['bmo'], [], ['bmo'])
            xsel = self.sb(es, "xsel", [128, 16, 2, 128], BF16)
            self.dma(xsel[:], C['xsel'], [], ['xsel'])
            tqm = self.sb(es, "tqm", [128, 2, 16], F32)
            self.dma(tqm[:], C['tqm'], [], ['tqm'])
            dtab = self.sb(es, "dtab", [128, 16, 31], F32)
            self.dma(dtab[:], C['dtab'], [], ['dtab'])
            slp = self.sb(es, "slp", [128, 16, 2], BF16)
            self.dma(slp[:], C['slp'], [], ['slp'])
            c30 = self.sb(es, "c30", [128, 16], F32)
            S.op('dve', lambda e: e.memset(c30[:], 30000.0), [], ['c30'])
            kmf = self.sb(es, "kmf", [128, 16], F32)
            kmT = self.sb(es, "kmT", [128, 2, 16], BF16)
            gss = Rot('gs', [self.sb(es, f"gs{i}", [128, 16], F32) for i in range(4)])
            t8s = Rot('t8', [self.sb(es, f"t8{i}", [128, 8], F32) for i in range(4)])
            s3s = Rot('s3', [self.sb(es, f"s3{i}", [128, 16], F32) for i in range(4)])
            vvs = Rot('vv', [self.sb(es, f"vv{i}", [128, 16], F32) for i in range(4)])
            yps = Rot('yp', [self.sb(es, f"yp{i}", [128, 128], BF16) for i in range(6)])
            for i in range(6):
                S.op('pool', lambda e, i=i: e.memset(yps.tiles[i][:], 0.0), [], [('yp', i)])
            Ys = Rot('Y', [self.sb(es, f"Y{i}", [128, 256], BF16) for i in range(3)])
            gp, gpres = self.pjs.tiles[1], ('pj', 1)
            wv = W[f'l{li}_w_qkv'].rearrange("(k p) n -> p k n", p=128)
            qT, kTz, vaug = self.qT, self.kTz, self.vaug
            allk = [('kT', c) for c in range(8)]
            for p in range(8):
                self.pair_proj(wv, p * 128, 1024 + p * 128, 2048 + p * 128, xnT)
                for slot in range(2):
                    h = 2 * p + slot
                    S.op('dve', lambda e, slot=slot: e.tensor_reduce(
                        out=kmf[:], in_=kTz[:, slot, :].rearrange("p (n k) -> p n k", k=256), axis=AX.X, op=ALU.add),
                        allk + ['kTzz'], ['kmf'])
                    S.op('dve', lambda e, slot=slot: e.tensor_scalar(kmT[:, slot, :], kmf[:], 1.0 / 256, None, ALU.mult),
                         ['kmf'], [('kmT', slot)])
                    ypd = {}
                    Yd = {}

                    def prepA(b, slot=slot, h=h, ypd=ypd):
                        for j in range(2):
                            tcols = slice(b * 256 + j * 128, b * 256 + (j + 1) * 128)
                            if b > 3:
                                self.mm(gp[:, 0:16], qT[:, tcols], kmT[:, slot, :], True, True,
                                        [('qT', b // 2), ('kmT', slot)], [gpres])
                                gs, gres = gss.next()
                                S.op('dve', lambda e, gs=gs: e.tensor_copy(gs[:], gp[:, 0:16]), [gpres], [gres])
                                S.op('dve', lambda e, gs=gs, b=b: e.memset(gs[:, b:16], -1e30), [], [gres])
                                t8, t8res = t8s.next()
                                S.op('dve', lambda e, gs=gs, t8=t8: e.max(t8[:], gs[:]), [gres], [t8res])
                                s3, s3res = s3s.next()
                                S.op('dve', lambda e, gs=gs, t8=t8, s3=s3: e.tensor_scalar(
                                    s3[:], gs[:], t8[:, 2:3], 30000.0, ALU.is_ge, ALU.mult), [gres, t8res], [s3res])
                            else:
                                s3, s3res = c30, 'c30'
                            vv, vres = vvs.next()
                            S.op('dve', lambda e, s3=s3, vv=vv, j=j: e.scalar_tensor_tensor(
                                out=vv[:], in0=s3[:], scalar=tqm[:, j, h:h + 1], in1=dtab[:, h, 15 - b:31 - b],
                                op0=ALU.add, op1=ALU.add), [s3res, 'tqm', 'dtab'], [vres])
                            yp, ypres = yps.next()
                            S.op('dve', lambda e, yp=yp, vv=vv: e.tensor_copy(yp[:, 0:16], vv[:]), [vres], [ypres])
                            S.op('dve', lambda e, yp=yp, vv=vv: e.tensor_tensor(yp[:, 16:32], vv[:], yp[:, 0:16],
                                                                              ALU.subtract), [vres, ypres], [ypres])
                            S.op('dve', lambda e, yp=yp: e.tensor_copy(yp[:, 32:34], slp[:, h, :]), ['slp'], [ypres])
                            ypd[(b, j)] = (yp, ypres)

                    def prepB(b, ypd=ypd, Yd=Yd):
                        Y, Yres = Ys.next()
                        for j in range(2):
                            yp, ypres = ypd[(b, j)]
                            tp, tres = self.tps.next()
                            self.tr(tp[:, 0:128], yp[:], [ypres], [tres])
                            S.op('dve', lambda e, Y=Y, tp=tp, j=j: e.tensor_copy(Y[:, j * 128:(j + 1) * 128],
                                                                             tp[:, 0:128]), [tres], [Yres])
                        Yd[b] = (Y, Yres)

                    def steps(b, slot=slot, h=h, p=p, Yd=Yd):
                        oa, ores = self.oas.next()
                        qcols = slice(b * 256, (b + 1) * 256)
                        first = True
                        for n in range(b):
                            Y, Yres = Yd[b]
                            for half in range(2):
                                kt = 2 * n + half
                                qk = [(kTz[:, slot, kt * 128:(kt + 1) * 128], qT[:, qcols],
                                       [('kT', kt // 4), 'kTzz', ('qT', b // 2)]),
                                      (xsel[:, n, half, :], Y[:, :], ['xsel', Yres])]
                                self.attn_step(256, qk, 0.125, (oa[:, 0:256], vaug[:, kt, slot, :],
                                                                [('vaug', kt // 4), 'vaug1'], ores, first, False))
                                first = False
                        for half in range(2):
                            kt = 2 * b + half
                            n = 256 - 128 * half
                            qc = slice(b * 256 + 128 * half, (b + 1) * 256)
                            qk = [(kTz[:, slot, kt * 128:(kt + 1) * 128], qT[:, qc],
                                   [('kT', kt // 4), 'kTzz', ('qT', b // 2)]),
                                  (self.ident[:], bmo[:, h, 0, 0:n], ['ident', 'bmo']),
                                  (self.ident[:], bmo[:, h, 1, 0:n], ['ident', 'bmo'])]
                            after = None
                            if half == 1:
                                after = (lambda oa=oa, ores=ores: self.normalize_out(
                                    lambda rows: oa[rows, 0:256], ores, 256, slot, p, b * 256))
                            self.attn_step(n, qk, 0.125, (oa[:, 128 * half:256], vaug[:, kt, slot, :],
                                                          [('vaug', kt // 4), 'vaug1'], ores, first, half == 1), after)
                            first = False

                    prepA(1)
                    prepA(2)
                    prepB(1)
                    for b in range(16):
                        if b >= 1 and b + 1 <= 15:
                            prepB(b + 1)
                        if b >= 1 and b + 2 <= 15:
                            prepA(b + 2)
                        steps(b)
                    self.attn_flush()
        S.barrier()

    def phase_mla(self, li, xnT):
        S = self.S
        W = self.W
        C = self.C
        with contextlib.ExitStack() as es:
            ckvT = self.sb(es, "ckvT", [128, 2, SEQ], BF16)
            krT = self.sb(es, "krT", [32, SEQ], BF16)
            wukv = self.sb(es, "wukv", [128, 2, 2048], BF16)
            mtri = self.sb(es, "mtri", [128, 512], BF16)
            self.dma(mtri[:], C['mtri'], [], ['mtri'])
            A, Ares = self.pjs.tiles[0], ('pj', 0)
            B, Bres = self.pjs.tiles[1], ('pj', 1)
            Cb, Cres = self.scs.tiles[0], ('sc', 0)
            Q = [(self.oas.tiles[0], ('oa', 0)), (self.oas.tiles[1], ('oa', 1)), (self.scs.tiles[1], ('sc', 1))]
            tpA, tAres = self.tps.tiles[0], ('tp', 0)
            tpB, tBres = self.tps.tiles[1], ('tp', 1)
            with contextlib.ExitStack() as es1:
                self.stg = Rot('stg', [self.sb(es1, f"stgm{i}", [128, 1024], F32) for i in range(2)])
                wdkv = self.sb(es1, "wdkv", [128, 8, 1056], BF16)
                wuq = self.sb(es1, "wuq", [128, 6, 1536], BF16)
                wd = W[f'l{li}_w_dkv'].rearrange("(k p) n -> p k n", p=128)
                for k in range(8):
                    self.load_cast(wdkv[:, k, 0:1024], wd[:, k, 0:1024], 1024, wres=('wdkv', k))
                if not DBG.get('skip_wdkvr'):
                    self.load_cast(wdkv[:, :, 1024:1056], wd[:, :, 1024:1056], 256, wres='wdkvr', shape3=(8, 32))
                wq = W[f'l{li}_w_uq'].rearrange("(k p) n -> p k n", p=128)
                for k in range(6):
                    self.load_cast(wuq[:, k, 0:1024], wq[:, k, 0:1024], 1024, wres=('wuq', k))
                    self.load_cast(wuq[:, k, 1024:1536], wq[:, k, 1024:1536], 512, wres=('wuq2', k))
                wk = W[f'l{li}_w_ukv'].rearrange("(k p) n -> p k n", p=128)
                for k in range(2):
                    for hf in range(2):
                        self.load_cast(wukv[:, k, hf * 1024:(hf + 1) * 1024], wk[:, k, hf * 1024:(hf + 1) * 1024], 1024,
                                       wres=('wukv', k, hf))
                qg = self.sb(es1, "qg", [128, 768], F32)
                kvg = self.sb(es1, "kvg", [128, 256], F32)
                if not DBG.get('skip_g'):
                    self.dma(qg[:], W[f'l{li}_q_norm'].partition_broadcast(128), [], ['qg'])
                    self.dma(kvg[:], W[f'l{li}_kv_norm'].partition_broadcast(128), [], ['kvg'])
                cs = self.sb(es1, "ropecs", [128, 32, 32], F32)
                sn = self.sb(es1, "ropesn", [128, 32, 32], F32)
                if not DBG.get('skip_rope'):
                    self.dma(cs[:], C['ropecs'], [], ['ropecs'])
                    self.dma(sn[:], C['ropesn'], [], ['ropesn'])
                cqns = Rot('cqn', [self.sb(es1, f"cqn{i}", [128, 768], BF16) for i in range(2)])
                ckvns = Rot('ckvn', [self.sb(es1, f"ckvn{i}", [128, 256], BF16) for i in range(2)])
                krrs = Rot('krr', [self.sb(es1, f"krr{i}", [128, 128], BF16) for i in range(2)])
                for i in range(2):
                    S.op('pool', lambda e, i=i: e.memset(krrs.tiles[i][:], 0.0), [], [('krr', i)])
                ra = self.sb(es1, "ra", [128, 32], F32)
                rb = self.sb(es1, "rb", [128, 32], F32)
                cqTs = Rot('cqT', [self.sb(es1, f"cqT{i}", [128, 6, 128], BF16) for i in range(2)])
                qf = self.sb(es1, "qf", [128, 1536], F32)
                qb = self.sb(es1, "qb", [128, 1536], BF16)
                ta = self.sb(es1, "ta", [128, 16, 32], F32)
                tb = self.sb(es1, "tb", [128, 16, 32], F32)
                qst = self.sb(es1, "qst", [96, 16, 512], BF16)
                ssa = self.sb(es1, "ssa", [128, NT], F32)
                ssb = self.sb(es1, "ssb", [128, NT], F32)
                ssk = self.sb(es1, "ssk", [128, NT], F32)
                rsq = self.sb(es1, "rsq", [128, NT], F32)
                rsk = self.sb(es1, "rsk", [128, NT], F32)
                for t in range(DBG.get('mla_nt', NT)):
                    ts = slice(t * 128, (t + 1) * 128)
                    t1 = slice(t, t + 1)
                    for (dst, dres, c0, c1) in ((A, Ares, 0, 512), (B, Bres, 512, 768), (Cb, Cres, 768, 1056)):
                        for k in range(8):
                            self.mm(dst[:, 0:c1 - c0], xnT[:, k, ts], wdkv[:, k, c0:c1], k == 0, k == 7,
                                    [('xnT', t // 4), ('wdkv', k), 'wdkvr'], [dres])
                    if DBG.get('mla_step', 99) <= 1:
                        continue
                    junk, jres = self.junk.next()
                    self.act(junk[:, 0:512], A[:], AF.Square, [Ares], [jres, ('ssa', t)], accum=ssa[:, t1])
                    junk, jres = self.junk.next()
                    self.act(junk[:, 0:256], B[:, 0:256], AF.Square, [Bres], [jres, ('ssb', t)], accum=ssb[:, t1])
                    junk, jres = self.junk.next()
                    self.act(junk[:, 0:256], Cb[:, 0:256], AF.Square, [Cres], [jres, ('ssk', t)], accum=ssk[:, t1])
                    S.op('dve', lambda e, t1=t1: e.tensor_tensor(ssa[:, t1], ssa[:, t1], ssb[:, t1], ALU.add),
                         [('ssa', t), ('ssb', t)], [('ssa', t)])
                    self.rms_rstd(ssa[:, t1], rsq[:, t1], ('ssa', t), ('rsq', t), 768)
                    self.rms_rstd(ssk[:, t1], rsk[:, t1], ('ssk', t), ('rsk', t), 256)
                    if DBG.get('mla_step', 99) <= 2:
                        continue
                    cqn, cqres = cqns.next()
                    ckvn, ckres = ckvns.next()
                    S.op('dve', lambda e, cqn=cqn, t1=t1: e.scalar_tensor_tensor(
                        out=cqn[:, 0:512], in0=A[:], scalar=rsq[:, t1], in1=qg[:, 0:512], op0=ALU.mult, op1=ALU.mult),
                        [Ares, ('rsq', t), 'qg'], [cqres])
                    S.op('dve', lambda e, cqn=cqn, t1=t1: e.scalar_tensor_tensor(
                        out=cqn[:, 512:768], in0=B[:, 0:256], scalar=rsq[:, t1], in1=qg[:, 512:768],
                        op0=ALU.mult, op1=ALU.mult), [Bres, ('rsq', t), 'qg'], [cqres])
                    S.op('dve', lambda e, ckvn=ckvn, t1=t1: e.scalar_tensor_tensor(
                        out=ckvn[:], in0=Cb[:, 0:256], scalar=rsk[:, t1], in1=kvg[:], op0=ALU.mult, op1=ALU.mult),
                        [Cres, ('rsk', t), 'kvg'], [ckres])
                    if DBG.get('mla_step', 99) <= 3:
                        continue
                    krr, krres = krrs.next()
                    S.op('dve', lambda e, t=t: e.tensor_tensor(ra[:], Cb[:, 256:288], cs[:, t, :], ALU.mult),
                         [Cres, 'ropecs', ('ssk', t)], ['ra'])
                    S.op('dve', lambda e, t=t: e.tensor_tensor(rb[:, 0:16], Cb[:, 272:288], sn[:, t, 0:16], ALU.mult),
                         [Cres, 'ropesn', ('ssk', t)], ['rb'])
                    S.op('dve', lambda e, t=t: e.tensor_tensor(rb[:, 16:32], Cb[:, 256:272], sn[:, t, 16:32], ALU.mult),
                         [Cres, 'ropesn', ('ssk', t)], ['rb'])
                    S.op('dve', lambda e, krr=krr: e.tensor_tensor(krr[:, 0:32], ra[:], rb[:], ALU.add),
                         ['ra', 'rb'], [krres])
                    if DBG.get('mla_step', 99) <= 4:
                        continue
                    m5 = DBG.get('mla5', 7)
                    cqT, cqTres = cqTs.next()
                    if m5 & 1:
                        for k in range(6):
                            self.tr(tpA[:, k * 128:(k + 1) * 128], cqn[:, k * 128:(k + 1) * 128], [cqres], [tAres])
                    if m5 & 2:
                        for k in range(2):
                            self.tr(tpA[:, 768 + k * 128:768 + (k + 1) * 128], ckvn[:, k * 128:(k + 1) * 128], [ckres],
                                    [tAres])
                    if m5 & 4:
                        self.tr(tpB[:, 0:128], krr[:], [krres], [tBres])
                    if m5 & 1:
                        self.act(cqT[:], tpA[:, 0:768].rearrange("p (k n) -> p k n", k=6), AF.Copy, [tAres], [cqTres])
                    if m5 & 2:
                        self.act(ckvT[:, :, ts], tpA[:, 768:1024].rearrange("p (k n) -> p k n", k=2), AF.Copy,
                                 [tAres], [('ckvT', t // 4)])
                    if m5 & 4:
                        S.op('dve', lambda e, ts=ts: e.tensor_copy(krT[0:32, ts], tpB[0:32, 0:128]), [tBres],
                             [('krT', t // 4)])
                    if DBG.get('mla_step', 99) <= 5:
                        continue
                    for j in range(3):
                        Qj, Qres = Q[j]
                        for k in range(6):
                            self.mm(Qj[:], cqT[:, k, :], wuq[:, k, j * 512:(j + 1) * 512], k == 0, k == 5,
                                    [cqTres, ('wuq', k), ('wuq2', k)], [Qres])
                        self.act(qf[:, j * 512:(j + 1) * 512], Qj[:], AF.Copy, [Qres], ['qf'])
                    if DBG.get('mla_step', 99) <= 6:
                        continue
                    qv = qf[:].rearrange("p (h d) -> p h d", h=16)
                    qbv = qb[:].rearrange("p (h d) -> p h d", h=16)
                    csb = cs[:, t:t + 1, :].broadcast_to([128, 16, 32])
                    snb = sn[:, t:t + 1, :].broadcast_to([128, 16, 32])
                    S.op('dve', lambda e, qv=qv, csb=csb: e.tensor_tensor(ta[:], qv[:, :, 64:96], csb, ALU.mult),
                         ['qf', 'ropecs'], ['ta'])
                    S.op('dve', lambda e, qv=qv, snb=snb: e.tensor_tensor(tb[:, :, 0:16], qv[:, :, 80:96],
                                                                      snb[:, :, 0:16], ALU.mult),
                         ['qf', 'ropesn'], ['tb'])
                    S.op('dve', lambda e, qv=qv, snb=snb: e.tensor_tensor(tb[:, :, 16:32], qv[:, :, 64:80],
                                                                      snb[:, :, 16:32], ALU.mult),
                         ['qf', 'ropesn'], ['tb'])
                    S.op('pool', lambda e, qv=qv, qbv=qbv: e.tensor_copy(qbv[:, :, 0:64], qv[:, :, 0:64]), ['qf'], ['qb'])
                    S.op('dve', lambda e, qbv=qbv: e.tensor_tensor(qbv[:, :, 64:96], ta[:], tb[:], ALU.add),
                         ['ta', 'tb'], ['qb'])
                    if DBG.get('mla_step', 99) <= 7:
                        continue
                    for hh in range(16):
                        tp_, tr_ = (tpA, tAres) if hh < 8 else (tpB, tBres)
                        self.tr(tp_[0:96, (hh % 8) * 128:(hh % 8 + 1) * 128], qb[:, hh * 96:(hh + 1) * 96], ['qb'], [tr_])
                    tsub = t % 4
                    self.act(qst[:, 0:8, tsub * 128:(tsub + 1) * 128], tpA[0:96, :].rearrange("p (h n) -> p h n", h=8),
                             AF.Copy, [tAres], ['qst'])
                    S.op('dve', lambda e, tsub=tsub: e.tensor_copy(qst[:, 8:16, tsub * 128:(tsub + 1) * 128],
                                                                 tpB[0:96, :].rearrange("p (h n) -> p h n", h=8)),
                         [tBres], ['qst'])
                    if DBG.get('mla_step', 99) <= 8:
                        continue
                    if tsub == 3:
                        g = t // 4
                        self.dma(self.qTs[:, :, g * 512:(g + 1) * 512].rearrange("h r n -> r h n"), qst[:], ['qst'],
                                 [('qTs', g)])
            S.barrier()
            with contextlib.ExitStack() as es2:
                self.alloc_attn_common(es2)
                if DBG.get('mla_stage1_only'):
                    return
                qThs = Rot('qTh', [self.sb(es2, f"qTh{i}", [96, SEQ], BF16) for i in range(2)])
                kThs = Rot('kTh', [self.sb(es2, f"kTh{i}", [96, SEQ], BF16) for i in range(2)])
                vaugs = [self.sb(es2, f"vaugm{i}", [128, NT, 128], BF16) for i in range(2)]
                S.op('pool', lambda e: e.memset(vaugs[0][:, :, 64:128], 1.0), [], [('vaugm1', 0)])
                S.op('pool', lambda e: e.memset(vaugs[1][:, :, 0:64], 1.0), [], [('vaugm1', 1)])
                vTz = [Rot(f'vTz{sl}', [self.sb(es2, f"vTz{sl}_{i}", [128, 512], BF16) for i in range(2)])
                       for sl in range(2)]
                for sl in range(2):
                    for i in range(2):
                        zr = slice(64, 128) if sl == 0 else slice(0, 64)
                        S.op('pool', lambda e, sl=sl, i=i, zr=zr: e.memset(vTz[sl].tiles[i][zr, :], 0.0), [],
                             [(f'vTz{sl}z', i)])
                scale = 96.0 ** -0.5
                for h in range(16):
                    slot = h % 2
                    qTh, qres = qThs.next()
                    self.dma(qTh[:], self.qTs[h], [('qTs', g) for g in range(8)], [qres])
                    kTh, kres = kThs.next()
                    va = vaugs[slot]
                    S.op('pool', lambda e, kTh=kTh: e.tensor_copy(kTh[64:96, :], krT[0:32, :]),
                         [('krT', g) for g in range(8)], [(kres, 'r')])
                    for c in range(8):
                        cs_ = slice(c * 512, (c + 1) * 512)
                        pj, pres = self.pjs.next()
                        for k in range(2):
                            self.mm(pj[:], wukv[:, k, h * 128:(h + 1) * 128], ckvT[:, k, cs_], k == 0, k == 1,
                                    [('wukv', k, h // 8), ('ckvT', c)], [pres])
                        self.act(kTh[0:64, cs_], pj[0:64, :], AF.Copy, [pres], [(kres, c)])
                        vt, vtres = vTz[slot].next()
                        zres = (f'vTz{slot}z', vtres[1])
                        if slot == 0:
                            S.op('dve', lambda e, vt=vt, pj=pj: e.tensor_copy(vt[0:64, :], pj[64:128, :]),
                                 [pres], [vtres])
                        else:
                            S.op('dve', lambda e, vt=vt, pj=pj: e.tensor_copy(vt[64:128, :], pj[64:128, :]),
                                 [pres], [vtres])
                        tp, tres = self.tps.next()
                        for jj in range(4):
                            self.tr(tp[:, jj * 128:(jj + 1) * 128], vt[:, jj * 128:(jj + 1) * 128], [vtres, zres], [tres])
                        tv = tp[:, 0:512].rearrange("p (a b) -> p a b", a=4)
                        vs = slice(0, 64) if slot == 0 else slice(64, 128)
                        S.op('dve', lambda e, va=va, tv=tv, c=c, vs=vs: e.tensor_copy(va[:, 4 * c:4 * c + 4, vs],
                                                                                  tv[:, :, vs]),
                             [tres, ('vaugm1', slot)], [('vaugm', slot, c)])
                    for c in range(8):
                        oa, ores = self.oas.next()
                        nk = 4 * c + 4
                        for kt in range(nk):
                            ks = slice(kt * 128, (kt + 1) * 128)
                            rd = [(kres, kt // 4), (kres, 'r'), qres]
                            if kt < 4 * c:
                                col0, n = 0, 512
                                qk = [(kTh[:, ks], qTh[:, c * 512:(c + 1) * 512], rd)]
                            else:
                                col0 = 128 * (kt - 4 * c)
                                n = 512 - col0
                                qk = [(kTh[:, ks], qTh[:, c * 512 + col0:(c + 1) * 512], rd),
                                      (self.ident[:], mtri[:, 0:n], ['ident', 'mtri'])]
                            after = None
                            if kt == nk - 1:
                                after = (lambda oa=oa, ores=ores, slot=slot, h=h, c=c: self.normalize_out(
                                    lambda rows: oa[rows, 0:512], ores, 512, slot, h // 2, c * 512))
                            self.attn_step(n, qk, scale, (oa[:, col0:512], va[:, kt, :],
                                                          [('vaugm', slot, kt // 4), ('vaugm1', slot)], ores,
                                                          kt == 0, kt == nk - 1), after)
                    self.attn_flush()
        S.barrier()

    def build(self):
        nc = self.nc
        S = self.S
        with contextlib.ExitStack() as es:
            self.ident = self.sb(es, "ident", [128, 128], BF16)
            self.dma(self.ident[:], self.C['ident'], [], ['ident'])
            self.epsc = self.sb(es, "epsc", [128, 1], F32)
            S.op('dve', lambda e: e.memset(self.epsc[:], EPS), [], ['epsc'])
            self.junk = Rot('junk', [self.sb(es, f"junk{i}", [128, DM], BF16) for i in range(2)])
            ps = lambda name, shape, dt: es.enter_context(nc.psum_tensor(name, shape, dt))
            self.tps = Rot('tp', [ps(f"tp{i}", [128, 1024], BF16) for i in range(2)])
            self.pjs = Rot('pj', [ps(f"pj{i}", [128, 512], F32) for i in range(2)])
            self.scs = Rot('sc', [ps(f"sc{i}", [128, 512], F32) for i in range(2)])
            self.oas = Rot('oa', [ps(f"oa{i}", [128, 512], F32) for i in range(2)])
            hsrc = self.x
            for n, li in enumerate(self.layers):
                last = (n == len(self.layers) - 1)
                with contextlib.ExitStack() as es2:
                    xnT = self.sb(es2, "xnT", [128, 8, SEQ], BF16)
                    with contextlib.ExitStack() as es3:
                        self.phase_norm(es3, hsrc, f'l{li}_attn_norm', xnT)
                    S.barrier()
                    npair = 8
                    if li == 3:
                        self.phase_swa(li, xnT)
                    elif li == 1:
                        npair = 4
                        self.phase_dilated(li, xnT)
                    elif li == 0:
                        self.phase_moba(li, xnT)
                    elif li == 2:
                        self.phase_mla(li, xnT)
                    else:
                        raise NotImplementedError
                self.phase_oproj(li, npair, hsrc)
                self.phase_ffn(li, last)
                hsrc = self.hbuf
            S.barrier()
            S.emit()
        return nc


_CONSTS = None


def run(inputs, layers=(0, 1, 2, 3), final=True, cores=8, x_override=None):
    global _CONSTS
    if _CONSTS is None:
        _CONSTS = host_consts()
    b = Builder(layers, final)
    nc = b.build()
    x = np.ascontiguousarray(inputs['x'], dtype=np.float32) if x_override is None else x_override
    in_maps = []
    for c in range(cores):
        m = {'x': np.ascontiguousarray(x[c])}
        for name in b.used_inputs():
            m[name] = np.ascontiguousarray(inputs[name], dtype=np.float32)
        for name in CONST_SPECS:
            m['c_' + name] = _CONSTS[name]
        in_maps.append(m)
    res = run_bass_kernel_spmd(nc, in_maps, core_ids=list(range(cores)))
    return np.stack([np.asarray(r['y']) for r in res.results], axis=0)


def kernel(**inputs):
    out = run(inputs)
    return out.astype(np.float32)
```

```python
import contextlib
import numpy as np
import ml_dtypes
import concourse.bass as bass
import concourse.mybir as mybir
from concourse.bass_utils import run_bass_kernel_spmd

F32 = mybir.dt.float32
BF16 = mybir.dt.bfloat16
AF = mybir.ActivationFunctionType
ALU = mybir.AluOpType
AX = mybir.AxisListType
NPBF = ml_dtypes.bfloat16

SEQ = 4096
DM = 1024
NT = SEQ // 128
DFF = 4096
EPS = 1e-6
NEG = -30000.0

ENGS = ['pe', 'act', 'dve', 'pool', 'sp']
NDMASEM = 8
DBG = {}


class Op:
    __slots__ = ('fn', 'waits', 'key', 'idx', 'signal', 'isdma')

    def __init__(self, fn, key, idx, isdma):
        self.fn = fn
        self.waits = []
        self.key = key
        self.idx = idx
        self.signal = False
        self.isdma = isdma


class Sched:
    def __init__(self, nc):
        self.nc = nc
        self.streams = {e: [] for e in ENGS}
        self.keyops = {}
        self.seen = {e: {} for e in ENGS}
        self.res = {}
        self.dma_rr = {e: 0 for e in ENGS}
        self.alias = {}

    def _expand(self, lst):
        if not self.alias:
            return lst
        out = []
        for r in lst:
            out.extend(self.alias.get(r, (r,)))
        return out

    def _need(self, eng, op, tok):
        key, idx = tok
        if self.seen[eng].get(key, -1) >= idx:
            return
        self.seen[eng][key] = idx
        op.waits.append(tok)

    def _deps(self, eng, op, reads, writes, mykey, same_ok):
        for r in reads:
            st = self.res.get(r)
            if st is None:
                continue
            w = st[0]
            if w is not None and not (same_ok and w[0] == mykey):
                self._need(eng, op, w)
        for r in writes:
            st = self.res.get(r)
            if st is None:
                continue
            w = st[0]
            if w is not None and w[0] != mykey:
                self._need(eng, op, w)
            for k, i in st[1].items():
                if k != mykey:
                    self._need(eng, op, (k, i))

    def _commit(self, tok, reads, writes):
        for r in reads:
            st = self.res.get(r)
            if st is None:
                st = self.res[r] = [None, {}]
            st[1][tok[0]] = tok[1]
        for r in writes:
            self.res[r] = [tok, {}]

    def op(self, eng, fn, reads=(), writes=()):
        reads = self._expand(reads)
        writes = self._expand(writes)
        key = eng
        lst = self.keyops.setdefault(key, [])
        o = Op(fn, key, len(lst), False)
        self._deps(eng, o, reads, writes, key, same_ok=(eng == 'pe'))
        lst.append(o)
        self.streams[eng].append(o)
        self._commit((key, o.idx), reads, writes)
        return o

    def dma(self, q, fn, reads=(), writes=()):
        reads = self._expand(reads)
        writes = self._expand(writes)
        j = self.dma_rr[q]
        self.dma_rr[q] = (j + 1) % NDMASEM
        key = ('dma', q, j)
        lst = self.keyops.setdefault(key, [])
        o = Op(fn, key, len(lst), True)
        if lst:
            self._need(q, o, (key, len(lst) - 1))
        self._deps(q, o, reads, writes, key, same_ok=False)
        lst.append(o)
        self.streams[q].append(o)
        self._commit((key, o.idx), reads, writes)
        return o

    def barrier(self):
        toks = [(key, len(lst) - 1) for key, lst in self.keyops.items() if lst]
        for e in ENGS:
            o = Op(None, None, None, False)
            for t in toks:
                self._need(e, o, t)
            if o.waits:
                self.streams[e].append(o)

    def emit(self):
        nc = self.nc
        for e in ENGS:
            for o in self.streams[e]:
                for (key, idx) in o.waits:
                    self.keyops[key][idx].signal = True
        semval = {}
        for key, lst in self.keyops.items():
            c = 0
            for o in lst:
                if o.isdma:
                    c += 16
                    semval[(key, o.idx)] = c
                    o.signal = True
                elif o.signal:
                    c += 1
                    semval[(key, o.idx)] = c
            assert c < 60000, (key, c)
        keys = [k for k, l in self.keyops.items() if l]
        with contextlib.ExitStack() as es:
            sems = {}
            for k in keys:
                nm = 's_' + ('_'.join(str(x) for x in k) if isinstance(k, tuple) else k)
                sems[k] = es.enter_context(nc.semaphore(nm))
            block = es.enter_context(nc.Block())

            def run(e):
                def body(eng):
                    for o in self.streams[e]:
                        for tok in o.waits:
                            eng.wait_ge(sems[tok[0]], semval[tok])
                        if o.fn is None:
                            continue
                        ins = o.fn(eng)
                        if o.signal:
                            ins.then_inc(sems[o.key], 16 if o.isdma else 1)
                return body
            if self.streams['pe']:
                block.tensor(run('pe'))
            if self.streams['act']:
                block.scalar(run('act'))
            if self.streams['dve']:
                block.vector(run('dve'))
            if self.streams['pool']:
                block.gpsimd(run('pool'))
            if self.streams['sp']:
                block.sync(run('sp'))


class Rot:
    def __init__(self, name, tiles):
        self.name = name
        self.tiles = tiles
        self.i = 0

    def next(self):
        j = self.i % len(self.tiles)
        self.i += 1
        return self.tiles[j], (self.name, j)


def alibi(n):
    return (2.0 ** (-8.0 * np.arange(1, n + 1, dtype=np.float64) / n))


def split_hi_lo(v):
    v = v.astype(np.float32)
    hi = v.astype(NPBF)
    lo = (v - hi.astype(np.float32)).astype(NPBF)
    return hi, lo


def band_bias(slope_eff, W, width=256):
    k = np.arange(128)[:, None].astype(np.float64)
    col = np.arange(width)[None, :].astype(np.float64)
    diff = col - k
    val = -slope_eff * diff * 8.0
    val = np.where((diff >= 0) & (diff < W), val, NEG)
    return val.astype(np.float32)


def host_consts():
    c = {}
    c['ident'] = np.eye(128, dtype=np.float32).astype(NPBF)
    sl = alibi(16)
    t = np.zeros((128, 16, 2, 256), NPBF)
    for h in range(16):
        hi, lo = split_hi_lo(band_bias(sl[h], 128))
        t[:, h, 0], t[:, h, 1] = hi, lo
    c['bsw'] = t
    sl24 = alibi(24)
    dils = (1, 4, 16)
    t = np.zeros((128, 24, 2, 256), NPBF)
    for g in range(3):
        for hs in range(8):
            hi, lo = split_hi_lo(band_bias(sl24[g * 8 + hs] * dils[g], 129))
            t[:, g * 8 + hs, 0], t[:, g * 8 + hs, 1] = hi, lo
    c['bdl'] = t
    t = np.zeros((128, 16, 2, 256), NPBF)
    for h in range(16):
        hi, lo = split_hi_lo(band_bias(sl[h], 10 ** 9))
        t[:, h, 0], t[:, h, 1] = hi, lo
    c['bmo'] = t
    p = np.arange(128)[:, None, None].astype(np.float64)
    j = np.arange(2)[None, :, None].astype(np.float64)
    c['tqm'] = (-sl[None, None, :] * 8.0 * (j * 128 + p) - 30000.0).astype(np.float32)
    idx = np.arange(31)[None, None, :].astype(np.float64)
    c['dtab'] = np.broadcast_to((-sl[None, :, None] * 2048.0 * (15 - idx)), (128, 16, 31)).astype(np.float32).copy()
    hi, lo = split_hi_lo(np.broadcast_to((sl * 8.0)[None, :], (128, 16)).copy())
    c['slp'] = np.stack([hi, lo], axis=-1)
    X = np.zeros((128, 16, 2, 128), np.float32)
    for n in range(16):
        X[n, n] = 1.0
        X[16 + n, n] = 1.0
    for half in range(2):
        X[32, :, half, :] = np.arange(128)[None, :] + 128 * half
        X[33, :, half, :] = np.arange(128)[None, :] + 128 * half
    c['xsel'] = X.astype(NPBF)
    XT = np.zeros((64, SEQ), np.float32)
    pos = np.arange(SEQ)
    for r in range(16):
        XT[r] = (pos // 256 == r)
        XT[16 + r] = (pos // 256 == r)
    XT[32] = pos % 256
    XT[33] = pos % 256
    c['xselT'] = XT.astype(NPBF)
    k = np.arange(128)[:, None]
    col = np.arange(512)[None, :]
    c['mtri'] = np.where(col >= k, 0.0, NEG).astype(np.float32).astype(NPBF)
    inv = 10000.0 ** (-np.arange(0, 32, 2, dtype=np.float64) / 32)
    pos = (np.arange(32)[None, :] * 128 + np.arange(128)[:, None]).astype(np.float64)
    ang = pos[:, :, None] * inv[None, None, :]
    ang = ang.astype(np.float32).astype(np.float64)
    cos, sin = np.cos(ang), np.sin(ang)
    c['ropecs'] = np.concatenate([cos, cos], axis=-1).astype(np.float32)
    c['ropesn'] = np.concatenate([-sin, sin], axis=-1).astype(np.float32)
    return c


CONST_SPECS = {
    'ident': ([128, 128], BF16), 'bsw': ([128, 16, 2, 256], BF16), 'bdl': ([128, 24, 2, 256], BF16),
    'bmo': ([128, 16, 2, 256], BF16), 'tqm': ([128, 2, 16], F32), 'dtab': ([128, 16, 31], F32),
    'slp': ([128, 16, 2], BF16), 'xsel': ([128, 16, 2, 128], BF16), 'xselT': ([64, SEQ], BF16), 'mtri': ([128, 512], BF16),
    'ropecs': ([128, 32, 32], F32), 'ropesn': ([128, 32, 32], F32),
}

WEIGHT_SPECS = [
    ('l0_attn_norm', [1024]), ('l0_w_qkv', [1024, 3072]), ('l0_w_o', [1024, 1024]), ('l0_mlp_norm', [1024]),
    ('l0_w_up', [1024, 4096]), ('l0_w_down', [4096, 1024]),
    ('l1_attn_norm', [1024]), ('l1_w_qkv', [1024, 4608]), ('l1_w_o', [512, 1024]), ('l1_mlp_norm', [1024]),
    ('l1_w_up', [1024, 4096]), ('l1_w_down', [4096, 1024]),
    ('l2_attn_norm', [1024]), ('l2_w_dkv', [1024, 1056]), ('l2_q_norm', [768]), ('l2_w_uq', [768, 1536]),
    ('l2_kv_norm', [256]), ('l2_w_ukv', [256, 2048]), ('l2_w_o', [1024, 1024]), ('l2_mlp_norm', [1024]),
    ('l2_w_up', [1024, 4096]), ('l2_w_down', [4096, 1024]),
    ('l3_attn_norm', [1024]), ('l3_w_qkv', [1024, 1280]), ('l3_sinks', [16]), ('l3_w_o', [1024, 1024]),
    ('l3_mlp_norm', [1024]), ('l3_w_up', [1024, 4096]), ('l3_w_down', [4096, 1024]),
    ('final_norm', [1024]),
]


class Builder:
    def __init__(self, layers=(0, 1, 2, 3), final=True):
        self.layers = tuple(layers)
        self.final = final
        nc = self.nc = bass.Bass("TRN2", target_bir_lowering=False)
        self.S = Sched(nc)
        self.x = nc.dram_tensor("x", [SEQ, DM], F32, kind="ExternalInput").ap()
        self.W = {}
        for name, shape in WEIGHT_SPECS:
            if name == 'final_norm' or int(name[1]) in self.layers:
                self.W[name] = nc.dram_tensor(name, shape, F32, kind="ExternalInput").ap()
        self.C = {}
        for name, (shape, dt) in CONST_SPECS.items():
            self.C[name] = nc.dram_tensor("c_" + name, shape, dt, kind="ExternalInput").ap()
        self.y = nc.dram_tensor("y", [SEQ, DM], F32, kind="ExternalOutput").ap()
        self.hbuf = nc.dram_tensor("hbuf", [SEQ, DM], F32, kind="Internal").ap()
        self.oTs = nc.dram_tensor("oTs", [8, 128, SEQ], BF16, kind="Internal").ap()
        self.qTs = nc.dram_tensor("qTs", [16, 96, SEQ], BF16, kind="Internal").ap()
        self.cast_rr = 0
        for c in range(8):
            self.S.alias[('xnT', c)] = [('xnTs', c, 0), ('xnTs', c, 1)]

    def used_inputs(self):
        return list(self.W.keys())

    def sb(self, es, name, shape, dt):
        self.uid = getattr(self, 'uid', 0) + 1
        return es.enter_context(self.nc.sbuf_tensor(f"{name}_{self.uid}", shape, dt))

    def mm(self, out, lhsT, rhs, start, stop, reads, writes, skip=False):
        self.S.op('pe', lambda e: e.matmul(out, lhsT=lhsT, rhs=rhs, start=start, stop=stop,
                                           skip_group_check=skip), reads, writes)

    def tr(self, out, in_, reads, writes):
        ident = self.ident
        self.S.op('pe', lambda e: e.transpose(out, in_, ident[:]), list(reads) + ['ident'], writes)

    def act(self, out, in_, func, reads, writes, scale=1.0, bias=None, accum=None):
        kw = {}
        if bias is not None:
            kw['bias'] = bias
        if accum is not None:
            kw['accum_out'] = accum
        self.S.op('act', lambda e: e.activation(out=out, in_=in_, func=func, scale=scale, **kw), reads, writes)

    def dma(self, out, in_, reads, writes, q='sp'):
        self.S.dma(q, lambda e: e.dma_start(out=out, in_=in_), reads, writes)

    def load_cast(self, dst, src, n, reads_src=(), wres=None, shape3=None, engs=('dve', 'pool')):
        stg, sres = self.stg.next()
        sv = stg[:, 0:n]
        if shape3 is not None:
            sv = sv.rearrange("p (a b) -> p a b", a=shape3[0])
        self.dma(sv, src, list(reads_src), [sres])
        eng = engs[self.cast_rr % len(engs)]
        self.cast_rr += 1
        if eng == 'act':
            self.act(dst, sv, AF.Copy, [sres], [wres])
        else:
            self.S.op(eng, lambda e: e.tensor_copy(dst, sv), [sres], [wres])

    def rms_stats(self, src_ap, ss_ap, reads, ssres):
        junk, jres = self.junk.next()
        self.act(junk[:], src_ap, AF.Square, reads, [jres, ssres], accum=ss_ap)

    def rms_rstd(self, ss_ap, rstd_ap, ssres, rres, n_feat):
        self.act(rstd_ap, ss_ap, AF.Ln, [ssres, 'epsc'], [rres], scale=1.0 / n_feat, bias=self.epsc[:, 0:1])
        self.act(rstd_ap, rstd_ap, AF.Exp, [rres], [rres], scale=-0.5)

    def phase_norm(self, es, src_dram, gname, xnT):
        S = self.S
        nc = self.nc
        gbc = self.sb(es, "gbc", [128, DM], F32)
        self.dma(gbc[:], self.W[gname].partition_broadcast(128), [], ['gbc'])
        hts = Rot('ht', [self.sb(es, f"ht{i}", [128, DM], F32) for i in range(4)])
        xns = Rot('xn', [self.sb(es, f"xn{i}", [128, DM], BF16) for i in range(3)])
        ss = self.sb(es, "ssn", [128, NT], F32)
        rs = self.sb(es, "rsn", [128, NT], F32)
        pend = None
        for t in range(NT + 1):
            cur = None
            if t < NT:
                ht, hres = hts.next()
                self.dma(ht[:], src_dram[t * 128:(t + 1) * 128, :], [('h', t // 4)], [hres])
                self.rms_stats(ht[:], ss[:, t:t + 1], [hres], ('ssn', t))
                self.rms_rstd(ss[:, t:t + 1], rs[:, t:t + 1], ('ssn', t), ('rsn', t), DM)
                xn, xres = xns.next()
                S.op('dve', lambda e, xn=xn, ht=ht, t=t: e.scalar_tensor_tensor(
                    out=xn[:], in0=ht[:], scalar=rs[:, t:t + 1], in1=gbc[:], op0=ALU.mult, op1=ALU.mult),
                    [hres, ('rsn', t), 'gbc'], [xres])
                tp, tres = self.tps.next()
                for k in range(8):
                    self.tr(tp[:, k * 128:(k + 1) * 128], xn[:, k * 128:(k + 1) * 128], [xres], [tres])
                cur = (t, tp, tres)
            if pend is not None:
                pt_, tp_, tres_ = pend
                dst = xnT[:, :, pt_ * 128:(pt_ + 1) * 128]
                src = tp_[:].rearrange("p (k n) -> p k n", k=8)
                if pt_ % 2 == 0:
                    self.act(dst, src, AF.Copy, [tres_], [('xnTs', pt_ // 4, 0)])
                else:
                    S.op('dve', lambda e, dst=dst, src=src: e.tensor_copy(dst, src), [tres_], [('xnTs', pt_ // 4, 1)])
            pend = cur

    def phase_of(self, li, npair, hsrc, last):
        S = self.S
        W = self.W
        fin = last and self.final
        with contextlib.ExitStack() as es:
            wup = self.sb(es, "wup", [128, 8, DFF], BF16)
            wdn = self.sb(es, "wdn", [128, 32, DM], BF16)
            self.stg = Rot('stg', [self.sb(es, f"stgf{i}", [128, 1024], F32) for i in range(3)])
            wupv = W[f'l{li}_w_up'].rearrange("(k p) n -> p k n", p=128)
            wdnv = W[f'l{li}_w_down'].rearrange("(k p) n -> p k n", p=128)
            pieces = []
            for qq in range(4):
                for k in range(8):
                    pieces.append((wup[:, k, qq * 1024:(qq + 1) * 1024], wupv[:, k, qq * 1024:(qq + 1) * 1024],
                                   ('wup', k, qq)))
            for c in range(32):
                pieces.append((wdn[:, c, :], wdnv[:, c, :], ('wdn', c)))

            def emit_pieces(n):
                for _ in range(n):
                    if pieces:
                        dst, src, wres = pieces.pop(0)
                        self.load_cast(dst, src, 1024, wres=wres, engs=('act', 'dve'))

            with contextlib.ExitStack() as es1:
                wo = self.sb(es1, "wo", [128, npair, DM], BF16)
                wov = W[f'l{li}_w_o'].rearrange("(k p) n -> p k n", p=128)
                for k in range(npair):
                    self.load_cast(wo[:, k, :], wov[:, k, :], DM, wres=('wo', k), engs=('act', 'dve'))
                oTg = Rot('oTg', [self.sb(es1, f"oTg{i}", [128, npair, 256], BF16) for i in range(2)])
                hgs = Rot('hgo', [self.sb(es1, f"hgo{i}", [128, 2, DM], F32) for i in range(2)])

                def loads(g):
                    og, ores = oTg.next()
                    hg, hres = hgs.next()
                    self.dma(og[:], self.oTs[0:npair, :, g * 256:(g + 1) * 256].rearrange("a p n -> p a n"),
                             [('oTs', g // 2)], [ores])
                    self.dma(hg[:], hsrc[g * 256:(g + 1) * 256, :].rearrange("(t p) n -> p t n", p=128),
                             [('h', g // 2)], [hres])
                    return og, ores, hg, hres
                nxt = loads(0)
                for g in range(16):
                    og, ores, hg, hres = nxt
                    if g + 1 < 16:
                        nxt = loads(g + 1)
                    for t in range(2):
                        for hf in range(2):
                            pj, pres = self.pjs.next()
                            for k in range(npair):
                                self.mm(pj[:], og[:, k, t * 128:(t + 1) * 128], wo[:, k, hf * 512:(hf + 1) * 512],
                                        k == 0, k == npair - 1, [ores, ('wo', k)], [pres])
                            S.op('dve', lambda e, hg=hg, pj=pj, t=t, hf=hf: e.tensor_tensor(
                                hg[:, t, hf * 512:(hf + 1) * 512], pj[:], hg[:, t, hf * 512:(hf + 1) * 512], ALU.add),
                                [pres, hres], [hres])
                    self.dma(self.hbuf[g * 256:(g + 1) * 256, :].rearrange("(t p) n -> p t n", p=128), hg[:],
                             [hres], [('h', g // 2)])
                    emit_pieces(4)
                emit_pieces(64)
            S.barrier()
            with contextlib.ExitStack() as es2:
                gbc = self.sb(es2, "gbcf", [128, DM], F32)
                self.dma(gbc[:], W[f'l{li}_mlp_norm'].partition_broadcast(128), [], ['gbcf'])
                if fin:
                    gfin = self.sb(es2, "gfin", [128, DM], F32)
                    self.dma(gfin[:], W['final_norm'].partition_broadcast(128), [], ['gfin'])
                hgs = Rot('hg', [self.sb(es2, f"hg{i}", [128, 2, DM], F32) for i in range(2)])
                xns = Rot('xnf', [self.sb(es2, f"xnf{i}", [128, DM], BF16) for i in range(2)])
                xTs = Rot('xT', [self.sb(es2, f"xT{i}", [128, 8, 256], BF16) for i in range(2)])
                uTs = Rot('uT', [self.sb(es2, f"uT{i}", [128, 8, 256], BF16) for i in range(2)])
                rl = Rot('rl', [self.sb(es2, f"rl{i}", [128, 256], F32) for i in range(3)])
                ss = self.sb(es2, "ssf", [128, NT], F32)
                rs = self.sb(es2, "rsf", [128, NT], F32)
                ssl = self.sb(es2, "ssl", [128, NT], F32)
                rsl = self.sb(es2, "rsl", [128, NT], F32)

                def load(g):
                    hg, hres = hgs.next()
                    self.dma(hg[:], self.hbuf[g * 256:(g + 1) * 256, :].rearrange("(t p) n -> p t n", p=128),
                             [('h', g // 2)], [hres])
                    return hg, hres

                def prep(g, hg, hres):
                    x2, x2res = xTs.next()
                    for t in range(2):
                        i = g * 2 + t
                        self.rms_stats(hg[:, t, :], ss[:, i:i + 1], [hres], ('ssf', i))
                        self.rms_rstd(ss[:, i:i + 1], rs[:, i:i + 1], ('ssf', i), ('rsf', i), DM)
                        xn, xres = xns.next()
                        S.op('dve', lambda e, xn=xn, hg=hg, t=t, i=i: e.scalar_tensor_tensor(
                            out=xn[:], in0=hg[:, t, :], scalar=rs[:, i:i + 1], in1=gbc[:], op0=ALU.mult, op1=ALU.mult),
                            [hres, ('rsf', i), 'gbcf'], [xres])
                        tp, tres = self.tps.next()
                        for k in range(8):
                            self.tr(tp[:, k * 128:(k + 1) * 128], xn[:, k * 128:(k + 1) * 128], [xres], [tres])
                        self.act(x2[:, :, t * 128:(t + 1) * 128], tp[:].rearrange("p (k n) -> p k n", k=8), AF.Copy,
                                 [tres], [x2res])
                    return x2, x2res

                cur = load(0)
                curx = prep(0, *cur)
                for g in range(16):
                    hg, hres = cur
                    x2, x2res = curx
                    if g + 1 < 16:
                        nxt = load(g + 1)
                    for q in range(4):
                        uT, ures = uTs.next()
                        for cc in range(8):
                            c = q * 8 + cc
                            sc, sres = self.scs.next()
                            for k in range(8):
                                self.mm(sc[:, 0:256], wup[:, k, c * 128:(c + 1) * 128], x2[:, k, :], k == 0, k == 7,
                                        [x2res, ('wup', k, c // 8)], [sres])
                            r, rres = rl.next()
                            self.act(r[:], sc[:, 0:256], AF.Relu, [sres], [rres])
                            S.op('dve', lambda e, r=r, cc=cc, uT=uT: e.tensor_tensor(uT[:, cc, :], r[:], r[:], ALU.mult),
                                 [rres], [(ures, cc)])
                        if q == 1 and g + 1 < 16:
                            nxtx = prep(g + 1, *nxt)
                        for t in range(2):
                            for hf in range(2):
                                pj, pres = self.pjs.next()
                                for cc in range(8):
                                    c = q * 8 + cc
                                    self.mm(pj[:], uT[:, cc, t * 128:(t + 1) * 128], wdn[:, c, hf * 512:(hf + 1) * 512],
                                            cc == 0, cc == 7, [(ures, cc), ('wdn', c)], [pres])
                                S.op('dve', lambda e, hg=hg, pj=pj, t=t, hf=hf: e.tensor_tensor(
                                    hg[:, t, hf * 512:(hf + 1) * 512], pj[:], hg[:, t, hf * 512:(hf + 1) * 512],
                                    ALU.add), [pres, hres], [hres])
                    rows = slice(g * 256, (g + 1) * 256)
                    if fin:
                        for t in range(2):
                            i = g * 2 + t
                            self.rms_stats(hg[:, t, :], ssl[:, i:i + 1], [hres], ('ssl', i))
                            self.rms_rstd(ssl[:, i:i + 1], rsl[:, i:i + 1], ('ssl', i), ('rsl', i), DM)
                            S.op('dve', lambda e, hg=hg, i=i, t=t: e.scalar_tensor_tensor(
                                out=hg[:, t, :], in0=hg[:, t, :], scalar=rsl[:, i:i + 1], in1=gfin[:],
                                op0=ALU.mult, op1=ALU.mult), [hres, ('rsl', i), 'gfin'], [hres])
                    dst = self.y if last else self.hbuf
                    self.dma(dst[rows, :].rearrange("(t p) n -> p t n", p=128), hg[:], [hres],
                             ['y'] if last else [('h', g // 2)])
                    if g + 1 < 16:
                        cur, curx = nxt, nxtx
        S.barrier()

    def normalize_out(self, src, sres, ncols, slot, pair, col0, extra=None, use_act=False):
        S = self.S
        orow = slice(0, 64) if slot == 0 else slice(64, 128)
        drow = slice(64, 128) if slot == 0 else slice(0, 64)
        rc, rres = self.rcs.next()
        if use_act:
            if extra is not None:
                sc_ap, sc_res = extra
                self.act(rc[orow, 0:ncols], src(drow), AF.Ln, [sres, sc_res], [rres], bias=sc_ap(orow))
            else:
                self.act(rc[orow, 0:ncols], src(drow), AF.Ln, [sres], [rres])
            self.act(rc[orow, 0:ncols], rc[orow, 0:ncols], AF.Exp, [rres], [rres], scale=-1.0)
        else:
            S.op('dve', lambda e: e.tensor_copy(rc[orow, 0:ncols], src(drow)), [sres], [rres])
            if extra is not None:
                sc_ap, sc_res = extra
                S.op('dve', lambda e: e.tensor_scalar(rc[orow, 0:ncols], rc[orow, 0:ncols], sc_ap(orow), None,
                                                      ALU.add), [rres, sc_res], [rres])
            S.op('dve', lambda e: e.reciprocal(rc[orow, 0:ncols], rc[orow, 0:ncols]), [rres], [rres])
        on, onres = self.ons.next()
        S.op('dve', lambda e: e.tensor_tensor(on[orow, 0:ncols], src(orow), rc[orow, 0:ncols], ALU.mult),
             [sres, rres], [onres])
        self.dma(self.oTs[pair, orow, col0:col0 + ncols], on[orow, 0:ncols], [onres], [('oTs', col0 // 512)])

    def alloc_attn_common(self, es, nslots=4):
        self.pts = Rot('pt', [self.sb(es, f"pt{i}", [128, 512], BF16) for i in range(6)])
        self.rcs = Rot('rc', [self.sb(es, f"rc{i}", [128, 512], F32) for i in range(3)])
        self.ons = Rot('on', [self.sb(es, f"on{i}", [128, 512], BF16) for i in range(2)])
        self.stg = Rot('stg', [self.sb(es, f"stga{i}", [128, 1024], F32) for i in range(3)])
        self.pending = []
        t0, t1 = self.scs.tiles
        p0, p1 = self.pjs.tiles
        if nslots == 4:
            self.scslots = [(t0, 0, ('sc', 0)), (t1, 0, ('sc', 1)), (p0, 0, ('pj', 0)), (p1, 0, ('pj', 1))]
        else:
            self.scslots = [(t0, 0, ('sc', 0)), (t1, 0, ('sc', 1)), (p0, 0, ('pj', 0))]
        self.skew = 2
        self.sci = 0

    def attn_step(self, n, qk_list, scale, pv, after=None):
        tile, off, sres = self.scslots[self.sci % len(self.scslots)]
        self.sci += 1
        sc = tile[:, off:off + n]
        for i, (lhsT, rhs, reads) in enumerate(qk_list):
            self.mm(sc, lhsT, rhs, i == 0, i == len(qk_list) - 1, reads, [sres])
        pt, ptres = self.pts.next()
        self.act(pt[:, 0:n], sc, AF.Exp, [sres], [ptres], scale=scale)
        self.pending.append((pv, pt, ptres, n, after))
        while len(self.pending) > self.skew:
            self._attn_pop()

    def _attn_pop(self):
        pv, pt, ptres, n, after = self.pending.pop(0)
        out_ap, lhsT, reads, ores, first, last = pv
        self.mm(out_ap, lhsT, pt[:, 0:n], first, last, list(reads) + [ptres], [ores], skip=True)
        if after is not None:
            after()

    def attn_flush(self):
        while self.pending:
            self._attn_pop()

    def phase_swa(self, li, xnT):
        S = self.S
        W = self.W
        with contextlib.ExitStack() as es:
            self.alloc_attn_common(es)
            bsw = self.sb(es, "bsw", [128, 16, 2, 256], BF16)
            self.dma(bsw[:], self.C['bsw'], [], ['bsw'])
            esink = self.sb(es, "esink", [128, 16], F32)
            self.dma(esink[:], W[f'l{li}_sinks'].partition_broadcast(128), [], ['esink'])
            self.act(esink[:], esink[:], AF.Exp, ['esink'], ['esink'])
            wkv = self.sb(es, "wkv", [128, 8, 256], BF16)
            wqs = Rot('wq', [self.sb(es, f"wq{i}", [128, 8, 128], BF16) for i in range(2)])
            kTv = self.sb(es, "kTv", [128, 2, 2, SEQ], BF16)
            vaug = self.sb(es, "vaug", [128, NT, 2, 2, 128], BF16)
            qTs = Rot('qT', [self.sb(es, f"qT{i}", [128, SEQ], BF16) for i in range(2)])
            wv = W[f'l{li}_w_qkv'].rearrange("(k p) n -> p k n", p=128)
            self.load_cast(wkv[:, :, 0:128], wv[:, :, 1024:1152], 1024, wres='wkv', shape3=(8, 128))
            self.load_cast(wkv[:, :, 128:256], wv[:, :, 1152:1280], 1024, wres='wkv2', shape3=(8, 128))
            S.op('pool', lambda e: e.memset(vaug[:, :, :, 0, 64:128], 1.0), [], ['vaug1'])
            S.op('pool', lambda e: e.memset(vaug[:, :, :, 1, 0:64], 1.0), [], ['vaug1'])
            for kvh in range(2):
                S.op('pool', lambda e, kvh=kvh: e.memset(kTv[64:128, kvh, 0, :], 0.0), [], ['kTz'])
                S.op('pool', lambda e, kvh=kvh: e.memset(kTv[0:64, kvh, 1, :], 0.0), [], ['kTz'])
            for c in range(8):
                pj, pres = self.pjs.next()
                cs = slice(c * 512, (c + 1) * 512)
                for k in range(8):
                    self.mm(pj[:], wkv[:, k, 0:128], xnT[:, k, cs], k == 0, k == 7, ['wkv', ('xnT', c)], [pres])
                self.act(kTv[0:64, 0, 0, cs], pj[0:64, :], AF.Copy, [pres, 'kTz'], [('kT', c)])
                self.act(kTv[64:128, 0, 1, cs], pj[0:64, :], AF.Copy, [pres, 'kTz'], [('kT', c)])
                S.op('dve', lambda e, pj=pj, cs=cs: e.tensor_copy(kTv[64:128, 1, 1, cs], pj[64:128, :]),
                     [pres, 'kTz'], [('kT', c)])
                S.op('dve', lambda e, pj=pj, cs=cs: e.tensor_copy(kTv[0:64, 1, 0, cs], pj[64:128, :]),
                     [pres, 'kTz'], [('kT', c)])
            for t in range(NT):
                pj, pres = self.pjs.next()
                for k in range(8):
                    self.mm(pj[:, 0:128], xnT[:, k, t * 128:(t + 1) * 128], wkv[:, k, 128:256], k == 0, k == 7,
                            ['wkv2', ('xnT', t // 4)], [pres])
                for kvh in range(2):
                    S.op('dve', lambda e, pj=pj, t=t, kvh=kvh: e.tensor_copy(
                        vaug[:, t, kvh, 0, 0:64], pj[:, kvh * 64:(kvh + 1) * 64]), [pres, 'vaug1'], [('vaug', t)])
                    S.op('dve', lambda e, pj=pj, t=t, kvh=kvh: e.tensor_copy(
                        vaug[:, t, kvh, 1, 64:128], pj[:, kvh * 64:(kvh + 1) * 64]), [pres, 'vaug1'], [('vaug', t)])
            def wq_load(p):
                wq, wqres = wqs.next()
                self.load_cast(wq[:], wv[:, :, p * 128:(p + 1) * 128], 1024, wres=wqres, shape3=(8, 128), engs=('pool',))
                return wq, wqres
            nextwq = wq_load(0)
            for p in range(8):
                wq, wqres = nextwq
                if p + 1 < 8:
                    nextwq = wq_load(p + 1)
                qT, qres = qTs.next()
                for c in range(8):
                    pj, pres = self.pjs.next()
                    for k in range(8):
                        self.mm(pj[:], wq[:, k, :], xnT[:, k, c * 512:(c + 1) * 512], k == 0, k == 7,
                                [wqres, ('xnT', c)], [pres])
                    self.act(qT[:, c * 512:(c + 1) * 512], pj[:], AF.Copy, [pres], [(qres, c)])
                for slot in range(2):
                    h = 2 * p + slot
                    kvh = h // 8
                    for c in range(8):
                        oa, ores = self.oas.next()
                        kts = [kt for kt in range(4 * c - 1, 4 * c + 4) if kt >= 0]
                        for si, kt in enumerate(kts):
                            qts = [qt for qt in (kt, kt + 1) if 4 * c <= qt <= 4 * c + 3]
                            col0 = (qts[0] - 4 * c) * 128
                            n = 128 * len(qts)
                            boff = 0 if qts[0] == kt else 128
                            qk = [(kTv[:, kvh, slot, kt * 128:(kt + 1) * 128], qT[:, c * 512 + col0:c * 512 + col0 + n],
                                   [('kT', kt // 4), 'kTz', (qres, c)]),
                                  (self.ident[:], bsw[:, h, 0, boff:boff + n], ['ident', 'bsw']),
                                  (self.ident[:], bsw[:, h, 1, boff:boff + n], ['ident', 'bsw'])]
                            last = si == len(kts) - 1
                            after = None
                            if last:
                                after = (lambda oa=oa, ores=ores, slot=slot, p=p, c=c, h=h: self.normalize_out(
                                    lambda rows: oa[rows, 0:512], ores, 512, slot, p, c * 512,
                                    extra=(lambda rows: esink[rows, h:h + 1], 'esink'), use_act=True))
                            self.attn_step(n, qk, 0.125, (oa[:, col0:col0 + n], vaug[:, kt, kvh, slot, :],
                                                          [('vaug', kt), 'vaug1'], ores, si == 0, last), after)
                self.attn_flush()
        S.barrier()

    def alloc_pair(self, es):
        S = self.S
        self.wqkv = Rot('wqkv', [self.sb(es, f"wqkv{i}", [128, 8, 384], BF16) for i in range(2)])
        self.qT = self.sb(es, "qTp", [128, SEQ], BF16)
        self.kTz = self.sb(es, "kTz", [128, 2, SEQ], BF16)
        self.vaug = self.sb(es, "vaugp", [128, NT, 2, 128], BF16)
        self.vTs = Rot('vT', [self.sb(es, f"vT{i}", [128, 512], BF16) for i in range(2)])
        S.op('pool', lambda e: e.memset(self.kTz[64:128, 0, :], 0.0), [], ['kTzz'])
        S.op('pool', lambda e: e.memset(self.kTz[0:64, 1, :], 0.0), [], ['kTzz'])
        S.op('pool', lambda e: e.memset(self.vaug[:, :, 0, 64:128], 1.0), [], ['vaug1'])
        S.op('pool', lambda e: e.memset(self.vaug[:, :, 1, 0:64], 1.0), [], ['vaug1'])

    def pair_load(self, wv, qc, kc, vc):
        w, wres = self.wqkv.next()
        for j, c0 in enumerate((qc, kc, vc)):
            self.load_cast(w[:, :, j * 128:(j + 1) * 128], wv[:, :, c0:c0 + 128], 1024, wres=(wres, j), shape3=(8, 128),
                           engs=('pool',))
        return w, wres

    def pair_proj(self, wl, xnT, colmap=None):
        S = self.S
        w, wres = wl
        qT, kTz, vaug = self.qT, self.kTz, self.vaug
        for c in range(8):
            cs = slice(c * 512, (c + 1) * 512)
            xres = ('xnT', c) if colmap is None else 'xnTall'
            outs = []
            for j in range(3):
                pj, pres = self.pjs.next()
                for k in range(8):
                    rhs = xnT[:, k, cs] if colmap is None else colmap(k, c)
                    o = pj[:] if len(rhs.shape) == 2 else pj[:].rearrange("p (a b) -> p a b", a=rhs.shape[1])
                    self.mm(o, w[:, k, j * 128:(j + 1) * 128], rhs, k == 0, k == 7,
                            [(wres, j)] + ([xres] if colmap is None else [('xnT', cc) for cc in range(8)]), [pres])
                if j == 0:
                    self.act(qT[:, cs], pj[:], AF.Copy, [pres], [('qT', c)])
                elif j == 1:
                    self.act(kTz[0:64, 0, cs], pj[0:64, :], AF.Copy, [pres, 'kTzz'], [('kT', c)])
                    S.op('dve', lambda e, pj=pj, cs=cs: e.tensor_copy(kTz[64:128, 1, cs], pj[64:128, :]),
                         [pres, 'kTzz'], [('kT', c)])
                else:
                    vT, vres = self.vTs.next()
                    self.act(vT[:], pj[:], AF.Copy, [pres], [vres])
                    tp, tres = self.tps.next()
                    for jj in range(4):
                        self.tr(tp[:, jj * 128:(jj + 1) * 128], vT[:, jj * 128:(jj + 1) * 128], [vres], [tres])
                    tv = tp[:, 0:512].rearrange("p (a b) -> p a b", a=4)
                    S.op('dve', lambda e, tv=tv, c=c: e.tensor_copy(vaug[:, 4 * c:4 * c + 4, 0, 0:64], tv[:, :, 0:64]),
                         [tres, 'vaug1'], [('vaug', c)])
                    S.op('dve', lambda e, tv=tv, c=c: e.tensor_copy(vaug[:, 4 * c:4 * c + 4, 1, 64:128],
                                                                  tv[:, :, 64:128]), [tres, 'vaug1'], [('vaug', c)])

    def phase_dilated(self, li, xnT):
        S = self.S
        W = self.W
        dils = (1, 4, 16)
        with contextlib.ExitStack() as es:
            self.alloc_attn_common(es)
            self.alloc_pair(es)
            bdl = self.sb(es, "bdl", [128, 24, 2, 256], BF16)
            self.dma(bdl[:], self.C['bdl'], [], ['bdl'])
            acc = [self.sb(es, f"acc{i}", [128, SEQ], F32) for i in range(2)]
            wv = W[f'l{li}_w_qkv'].rearrange("(k p) n -> p k n", p=128)
            qT, kTz, vaug = self.qT, self.kTz, self.vaug
            nextw = self.pair_load(wv, 0, 512, 1024)
            for pp in range(4):
                for g in range(3):
                    d = dils[g]
                    Sd = SEQ // d
                    nseg = Sd // 128

                    def colmap(k, c, d=d, Sd=Sd):
                        if d == 1:
                            return xnT[:, k, c * 512:(c + 1) * 512]
                        if Sd >= 512:
                            r, i0 = divmod(c * 512, Sd)
                            st = r + d * i0
                            return xnT[:, k, st:st + d * 511 + 1:d]
                        v = xnT[:, k, :].rearrange("p (j d) -> p d j", d=d)
                        nr = 512 // Sd
                        return v[:, c * nr:(c + 1) * nr, :]

                    wl = nextw
                    ni = pp * 3 + g + 1
                    if ni < 12:
                        npp, ng = divmod(ni, 3)
                        nb_ = (ng * 3) * 512
                        nextw = self.pair_load(wv, nb_ + npp * 128, nb_ + 512 + npp * 128, nb_ + 1024 + npp * 128)
                    self.pair_proj(wl, xnT, colmap=(None if d == 1 else colmap))
                    for slot in range(2):
                        hs = 2 * pp + slot
                        hb = g * 8 + hs
                        for c in range(8):
                            oa, ores = self.oas.next()
                            steps = []
                            for kt in range(4 * c - 1, 4 * c + 4):
                                if kt < 0:
                                    continue
                                qts = [kt] + ([kt + 1] if (kt + 1) % nseg != 0 else [])
                                qts = [qt for qt in qts if 4 * c <= qt <= 4 * c + 3]
                                if qts:
                                    steps.append((kt, qts))
                            A = acc[slot]
                            if d == 1:
                                def after(oa=oa, ores=ores, A=A, c=c, slot=slot):
                                    self.act(A[:, c * 512:(c + 1) * 512], oa[:], AF.Copy, [ores, ('accall', slot)],
                                             [('acc', slot, c)])
                            else:
                                if Sd >= 512:
                                    r, i0 = divmod(c * 512, Sd)
                                    st = r + d * i0
                                    pieces = [(A[:, st:st + d * 511 + 1:d], oa[:, 0:512])]
                                else:
                                    nr = 512 // Sd
                                    pieces = []
                                    for rr in range(nr):
                                        r = c * nr + rr
                                        pieces.append((A[:, r:r + d * (Sd - 1) + 1:d], oa[:, rr * Sd:(rr + 1) * Sd]))

                                def after(pieces=pieces, ores=ores, slot=slot):
                                    for (av, ov) in pieces:
                                        S.op('dve', lambda e, av=av, ov=ov: e.tensor_tensor(av, ov, av, ALU.add),
                                             [ores] + [('acc', slot, cc) for cc in range(8)], [('accall', slot)])
                            for si, (kt, qts) in enumerate(steps):
                                col0 = (qts[0] - 4 * c) * 128
                                n = 128 * len(qts)
                                boff = 0 if qts[0] == kt else 128
                                qk = [(kTz[:, slot, kt * 128:(kt + 1) * 128], qT[:, c * 512 + col0:c * 512 + col0 + n],
                                       [('kT', kt // 4), 'kTzz', ('qT', c)]),
                                      (self.ident[:], bdl[:, hb, 0, boff:boff + n], ['ident', 'bdl']),
                                      (self.ident[:], bdl[:, hb, 1, boff:boff + n], ['ident', 'bdl'])]
                                last = si == len(steps) - 1
                                self.attn_step(n, qk, 0.125, (oa[:, col0:col0 + n], vaug[:, kt, slot, :],
                                                              [('vaug', kt // 4), 'vaug1'], ores, si == 0, last),
                                               after if last else None)
                    self.attn_flush()
                for slot in range(2):
                    A = acc[slot]
                    for c in range(8):
                        self.normalize_out(lambda rows, A=A, c=c: A[rows, c * 512:(c + 1) * 512], ('accall', slot),
                                           512, slot, pp, c * 512, use_act=True)
        S.barrier()

    def phase_moba(self, li, xnT):
        S = self.S
        W = self.W
        C = self.C
        with contextlib.ExitStack() as es:
            self.alloc_attn_common(es, nslots=3)
            self.wqkv = Rot('wqkv', [self.sb(es, f"wqkv{i}", [128, 8, 384], BF16) for i in range(2)])
            qY = self.sb(es, "qY", [128, 2, SEQ], BF16)
            kX = self.sb(es, "kX", [128, 2, SEQ], BF16)
            vaug = self.sb(es, "vaugp", [128, NT, 2, 128], BF16)
            self.vTs = Rot('vT', [self.sb(es, f"vT{i}", [128, 512], BF16) for i in range(2)])
            for sl in range(2):
                S.op('pool', lambda e, sl=sl: e.memset(qY[64:128, sl, :], 0.0), [], [('qYz', sl)])
                self.dma(kX[64:128, sl, :], C['xselT'], [], ['xselT'])
            S.op('pool', lambda e: e.memset(vaug[:, :, 0, 64:128], 1.0), [], ['vaug1'])
            S.op('pool', lambda e: e.memset(vaug[:, :, 1, 0:64], 1.0), [], ['vaug1'])
            qZs = Rot('qZ', [self.sb(es, f"qZ{i}", [128, 256], BF16) for i in range(3)])
            for i in range(3):
                S.op('pool', lambda e, i=i: e.memset(qZs.tiles[i][64:128, :], 0.0), [], [('qZ', i)])
            bmo = self.sb(es, "bmo", [128, 16, 2, 256], BF16)
            self.dma(bmo[:], C['bmo'], [], ['bmo'])
            tqm = self.sb(es, "tqm", [128, 2, 16], F32)
            self.dma(tqm[:], C['tqm'], [], ['tqm'])
            dtab = self.sb(es, "dtab", [128, 16, 31], F32)
            self.dma(dtab[:], C['dtab'], [], ['dtab'])
            slp = self.sb(es, "slp", [128, 16, 2], BF16)
            self.dma(slp[:], C['slp'], [], ['slp'])
            c30 = self.sb(es, "c30", [128, 16], F32)
            S.op('dve', lambda e: e.memset(c30[:], 30000.0), [], ['c30'])
            kmf = self.sb(es, "kmf", [64, 16], F32)
            kmT = self.sb(es, "kmT", [128, 2, 16], BF16)
            S.op('pool', lambda e: e.memset(kmT[:], 0.0), [], [('kmT', 0), ('kmT', 1)])
            gss = Rot('gs', [self.sb(es, f"gs{i}", [128, 16], F32) for i in range(4)])
            t8s = Rot('t8', [self.sb(es, f"t8{i}", [128, 8], F32) for i in range(4)])
            s3s = Rot('s3', [self.sb(es, f"s3{i}", [128, 16], F32) for i in range(4)])
            vvs = Rot('vv', [self.sb(es, f"vv{i}", [128, 16], F32) for i in range(4)])
            yps = Rot('yp', [self.sb(es, f"yp{i}", [128, 128], BF16) for i in range(6)])
            for i in range(6):
                S.op('pool', lambda e, i=i: e.memset(yps.tiles[i][:], 0.0), [], [('yp', i)])
            gp, gpres = self.pjs.tiles[1], ('pj', 1)
            wv = W[f'l{li}_w_qkv'].rearrange("(k p) n -> p k n", p=128)
            nextw = self.pair_load(wv, 0, 1024, 2048)
            for p in range(8):
                w, wres = nextw
                if p + 1 < 8:
                    nextw = self.pair_load(wv, (p + 1) * 128, 1024 + (p + 1) * 128, 2048 + (p + 1) * 128)
                for c in range(8):
                    cs = slice(c * 512, (c + 1) * 512)
                    for j in range(3):
                        pj, pres = self.pjs.next()
                        for k in range(8):
                            self.mm(pj[:], w[:, k, j * 128:(j + 1) * 128], xnT[:, k, cs], k == 0, k == 7,
                                    [(wres, j), ('xnT', c)], [pres])
                        if j < 2:
                            dst = qY if j == 0 else kX
                            nm = 'qT' if j == 0 else 'kT'
                            self.act(dst[0:64, 0, cs], pj[0:64, :], AF.Copy, [pres], [(nm, 0, c)])
                            S.op('dve', lambda e, pj=pj, cs=cs, dst=dst: e.tensor_copy(dst[0:64, 1, cs], pj[64:128, :]),
                                 [pres], [(nm, 1, c)])
                        else:
                            vT, vres = self.vTs.next()
                            self.act(vT[:], pj[:], AF.Copy, [pres], [vres])
                            tp, tres = self.tps.next()
                            for jj in range(4):
                                self.tr(tp[:, jj * 128:(jj + 1) * 128], vT[:, jj * 128:(jj + 1) * 128], [vres], [tres])
                            tv = tp[:, 0:512].rearrange("p (a b) -> p a b", a=4)
                            S.op('dve', lambda e, tv=tv, c=c: e.tensor_copy(vaug[:, 4 * c:4 * c + 4, 0, 0:64],
                                                                          tv[:, :, 0:64]), [tres, 'vaug1'], [('vaug', c)])
                            S.op('dve', lambda e, tv=tv, c=c: e.tensor_copy(vaug[:, 4 * c:4 * c + 4, 1, 64:128],
                                                                          tv[:, :, 64:128]), [tres, 'vaug1'],
                                 [('vaug', c)])
                for slot in range(2):
                    h = 2 * p + slot
                    allk = [('kT', slot, c) for c in range(8)]
                    S.op('dve', lambda e, slot=slot: e.tensor_reduce(
                        out=kmf[:], in_=kX[0:64, slot, :].rearrange("p (n k) -> p n k", k=256), axis=AX.X, op=ALU.add),
                        allk, ['kmf'])
                    S.op('dve', lambda e, slot=slot: e.tensor_scalar(kmT[0:64, slot, :], kmf[:], 1.0 / 256, None, ALU.mult),
                         ['kmf'], [('kmT', slot)])
                    ypd = {}

                    def prepA(b, slot=slot, h=h, ypd=ypd):
                        for j in range(2):
                            tcols = slice(b * 256 + j * 128, b * 256 + (j + 1) * 128)
                            if b > 3:
                                self.mm(gp[:, 0:16], qY[:, slot, tcols], kmT[:, slot, :], True, True,
                                        [('qT', slot, b // 2), ('qYz', slot), ('kmT', slot)], [gpres])
                                gs, gres = gss.next()
                                S.op('dve', lambda e, gs=gs: e.tensor_copy(gs[:], gp[:, 0:16]), [gpres], [gres])
                                S.op('dve', lambda e, gs=gs, b=b: e.memset(gs[:, b:16], -1e30), [], [gres])
                                t8, t8res = t8s.next()
                                S.op('dve', lambda e, gs=gs, t8=t8: e.max(t8[:], gs[:]), [gres], [t8res])
                                s3, s3res = s3s.next()
                                S.op('dve', lambda e, gs=gs, t8=t8, s3=s3: e.tensor_scalar(
                                    s3[:], gs[:], t8[:, 2:3], 30000.0, ALU.is_ge, ALU.mult), [gres, t8res], [s3res])
                            else:
                                s3, s3res = c30, 'c30'
                            vv, vres = vvs.next()
                            S.op('dve', lambda e, s3=s3, vv=vv, j=j: e.scalar_tensor_tensor(
                                out=vv[:], in0=s3[:], scalar=tqm[:, j, h:h + 1], in1=dtab[:, h, 15 - b:31 - b],
                                op0=ALU.add, op1=ALU.add), [s3res, 'tqm', 'dtab'], [vres])
                            yp, ypres = yps.next()
                            S.op('dve', lambda e, yp=yp, vv=vv: e.tensor_copy(yp[:, 0:16], vv[:]), [vres], [ypres])
                            S.op('dve', lambda e, yp=yp, vv=vv: e.tensor_tensor(yp[:, 16:32], vv[:], yp[:, 0:16],
                                                                              ALU.subtract), [vres, ypres], [ypres])
                            S.op('dve', lambda e, yp=yp: e.tensor_copy(yp[:, 32:34], slp[:, h, :]), ['slp'], [ypres])
                            ypd[(b, j)] = (yp, ypres)

                    def prepB(b, slot=slot, ypd=ypd):
                        for j in range(2):
                            yp, ypres = ypd[(b, j)]
                            tp, tres = self.tps.next()
                            self.tr(tp[:, 0:128], yp[:], [ypres], [tres])
                            cols = slice(b * 256 + j * 128, b * 256 + (j + 1) * 128)
                            S.op('dve', lambda e, tp=tp, cols=cols: e.tensor_copy(qY[64:128, slot, cols], tp[0:64, 0:128]),
                                 [tres, ('qYz', slot)], [('qY', slot, b)])

                    def steps(b, slot=slot, h=h, p=p):
                        oa, ores = self.oas.next()
                        qcols = slice(b * 256, (b + 1) * 256)
                        qz, qzres = qZs.next()
                        S.op('pool', lambda e, qz=qz: e.tensor_copy(qz[0:64, :], qY[0:64, slot, qcols]),
                             [('qT', slot, b // 2)], [qzres])
                        first = True
                        for n in range(b):
                            for half in range(2):
                                kt = 2 * n + half
                                qk = [(kX[:, slot, kt * 128:(kt + 1) * 128], qY[:, slot, qcols],
                                       [('kT', slot, kt // 4), 'xselT', ('qT', slot, b // 2), ('qY', slot, b)])]
                                self.attn_step(256, qk, 0.125, (oa[:, 0:256], vaug[:, kt, slot, :],
                                                                [('vaug', kt // 4), 'vaug1'], ores, first, False))
                                first = False
                        for half in range(2):
                            kt = 2 * b + half
                            n = 256 - 128 * half
                            qk = [(kX[:, slot, kt * 128:(kt + 1) * 128], qz[:, 128 * half:256],
                                   [('kT', slot, kt // 4), 'xselT', qzres]),
                                  (self.ident[:], bmo[:, h, 0, 0:n], ['ident', 'bmo']),
                                  (self.ident[:], bmo[:, h, 1, 0:n], ['ident', 'bmo'])]
                            after = None
                            if half == 1:
                                after = (lambda oa=oa, ores=ores: self.normalize_out(
                                    lambda rows: oa[rows, 0:256], ores, 256, slot, p, b * 256))
                            self.attn_step(n, qk, 0.125, (oa[:, 128 * half:256], vaug[:, kt, slot, :],
                                                          [('vaug', kt // 4), 'vaug1'], ores, first, half == 1), after)
                            first = False

                    prepA(1)
                    prepA(2)
                    prepB(1)
                    for b in range(16):
                        if b >= 1 and b + 1 <= 15:
                            prepB(b + 1)
                        if b >= 1 and b + 2 <= 15:
                            prepA(b + 2)
                        steps(b)
                    self.attn_flush()
        S.barrier()

    def phase_mla(self, li, xnT):
        S = self.S
        W = self.W
        C = self.C
        with contextlib.ExitStack() as es:
            ckvT = self.sb(es, "ckvT", [128, 2, SEQ], BF16)
            krT = self.sb(es, "krT", [32, SEQ], BF16)
            wukv = self.sb(es, "wukv", [128, 2, 2048], BF16)
            mtri = self.sb(es, "mtri", [128, 512], BF16)
            self.dma(mtri[:], C['mtri'], [], ['mtri'])
            A, Ares = self.pjs.tiles[0], ('pj', 0)
            B, Bres = self.pjs.tiles[1], ('pj', 1)
            Cb, Cres = self.scs.tiles[0], ('sc', 0)
            Q = [(self.oas.tiles[0], ('oa', 0)), (self.oas.tiles[1], ('oa', 1)), (self.scs.tiles[1], ('sc', 1))]
            tpA, tAres = self.tps.tiles[0], ('tp', 0)
            tpB, tBres = self.tps.tiles[1], ('tp', 1)
            with contextlib.ExitStack() as es1:
                self.stg = Rot('stg', [self.sb(es1, f"stgm{i}", [128, 1024], F32) for i in range(2)])
                wdkv = self.sb(es1, "wdkv", [128, 8, 1056], BF16)
                wuq = self.sb(es1, "wuq", [128, 6, 1536], BF16)
                wd = W[f'l{li}_w_dkv'].rearrange("(k p) n -> p k n", p=128)
                for k in range(8):
                    self.load_cast(wdkv[:, k, 0:1024], wd[:, k, 0:1024], 1024, wres=('wdkv', k))
                if not DBG.get('skip_wdkvr'):
                    self.load_cast(wdkv[:, :, 1024:1056], wd[:, :, 1024:1056], 256, wres='wdkvr', shape3=(8, 32))
                wq = W[f'l{li}_w_uq'].rearrange("(k p) n -> p k n", p=128)
                for k in range(6):
                    self.load_cast(wuq[:, k, 0:1024], wq[:, k, 0:1024], 1024, wres=('wuq', k))
                    self.load_cast(wuq[:, k, 1024:1536], wq[:, k, 1024:1536], 512, wres=('wuq2', k))
                wk = W[f'l{li}_w_ukv'].rearrange("(k p) n -> p k n", p=128)
                for k in range(2):
                    for hf in range(2):
                        self.load_cast(wukv[:, k, hf * 1024:(hf + 1) * 1024], wk[:, k, hf * 1024:(hf + 1) * 1024], 1024,
                                       wres=('wukv', k, hf))
                qg = self.sb(es1, "qg", [128, 768], F32)
                kvg = self.sb(es1, "kvg", [128, 256], F32)
                if not DBG.get('skip_g'):
                    self.dma(qg[:], W[f'l{li}_q_norm'].partition_broadcast(128), [], ['qg'])
                    self.dma(kvg[:], W[f'l{li}_kv_norm'].partition_broadcast(128), [], ['kvg'])
                cs = self.sb(es1, "ropecs", [128, 32, 32], F32)
                sn = self.sb(es1, "ropesn", [128, 32, 32], F32)
                if not DBG.get('skip_rope'):
                    self.dma(cs[:], C['ropecs'], [], ['ropecs'])
                    self.dma(sn[:], C['ropesn'], [], ['ropesn'])
                cqns = Rot('cqn', [self.sb(es1, f"cqn{i}", [128, 768], BF16) for i in range(2)])
                ckvns = Rot('ckvn', [self.sb(es1, f"ckvn{i}", [128, 256], BF16) for i in range(2)])
                krrs = Rot('krr', [self.sb(es1, f"krr{i}", [128, 128], BF16) for i in range(2)])
                for i in range(2):
                    S.op('pool', lambda e, i=i: e.memset(krrs.tiles[i][:], 0.0), [], [('krr', i)])
                ra = self.sb(es1, "ra", [128, 32], F32)
                rb = self.sb(es1, "rb", [128, 32], F32)
                cqTs = Rot('cqT', [self.sb(es1, f"cqT{i}", [128, 6, 128], BF16) for i in range(2)])
                qf = self.sb(es1, "qf", [128, 1536], F32)
                qb = self.sb(es1, "qb", [128, 1536], BF16)
                ta = self.sb(es1, "ta", [128, 16, 32], F32)
                tb = self.sb(es1, "tb", [128, 16, 32], F32)
                qst = self.sb(es1, "qst", [96, 16, 512], BF16)
                ssa = self.sb(es1, "ssa", [128, NT], F32)
                ssb = self.sb(es1, "ssb", [128, NT], F32)
                ssk = self.sb(es1, "ssk", [128, NT], F32)
                rsq = self.sb(es1, "rsq", [128, NT], F32)
                rsk = self.sb(es1, "rsk", [128, NT], F32)
                def stageX(t):
                    ts = slice(t * 128, (t + 1) * 128)
                    t1 = slice(t, t + 1)
                    for (dst, dres, c0, c1) in ((A, Ares, 0, 512), (B, Bres, 512, 768), (Cb, Cres, 768, 1056)):
                        for k in range(8):
                            self.mm(dst[:, 0:c1 - c0], xnT[:, k, ts], wdkv[:, k, c0:c1], k == 0, k == 7,
                                    [('xnT', t // 4), ('wdkv', k), 'wdkvr'], [dres])
                    junk, jres = self.junk.next()
                    self.act(junk[:, 0:512], A[:], AF.Square, [Ares], [jres, ('ssa', t)], accum=ssa[:, t1])
                    junk, jres = self.junk.next()
                    self.act(junk[:, 0:256], B[:, 0:256], AF.Square, [Bres], [jres, ('ssb', t)], accum=ssb[:, t1])
                    junk, jres = self.junk.next()
                    self.act(junk[:, 0:256], Cb[:, 0:256], AF.Square, [Cres], [jres, ('ssk', t)], accum=ssk[:, t1])
                    S.op('dve', lambda e, t1=t1: e.tensor_tensor(ssa[:, t1], ssa[:, t1], ssb[:, t1], ALU.add),
                         [('ssa', t), ('ssb', t)], [('ssa', t)])
                    self.rms_rstd(ssa[:, t1], rsq[:, t1], ('ssa', t), ('rsq', t), 768)
                    self.rms_rstd(ssk[:, t1], rsk[:, t1], ('ssk', t), ('rsk', t), 256)
                    cqn, cqres = cqns.next()
                    ckvn, ckres = ckvns.next()
                    S.op('dve', lambda e, cqn=cqn, t1=t1: e.scalar_tensor_tensor(
                        out=cqn[:, 0:512], in0=A[:], scalar=rsq[:, t1], in1=qg[:, 0:512], op0=ALU.mult, op1=ALU.mult),
                        [Ares, ('rsq', t), 'qg'], [cqres])
                    S.op('dve', lambda e, cqn=cqn, t1=t1: e.scalar_tensor_tensor(
                        out=cqn[:, 512:768], in0=B[:, 0:256], scalar=rsq[:, t1], in1=qg[:, 512:768],
                        op0=ALU.mult, op1=ALU.mult), [Bres, ('rsq', t), 'qg'], [cqres])
                    S.op('dve', lambda e, ckvn=ckvn, t1=t1: e.scalar_tensor_tensor(
                        out=ckvn[:], in0=Cb[:, 0:256], scalar=rsk[:, t1], in1=kvg[:], op0=ALU.mult, op1=ALU.mult),
                        [Cres, ('rsk', t), 'kvg'], [ckres])
                    krr, krres = krrs.next()
                    S.op('dve', lambda e, t=t: e.tensor_tensor(ra[:], Cb[:, 256:288], cs[:, t, :], ALU.mult),
                         [Cres, 'ropecs', ('ssk', t)], ['ra'])
                    S.op('dve', lambda e, t=t: e.tensor_tensor(rb[:, 0:16], Cb[:, 272:288], sn[:, t, 0:16], ALU.mult),
                         [Cres, 'ropesn', ('ssk', t)], ['rb'])
                    S.op('dve', lambda e, t=t: e.tensor_tensor(rb[:, 16:32], Cb[:, 256:272], sn[:, t, 16:32], ALU.mult),
                         [Cres, 'ropesn', ('ssk', t)], ['rb'])
                    S.op('dve', lambda e, krr=krr: e.tensor_tensor(krr[:, 0:32], ra[:], rb[:], ALU.add),
                         ['ra', 'rb'], [krres])
                    m5 = 7
                    cqT, cqTres = cqTs.next()
                    if m5 & 1:
                        for k in range(6):
                            self.tr(tpA[:, k * 128:(k + 1) * 128], cqn[:, k * 128:(k + 1) * 128], [cqres], [tAres])
                    if m5 & 2:
                        for k in range(2):
                            self.tr(tpA[:, 768 + k * 128:768 + (k + 1) * 128], ckvn[:, k * 128:(k + 1) * 128], [ckres],
                                    [tAres])
                    if m5 & 4:
                        self.tr(tpB[:, 0:128], krr[:], [krres], [tBres])
                    if m5 & 1:
                        self.act(cqT[:], tpA[:, 0:768].rearrange("p (k n) -> p k n", k=6), AF.Copy, [tAres], [cqTres])
                    if m5 & 2:
                        self.act(ckvT[:, :, ts], tpA[:, 768:1024].rearrange("p (k n) -> p k n", k=2), AF.Copy,
                                 [tAres], [('ckvT', t // 4)])
                    if m5 & 4:
                        S.op('dve', lambda e, ts=ts: e.tensor_copy(krT[0:32, ts], tpB[0:32, 0:128]), [tBres],
                             [('krT', t // 4)])
                    return cqT, cqTres

                def stageY(t, cqT, cqTres):
                    ts = slice(t * 128, (t + 1) * 128)
                    for j in range(3):
                        Qj, Qres = Q[j]
                        for k in range(6):
                            self.mm(Qj[:], cqT[:, k, :], wuq[:, k, j * 512:(j + 1) * 512], k == 0, k == 5,
                                    [cqTres, ('wuq', k), ('wuq2', k)], [Qres])
                        self.act(qf[:, j * 512:(j + 1) * 512], Qj[:], AF.Copy, [Qres], ['qf'])
                    qv = qf[:].rearrange("p (h d) -> p h d", h=16)
                    qbv = qb[:].rearrange("p (h d) -> p h d", h=16)
                    csb = cs[:, t:t + 1, :].broadcast_to([128, 16, 32])
                    snb = sn[:, t:t + 1, :].broadcast_to([128, 16, 32])
                    S.op('dve', lambda e, qv=qv, csb=csb: e.tensor_tensor(ta[:], qv[:, :, 64:96], csb, ALU.mult),
                         ['qf', 'ropecs'], ['ta'])
                    S.op('dve', lambda e, qv=qv, snb=snb: e.tensor_tensor(tb[:, :, 0:16], qv[:, :, 80:96],
                                                                      snb[:, :, 0:16], ALU.mult),
                         ['qf', 'ropesn'], ['tb'])
                    S.op('dve', lambda e, qv=qv, snb=snb: e.tensor_tensor(tb[:, :, 16:32], qv[:, :, 64:80],
                                                                      snb[:, :, 16:32], ALU.mult),
                         ['qf', 'ropesn'], ['tb'])
                    S.op('pool', lambda e, qv=qv, qbv=qbv: e.tensor_copy(qbv[:, :, 0:64], qv[:, :, 0:64]), ['qf'], ['qb'])
                    S.op('dve', lambda e, qbv=qbv: e.tensor_tensor(qbv[:, :, 64:96], ta[:], tb[:], ALU.add),
                         ['ta', 'tb'], ['qb'])
                    for hh in range(16):
                        tp_, tr_ = (tpA, tAres) if hh < 8 else (tpB, tBres)
                        self.tr(tp_[0:96, (hh % 8) * 128:(hh % 8 + 1) * 128], qb[:, hh * 96:(hh + 1) * 96], ['qb'], [tr_])
                    tsub = t % 4
                    self.act(qst[:, 0:8, tsub * 128:(tsub + 1) * 128], tpA[0:96, :].rearrange("p (h n) -> p h n", h=8),
                             AF.Copy, [tAres], ['qst'])
                    S.op('dve', lambda e, tsub=tsub: e.tensor_copy(qst[:, 8:16, tsub * 128:(tsub + 1) * 128],
                                                                 tpB[0:96, :].rearrange("p (h n) -> p h n", h=8)),
                         [tBres], ['qst'])
                    if tsub == 3:
                        g = t // 4
                        self.dma(self.qTs[:, :, g * 512:(g + 1) * 512].rearrange("h r n -> r h n"), qst[:], ['qst'],
                                 [('qTs', g)])

                nt = DBG.get('mla_nt', NT)
                cur = stageX(0)
                for t in range(nt):
                    nxt = stageX(t + 1) if t + 1 < nt else None
                    stageY(t, *cur)
                    cur = nxt
            S.barrier()
            with contextlib.ExitStack() as es2:
                self.alloc_attn_common(es2)
                if DBG.get('mla_stage1_only'):
                    return
                qThs = Rot('qTh', [self.sb(es2, f"qTh{i}", [96, SEQ], BF16) for i in range(2)])
                kThs = Rot('kTh', [self.sb(es2, f"kTh{i}", [96, SEQ], BF16) for i in range(2)])
                vaugs = [self.sb(es2, f"vaugm{i}", [128, NT, 128], BF16) for i in range(2)]
                S.op('pool', lambda e: e.memset(vaugs[0][:, :, 64:128], 1.0), [], [('vaugm1', 0)])
                S.op('pool', lambda e: e.memset(vaugs[1][:, :, 0:64], 1.0), [], [('vaugm1', 1)])
                vTz = [Rot(f'vTz{sl}', [self.sb(es2, f"vTz{sl}_{i}", [128, 512], BF16) for i in range(2)])
                       for sl in range(2)]
                for sl in range(2):
                    for i in range(2):
                        zr = slice(64, 128) if sl == 0 else slice(0, 64)
                        S.op('pool', lambda e, sl=sl, i=i, zr=zr: e.memset(vTz[sl].tiles[i][zr, :], 0.0), [],
                             [(f'vTz{sl}z', i)])
                scale = 96.0 ** -0.5
                for h in range(16):
                    slot = h % 2
                    qTh, qres = qThs.next()
                    self.dma(qTh[:], self.qTs[h], [('qTs', g) for g in range(8)], [qres])
                    kTh, kres = kThs.next()
                    va = vaugs[slot]
                    S.op('pool', lambda e, kTh=kTh: e.tensor_copy(kTh[64:96, :], krT[0:32, :]),
                         [('krT', g) for g in range(8)], [(kres, 'r')])
                    for c in range(8):
                        cs_ = slice(c * 512, (c + 1) * 512)
                        pj, pres = self.pjs.next()
                        for k in range(2):
                            self.mm(pj[:], wukv[:, k, h * 128:(h + 1) * 128], ckvT[:, k, cs_], k == 0, k == 1,
                                    [('wukv', k, h // 8), ('ckvT', c)], [pres])
                        self.act(kTh[0:64, cs_], pj[0:64, :], AF.Copy, [pres], [(kres, c)])
                        vt, vtres = vTz[slot].next()
                        zres = (f'vTz{slot}z', vtres[1])
                        if slot == 0:
                            S.op('dve', lambda e, vt=vt, pj=pj: e.tensor_copy(vt[0:64, :], pj[64:128, :]),
                                 [pres], [vtres])
                        else:
                            S.op('dve', lambda e, vt=vt, pj=pj: e.tensor_copy(vt[64:128, :], pj[64:128, :]),
                                 [pres], [vtres])
                        tp, tres = self.tps.next()
                        for jj in range(4):
                            self.tr(tp[:, jj * 128:(jj + 1) * 128], vt[:, jj * 128:(jj + 1) * 128], [vtres, zres], [tres])
                        tv = tp[:, 0:512].rearrange("p (a b) -> p a b", a=4)
                        vs = slice(0, 64) if slot == 0 else slice(64, 128)
                        S.op('dve', lambda e, va=va, tv=tv, c=c, vs=vs: e.tensor_copy(va[:, 4 * c:4 * c + 4, vs],
                                                                                  tv[:, :, vs]),
                             [tres, ('vaugm1', slot)], [('vaugm', slot, c)])
                    for c in range(8):
                        oa, ores = self.oas.next()
                        nk = 4 * c + 4
                        for kt in range(nk):
                            ks = slice(kt * 128, (kt + 1) * 128)
                            rd = [(kres, kt // 4), (kres, 'r'), qres]
                            if kt < 4 * c:
                                col0, n = 0, 512
                                qk = [(kTh[:, ks], qTh[:, c * 512:(c + 1) * 512], rd)]
                            else:
                                col0 = 128 * (kt - 4 * c)
                                n = 512 - col0
                                qk = [(kTh[:, ks], qTh[:, c * 512 + col0:(c + 1) * 512], rd),
                                      (self.ident[:], mtri[:, 0:n], ['ident', 'mtri'])]
                            after = None
                            if kt == nk - 1:
                                after = (lambda oa=oa, ores=ores, slot=slot, h=h, c=c: self.normalize_out(
                                    lambda rows: oa[rows, 0:512], ores, 512, slot, h // 2, c * 512))
                            self.attn_step(n, qk, scale, (oa[:, col0:512], va[:, kt, :],
                                                          [('vaugm', slot, kt // 4), ('vaugm1', slot)], ores,
                                                          kt == 0, kt == nk - 1), after)
                    self.attn_flush()
        S.barrier()

    def build(self):
        nc = self.nc
        S = self.S
        with contextlib.ExitStack() as es:
            self.ident = self.sb(es, "ident", [128, 128], BF16)
            self.dma(self.ident[:], self.C['ident'], [], ['ident'])
            self.epsc = self.sb(es, "epsc", [128, 1], F32)
            S.op('dve', lambda e: e.memset(self.epsc[:], EPS), [], ['epsc'])
            self.junk = Rot('junk', [self.sb(es, f"junk{i}", [128, DM], BF16) for i in range(2)])
            ps = lambda name, shape, dt: es.enter_context(nc.psum_tensor(name, shape, dt))
            self.tps = Rot('tp', [ps(f"tp{i}", [128, 1024], BF16) for i in range(2)])
            self.pjs = Rot('pj', [ps(f"pj{i}", [128, 512], F32) for i in range(2)])
            self.scs = Rot('sc', [ps(f"sc{i}", [128, 512], F32) for i in range(2)])
            self.oas = Rot('oa', [ps(f"oa{i}", [128, 512], F32) for i in range(2)])
            hsrc = self.x
            for n, li in enumerate(self.layers):
                last = (n == len(self.layers) - 1)
                with contextlib.ExitStack() as es2:
                    xnT = self.sb(es2, "xnT", [128, 8, SEQ], BF16)
                    with contextlib.ExitStack() as es3:
                        self.phase_norm(es3, hsrc, f'l{li}_attn_norm', xnT)
                    S.barrier()
                    npair = 8
                    if li == 3:
                        self.phase_swa(li, xnT)
                    elif li == 1:
                        npair = 4
                        self.phase_dilated(li, xnT)
                    elif li == 0:
                        self.phase_moba(li, xnT)
                    elif li == 2:
                        self.phase_mla(li, xnT)
                    else:
                        raise NotImplementedError
                self.phase_of(li, npair, hsrc, last)
                hsrc = self.hbuf
            S.barrier()
            S.emit()
        return nc


_CONSTS = None


def run(inputs, layers=(0, 1, 2, 3), final=True, cores=8, x_override=None):
    global _CONSTS
    if _CONSTS is None:
        _CONSTS = host_consts()
    b = Builder(layers, final)
    nc = b.build()
    x = np.ascontiguousarray(inputs['x'], dtype=np.float32) if x_override is None else x_override
    in_maps = []
    for c in range(cores):
        m = {'x': np.ascontiguousarray(x[c])}
        for name in b.used_inputs():
            m[name] = np.ascontiguousarray(inputs[name], dtype=np.float32)
        for name in CONST_SPECS:
            m['c_' + name] = _CONSTS[name]
        in_maps.append(m)
    res = run_bass_kernel_spmd(nc, in_maps, core_ids=list(range(cores)))
    return np.stack([np.asarray(r['y']) for r in res.results], axis=0)


def kernel(**inputs):
    out = run(inputs)
    return out.astype(np.float32)
```

```python
import contextlib
import numpy as np
import ml_dtypes
import concourse.bass as bass
import concourse.mybir as mybir
from concourse.bass_utils import run_bass_kernel_spmd

F32 = mybir.dt.float32
BF16 = mybir.dt.bfloat16
AF = mybir.ActivationFunctionType
ALU = mybir.AluOpType
AX = mybir.AxisListType
NPBF = ml_dtypes.bfloat16

SEQ = 4096
DM = 1024
NT = SEQ // 128
DFF = 4096
EPS = 1e-6
NEG = -30000.0

ENGS = ['pe', 'act', 'dve', 'pool', 'sp']
NDMASEM = 8
DBG = {}


class Op:
    __slots__ = ('fn', 'waits', 'key', 'idx', 'signal', 'isdma')

    def __init__(self, fn, key, idx, isdma):
        self.fn = fn
        self.waits = []
        self.key = key
        self.idx = idx
        self.signal = False
        self.isdma = isdma


class Sched:
    def __init__(self, nc):
        self.nc = nc
        self.streams = {e: [] for e in ENGS}
        self.keyops = {}
        self.seen = {e: {} for e in ENGS}
        self.res = {}
        self.dma_rr = {e: 0 for e in ENGS}
        self.alias = {}

    def _expand(self, lst):
        if not self.alias:
            return lst
        out = []
        for r in lst:
            out.extend(self.alias.get(r, (r,)))
        return out

    def _need(self, eng, op, tok):
        key, idx = tok
        if self.seen[eng].get(key, -1) >= idx:
            return
        self.seen[eng][key] = idx
        op.waits.append(tok)

    def _deps(self, eng, op, reads, writes, mykey, same_ok):
        for r in reads:
            st = self.res.get(r)
            if st is None:
                continue
            w = st[0]
            if w is not None and not (same_ok and w[0] == mykey):
                self._need(eng, op, w)
        for r in writes:
            st = self.res.get(r)
            if st is None:
                continue
            w = st[0]
            if w is not None and w[0] != mykey:
                self._need(eng, op, w)
            for k, i in st[1].items():
                if k != mykey:
                    self._need(eng, op, (k, i))

    def _commit(self, tok, reads, writes):
        for r in reads:
            st = self.res.get(r)
            if st is None:
                st = self.res[r] = [None, {}]
            st[1][tok[0]] = tok[1]
        for r in writes:
            self.res[r] = [tok, {}]

    def op(self, eng, fn, reads=(), writes=()):
        reads = self._expand(reads)
        writes = self._expand(writes)
        key = eng
        lst = self.keyops.setdefault(key, [])
        o = Op(fn, key, len(lst), False)
        self._deps(eng, o, reads, writes, key, same_ok=(eng == 'pe'))
        lst.append(o)
        self.streams[eng].append(o)
        self._commit((key, o.idx), reads, writes)
        return o

    def dma(self, q, fn, reads=(), writes=()):
        reads = self._expand(reads)
        writes = self._expand(writes)
        j = self.dma_rr[q]
        self.dma_rr[q] = (j + 1) % NDMASEM
        key = ('dma', q, j)
        lst = self.keyops.setdefault(key, [])
        o = Op(fn, key, len(lst), True)
        if lst:
            self._need(q, o, (key, len(lst) - 1))
        self._deps(q, o, reads, writes, key, same_ok=False)
        lst.append(o)
        self.streams[q].append(o)
        self._commit((key, o.idx), reads, writes)
        return o

    def barrier(self):
        toks = [(key, len(lst) - 1) for key, lst in self.keyops.items() if lst]
        for e in ENGS:
            o = Op(None, None, None, False)
            for t in toks:
                self._need(e, o, t)
            if o.waits:
                self.streams[e].append(o)

    def emit(self):
        nc = self.nc
        for e in ENGS:
            for o in self.streams[e]:
                for (key, idx) in o.waits:
                    self.keyops[key][idx].signal = True
        semval = {}
        for key, lst in self.keyops.items():
            c = 0
            for o in lst:
                if o.isdma:
                    c += 16
                    semval[(key, o.idx)] = c
                    o.signal = True
                elif o.signal:
                    c += 1
                    semval[(key, o.idx)] = c
            assert c < 60000, (key, c)
        keys = [k for k, l in self.keyops.items() if l]
        with contextlib.ExitStack() as es:
            sems = {}
            for k in keys:
                nm = 's_' + ('_'.join(str(x) for x in k) if isinstance(k, tuple) else k)
                sems[k] = es.enter_context(nc.semaphore(nm))
            block = es.enter_context(nc.Block())

            def run(e):
                def body(eng):
                    for o in self.streams[e]:
                        for tok in o.waits:
                            eng.wait_ge(sems[tok[0]], semval[tok])
                        if o.fn is None:
                            continue
                        ins = o.fn(eng)
                        if o.signal:
                            ins.then_inc(sems[o.key], 16 if o.isdma else 1)
                return body
            if self.streams['pe']:
                block.tensor(run('pe'))
            if self.streams['act']:
                block.scalar(run('act'))
            if self.streams['dve']:
                block.vector(run('dve'))
            if self.streams['pool']:
                block.gpsimd(run('pool'))
            if self.streams['sp']:
                block.sync(run('sp'))


class Rot:
    def __init__(self, name, tiles):
        self.name = name
        self.tiles = tiles
        self.i = 0

    def next(self):
        j = self.i % len(self.tiles)
        self.i += 1
        return self.tiles[j], (self.name, j)


def alibi(n):
    return (2.0 ** (-8.0 * np.arange(1, n + 1, dtype=np.float64) / n))


def split_hi_lo(v):
    v = v.astype(np.float32)
    hi = v.astype(NPBF)
    lo = (v - hi.astype(np.float32)).astype(NPBF)
    return hi, lo


def band_bias(slope_eff, W, width=256):
    k = np.arange(128)[:, None].astype(np.float64)
    col = np.arange(width)[None, :].astype(np.float64)
    diff = col - k
    val = -slope_eff * diff * 8.0
    val = np.where((diff >= 0) & (diff < W), val, NEG)
    return val.astype(np.float32)


def host_consts():
    c = {}
    c['ident'] = np.eye(128, dtype=np.float32).astype(NPBF)
    sl = alibi(16)
    t = np.zeros((128, 16, 2, 256), NPBF)
    for h in range(16):
        hi, lo = split_hi_lo(band_bias(sl[h], 128))
        t[:, h, 0], t[:, h, 1] = hi, lo
    c['bsw'] = t
    sl24 = alibi(24)
    dils = (1, 4, 16)
    t = np.zeros((128, 24, 2, 256), NPBF)
    for g in range(3):
        for hs in range(8):
            hi, lo = split_hi_lo(band_bias(sl24[g * 8 + hs] * dils[g], 129))
            t[:, g * 8 + hs, 0], t[:, g * 8 + hs, 1] = hi, lo
    c['bdl'] = t
    t = np.zeros((128, 16, 2, 256), NPBF)
    for h in range(16):
        hi, lo = split_hi_lo(band_bias(sl[h], 10 ** 9))
        t[:, h, 0], t[:, h, 1] = hi, lo
    c['bmo'] = t
    p = np.arange(128)[:, None, None].astype(np.float64)
    j = np.arange(2)[None, :, None].astype(np.float64)
    c['tqm'] = (-sl[None, None, :] * 8.0 * (j * 128 + p) - 30000.0).astype(np.float32)
    idx = np.arange(31)[None, None, :].astype(np.float64)
    c['dtab'] = np.broadcast_to((-sl[None, :, None] * 2048.0 * (15 - idx)), (128, 16, 31)).astype(np.float32).copy()
    hi, lo = split_hi_lo(np.broadcast_to((sl * 8.0)[None, :], (128, 16)).copy())
    c['slp'] = np.stack([hi, lo], axis=-1)
    X = np.zeros((128, 16, 2, 128), np.float32)
    for n in range(16):
        X[n, n] = 1.0
        X[16 + n, n] = 1.0
    for half in range(2):
        X[32, :, half, :] = np.arange(128)[None, :] + 128 * half
        X[33, :, half, :] = np.arange(128)[None, :] + 128 * half
    c['xsel'] = X.astype(NPBF)
    XT = np.zeros((64, SEQ), np.float32)
    pos = np.arange(SEQ)
    for r in range(16):
        XT[r] = (pos // 256 == r)
        XT[16 + r] = (pos // 256 == r)
    XT[32] = pos % 256
    XT[33] = pos % 256
    c['xselT'] = XT.astype(NPBF)
    k = np.arange(128)[:, None]
    col = np.arange(512)[None, :]
    c['mtri'] = np.where(col >= k, 0.0, NEG).astype(np.float32).astype(NPBF)
    inv = 10000.0 ** (-np.arange(0, 32, 2, dtype=np.float64) / 32)
    pos = (np.arange(32)[None, :] * 128 + np.arange(128)[:, None]).astype(np.float64)
    ang = pos[:, :, None] * inv[None, None, :]
    ang = ang.astype(np.float32).astype(np.float64)
    cos, sin = np.cos(ang), np.sin(ang)
    c['ropecs'] = np.concatenate([cos, cos], axis=-1).astype(np.float32)
    c['ropesn'] = np.concatenate([-sin, sin], axis=-1).astype(np.float32)
    return c


CONST_SPECS = {
    'ident': ([128, 128], BF16), 'bsw': ([128, 16, 2, 256], BF16), 'bdl': ([128, 24, 2, 256], BF16),
    'bmo': ([128, 16, 2, 256], BF16), 'tqm': ([128, 2, 16], F32), 'dtab': ([128, 16, 31], F32),
    'slp': ([128, 16, 2], BF16), 'xsel': ([128, 16, 2, 128], BF16), 'xselT': ([64, SEQ], BF16), 'mtri': ([128, 512], BF16),
    'ropecs': ([128, 32, 32], F32), 'ropesn': ([128, 32, 32], F32),
}

WEIGHT_SPECS = [
    ('l0_attn_norm', [1024]), ('l0_w_qkv', [1024, 3072]), ('l0_w_o', [1024, 1024]), ('l0_mlp_norm', [1024]),
    ('l0_w_up', [1024, 4096]), ('l0_w_down', [4096, 1024]),
    ('l1_attn_norm', [1024]), ('l1_w_qkv', [1024, 4608]), ('l1_w_o', [512, 1024]), ('l1_mlp_norm', [1024]),
    ('l1_w_up', [1024, 4096]), ('l1_w_down', [4096, 1024]),
    ('l2_attn_norm', [1024]), ('l2_w_dkv', [1024, 1056]), ('l2_q_norm', [768]), ('l2_w_uq', [768, 1536]),
    ('l2_kv_norm', [256]), ('l2_w_ukv', [256, 2048]), ('l2_w_o', [1024, 1024]), ('l2_mlp_norm', [1024]),
    ('l2_w_up', [1024, 4096]), ('l2_w_down', [4096, 1024]),
    ('l3_attn_norm', [1024]), ('l3_w_qkv', [1024, 1280]), ('l3_sinks', [16]), ('l3_w_o', [1024, 1024]),
    ('l3_mlp_norm', [1024]), ('l3_w_up', [1024, 4096]), ('l3_w_down', [4096, 1024]),
    ('final_norm', [1024]),
]


class Builder:
    def __init__(self, layers=(0, 1, 2, 3), final=True):
        self.layers = tuple(layers)
        self.final = final
        nc = self.nc = bass.Bass("TRN2", target_bir_lowering=False)
        self.S = Sched(nc)
        self.x = nc.dram_tensor("x", [SEQ, DM], F32, kind="ExternalInput").ap()
        self.W = {}
        for name, shape in WEIGHT_SPECS:
            if name == 'final_norm' or int(name[1]) in self.layers:
                self.W[name] = nc.dram_tensor(name, shape, F32, kind="ExternalInput").ap()
        self.C = {}
        for name, (shape, dt) in CONST_SPECS.items():
            self.C[name] = nc.dram_tensor("c_" + name, shape, dt, kind="ExternalInput").ap()
        self.y = nc.dram_tensor("y", [SEQ, DM], F32, kind="ExternalOutput").ap()
        self.hbuf = nc.dram_tensor("hbuf", [SEQ, DM], F32, kind="Internal").ap()
        self.oTs = nc.dram_tensor("oTs", [8, 128, SEQ], BF16, kind="Internal").ap()
        self.qTs = nc.dram_tensor("qTs", [16, 96, SEQ], BF16, kind="Internal").ap()
        self.cast_rr = 0
        for c in range(8):
            self.S.alias[('xnT', c)] = [('xnTs', c, 0), ('xnTs', c, 1)]

    def used_inputs(self):
        return list(self.W.keys())

    def sb(self, es, name, shape, dt):
        self.uid = getattr(self, 'uid', 0) + 1
        return es.enter_context(self.nc.sbuf_tensor(f"{name}_{self.uid}", shape, dt))

    def mm(self, out, lhsT, rhs, start, stop, reads, writes, skip=False):
        self.S.op('pe', lambda e: e.matmul(out, lhsT=lhsT, rhs=rhs, start=start, stop=stop,
                                           skip_group_check=skip), reads, writes)

    def tr(self, out, in_, reads, writes):
        ident = self.ident
        self.S.op('pe', lambda e: e.transpose(out, in_, ident[:]), list(reads) + ['ident'], writes)

    def act(self, out, in_, func, reads, writes, scale=1.0, bias=None, accum=None):
        kw = {}
        if bias is not None:
            kw['bias'] = bias
        if accum is not None:
            kw['accum_out'] = accum
        self.S.op('act', lambda e: e.activation(out=out, in_=in_, func=func, scale=scale, **kw), reads, writes)

    def dma(self, out, in_, reads, writes, q='sp'):
        self.S.dma(q, lambda e: e.dma_start(out=out, in_=in_), reads, writes)

    def load_cast(self, dst, src, n, reads_src=(), wres=None, shape3=None, engs=('dve', 'pool')):
        stg, sres = self.stg.next()
        sv = stg[:, 0:n]
        if shape3 is not None:
            sv = sv.rearrange("p (a b) -> p a b", a=shape3[0])
        self.dma(sv, src, list(reads_src), [sres])
        eng = engs[self.cast_rr % len(engs)]
        self.cast_rr += 1
        if eng == 'act':
            self.act(dst, sv, AF.Copy, [sres], [wres])
        else:
            self.S.op(eng, lambda e: e.tensor_copy(dst, sv), [sres], [wres])

    def rms_stats(self, src_ap, ss_ap, reads, ssres):
        junk, jres = self.junk.next()
        self.act(junk[:], src_ap, AF.Square, reads, [jres, ssres], accum=ss_ap)

    def rms_rstd(self, ss_ap, rstd_ap, ssres, rres, n_feat):
        self.act(rstd_ap, ss_ap, AF.Ln, [ssres, 'epsc'], [rres], scale=1.0 / n_feat, bias=self.epsc[:, 0:1])
        self.act(rstd_ap, rstd_ap, AF.Exp, [rres], [rres], scale=-0.5)

    def phase_norm(self, es, src_dram, gname, xnT):
        S = self.S
        nc = self.nc
        gbc = self.sb(es, "gbc", [128, DM], F32)
        self.dma(gbc[:], self.W[gname].partition_broadcast(128), [], ['gbc'])
        hts = Rot('ht', [self.sb(es, f"ht{i}", [128, DM], F32) for i in range(4)])
        xns = Rot('xn', [self.sb(es, f"xn{i}", [128, DM], BF16) for i in range(3)])
        ss = self.sb(es, "ssn", [128, NT], F32)
        rs = self.sb(es, "rsn", [128, NT], F32)
        pend = None
        for t in range(NT + 1):
            cur = None
            if t < NT:
                ht, hres = hts.next()
                self.dma(ht[:], src_dram[t * 128:(t + 1) * 128, :], [('h', t // 4)], [hres])
                self.rms_stats(ht[:], ss[:, t:t + 1], [hres], ('ssn', t))
                self.rms_rstd(ss[:, t:t + 1], rs[:, t:t + 1], ('ssn', t), ('rsn', t), DM)
                xn, xres = xns.next()
                S.op('dve', lambda e, xn=xn, ht=ht, t=t: e.scalar_tensor_tensor(
                    out=xn[:], in0=ht[:], scalar=rs[:, t:t + 1], in1=gbc[:], op0=ALU.mult, op1=ALU.mult),
                    [hres, ('rsn', t), 'gbc'], [xres])
                tp, tres = self.tps.next()
                for k in range(8):
                    self.tr(tp[:, k * 128:(k + 1) * 128], xn[:, k * 128:(k + 1) * 128], [xres], [tres])
                cur = (t, tp, tres)
            if pend is not None:
                pt_, tp_, tres_ = pend
                dst = xnT[:, :, pt_ * 128:(pt_ + 1) * 128]
                src = tp_[:].rearrange("p (k n) -> p k n", k=8)
                if pt_ % 2 == 0:
                    self.act(dst, src, AF.Copy, [tres_], [('xnTs', pt_ // 4, 0)])
                else:
                    S.op('dve', lambda e, dst=dst, src=src: e.tensor_copy(dst, src), [tres_], [('xnTs', pt_ // 4, 1)])
            pend = cur

    def phase_of(self, li, npair, hsrc, last):
        S = self.S
        W = self.W
        fin = last and self.final
        with contextlib.ExitStack() as es:
            wup = self.sb(es, "wup", [128, 8, DFF], BF16)
            wdn = self.sb(es, "wdn", [128, 32, DM], BF16)
            self.stg = Rot('stg', [self.sb(es, f"stgf{i}", [128, 1024], F32) for i in range(4)])
            wupv = W[f'l{li}_w_up'].rearrange("(k p) n -> p k n", p=128)
            wdnv = W[f'l{li}_w_down'].rearrange("(k p) n -> p k n", p=128)
            pieces = []
            for qq in range(4):
                for k in range(8):
                    pieces.append((wup[:, k, qq * 1024:(qq + 1) * 1024], wupv[:, k, qq * 1024:(qq + 1) * 1024],
                                   ('wup', k, qq)))
            for c in range(32):
                pieces.append((wdn[:, c, :], wdnv[:, c, :], ('wdn', c)))

            def emit_pieces(n):
                for _ in range(n):
                    if pieces:
                        dst, src, wres = pieces.pop(0)
                        self.load_cast(dst, src, 1024, wres=wres, engs=('act',))

            with contextlib.ExitStack() as es1:
                wo = self.sb(es1, "wo", [128, npair, DM], BF16)
                wov = W[f'l{li}_w_o'].rearrange("(k p) n -> p k n", p=128)
                for k in range(npair):
                    self.load_cast(wo[:, k, :], wov[:, k, :], DM, wres=('wo', k), engs=('act',))
                oTg = Rot('oTg', [self.sb(es1, f"oTg{i}", [128, npair, 256], BF16) for i in range(2)])
                hgs = Rot('hgo', [self.sb(es1, f"hgo{i}", [128, 2, DM], F32) for i in range(2)])

                def loads(g):
                    og, ores = oTg.next()
                    hg, hres = hgs.next()
                    self.dma(og[:], self.oTs[0:npair, :, g * 256:(g + 1) * 256].rearrange("a p n -> p a n"),
                             [('oTs', g // 2)], [ores])
                    self.dma(hg[:], hsrc[g * 256:(g + 1) * 256, :].rearrange("(t p) n -> p t n", p=128),
                             [('h', g // 2)], [hres])
                    return og, ores, hg, hres
                nxt = loads(0)
                for g in range(16):
                    og, ores, hg, hres = nxt
                    if g + 1 < 16:
                        nxt = loads(g + 1)
                    for t in range(2):
                        for hf in range(2):
                            pj, pres = self.pjs.next()
                            for k in range(npair):
                                self.mm(pj[:], og[:, k, t * 128:(t + 1) * 128], wo[:, k, hf * 512:(hf + 1) * 512],
                                        k == 0, k == npair - 1, [ores, ('wo', k)], [pres])
                            S.op('dve', lambda e, hg=hg, pj=pj, t=t, hf=hf: e.tensor_tensor(
                                hg[:, t, hf * 512:(hf + 1) * 512], pj[:], hg[:, t, hf * 512:(hf + 1) * 512], ALU.add),
                                [pres, hres], [hres])
                    self.dma(self.hbuf[g * 256:(g + 1) * 256, :].rearrange("(t p) n -> p t n", p=128), hg[:],
                             [hres], [('h', g // 2)])
                    emit_pieces(4)
                emit_pieces(64)
            S.barrier()
            with contextlib.ExitStack() as es2:
                gbc = self.sb(es2, "gbcf", [128, DM], F32)
                self.dma(gbc[:], W[f'l{li}_mlp_norm'].partition_broadcast(128), [], ['gbcf'])
                if fin:
                    gfin = self.sb(es2, "gfin", [128, DM], F32)
                    self.dma(gfin[:], W['final_norm'].partition_broadcast(128), [], ['gfin'])
                hgs = Rot('hg', [self.sb(es2, f"hg{i}", [128, 2, DM], F32) for i in range(2)])
                xns = Rot('xnf', [self.sb(es2, f"xnf{i}", [128, DM], BF16) for i in range(2)])
                xTs = Rot('xT', [self.sb(es2, f"xT{i}", [128, 8, 256], BF16) for i in range(2)])
                uTs = Rot('uT', [self.sb(es2, f"uT{i}", [128, 8, 256], BF16) for i in range(2)])
                rl = Rot('rl', [self.sb(es2, f"rl{i}", [128, 256], F32) for i in range(3)])
                ss = self.sb(es2, "ssf", [128, NT], F32)
                rs = self.sb(es2, "rsf", [128, NT], F32)
                ssl = self.sb(es2, "ssl", [128, NT], F32)
                rsl = self.sb(es2, "rsl", [128, NT], F32)

                def load(g):
                    hg, hres = hgs.next()
                    self.dma(hg[:], self.hbuf[g * 256:(g + 1) * 256, :].rearrange("(t p) n -> p t n", p=128),
                             [('h', g // 2)], [hres])
                    return hg, hres

                def prep(g, hg, hres):
                    x2, x2res = xTs.next()
                    for t in range(2):
                        i = g * 2 + t
                        self.rms_stats(hg[:, t, :], ss[:, i:i + 1], [hres], ('ssf', i))
                        self.rms_rstd(ss[:, i:i + 1], rs[:, i:i + 1], ('ssf', i), ('rsf', i), DM)
                        xn, xres = xns.next()
                        S.op('dve', lambda e, xn=xn, hg=hg, t=t, i=i: e.scalar_tensor_tensor(
                            out=xn[:], in0=hg[:, t, :], scalar=rs[:, i:i + 1], in1=gbc[:], op0=ALU.mult, op1=ALU.mult),
                            [hres, ('rsf', i), 'gbcf'], [xres])
                        tp, tres = self.tps.next()
                        for k in range(8):
                            self.tr(tp[:, k * 128:(k + 1) * 128], xn[:, k * 128:(k + 1) * 128], [xres], [tres])
                        self.act(x2[:, :, t * 128:(t + 1) * 128], tp[:].rearrange("p (k n) -> p k n", k=8), AF.Copy,
                                 [tres], [x2res])
                    return x2, x2res

                cur = load(0)
                curx = prep(0, *cur)
                for g in range(16):
                    hg, hres = cur
                    x2, x2res = curx
                    if g + 1 < 16:
                        nxt = load(g + 1)
                    for q in range(4):
                        uT, ures = uTs.next()
                        for cc in range(8):
                            c = q * 8 + cc
                            sc, sres = self.scs.next()
                            for k in range(8):
                                self.mm(sc[:, 0:256], wup[:, k, c * 128:(c + 1) * 128], x2[:, k, :], k == 0, k == 7,
                                        [x2res, ('wup', k, c // 8)], [sres])
                            r, rres = rl.next()
                            self.act(r[:], sc[:, 0:256], AF.Relu, [sres], [rres])
                            S.op('dve', lambda e, r=r, cc=cc, uT=uT: e.tensor_tensor(uT[:, cc, :], r[:], r[:], ALU.mult),
                                 [rres], [(ures, cc)])
                        if q == 1 and g + 1 < 16:
                            nxtx = prep(g + 1, *nxt)
                        for t in range(2):
                            for hf in range(2):
                                pj, pres = self.pjs.next()
                                for cc in range(8):
                                    c = q * 8 + cc
                                    self.mm(pj[:], uT[:, cc, t * 128:(t + 1) * 128], wdn[:, c, hf * 512:(hf + 1) * 512],
                                            cc == 0, cc == 7, [(ures, cc), ('wdn', c)], [pres])
                                S.op('dve', lambda e, hg=hg, pj=pj, t=t, hf=hf: e.tensor_tensor(
                                    hg[:, t, hf * 512:(hf + 1) * 512], pj[:], hg[:, t, hf * 512:(hf + 1) * 512],
                                    ALU.add), [pres, hres], [hres])
                    rows = slice(g * 256, (g + 1) * 256)
                    if fin:
                        for t in range(2):
                            i = g * 2 + t
                            self.rms_stats(hg[:, t, :], ssl[:, i:i + 1], [hres], ('ssl', i))
                            self.rms_rstd(ssl[:, i:i + 1], rsl[:, i:i + 1], ('ssl', i), ('rsl', i), DM)
                            S.op('dve', lambda e, hg=hg, i=i, t=t: e.scalar_tensor_tensor(
                                out=hg[:, t, :], in0=hg[:, t, :], scalar=rsl[:, i:i + 1], in1=gfin[:],
                                op0=ALU.mult, op1=ALU.mult), [hres, ('rsl', i), 'gfin'], [hres])
                    dst = self.y if last else self.hbuf
                    self.dma(dst[rows, :].rearrange("(t p) n -> p t n", p=128), hg[:], [hres],
                             ['y'] if last else [('h', g // 2)])
                    if g + 1 < 16:
                        cur, curx = nxt, nxtx
        S.barrier()

    def normalize_out(self, src, sres, ncols, slot, pair, col0, extra=None, use_act=False):
        S = self.S
        orow = slice(0, 64) if slot == 0 else slice(64, 128)
        drow = slice(64, 128) if slot == 0 else slice(0, 64)
        rc, rres = self.rcs.next()
        if use_act:
            if extra is not None:
                sc_ap, sc_res = extra
                self.act(rc[orow, 0:ncols], src(drow), AF.Ln, [sres, sc_res], [rres], bias=sc_ap(orow))
            else:
                self.act(rc[orow, 0:ncols], src(drow), AF.Ln, [sres], [rres])
            self.act(rc[orow, 0:ncols], rc[orow, 0:ncols], AF.Exp, [rres], [rres], scale=-1.0)
        else:
            S.op('dve', lambda e: e.tensor_copy(rc[orow, 0:ncols], src(drow)), [sres], [rres])
            if extra is not None:
                sc_ap, sc_res = extra
                S.op('dve', lambda e: e.tensor_scalar(rc[orow, 0:ncols], rc[orow, 0:ncols], sc_ap(orow), None,
                                                      ALU.add), [rres, sc_res], [rres])
            S.op('dve', lambda e: e.reciprocal(rc[orow, 0:ncols], rc[orow, 0:ncols]), [rres], [rres])
        on, onres = self.ons.next()
        S.op('dve', lambda e: e.tensor_tensor(on[orow, 0:ncols], src(orow), rc[orow, 0:ncols], ALU.mult),
             [sres, rres], [onres])
        self.dma(self.oTs[pair, orow, col0:col0 + ncols], on[orow, 0:ncols], [onres], [('oTs', col0 // 512)])

    def alloc_attn_common(self, es, nslots=4):
        self.pts = Rot('pt', [self.sb(es, f"pt{i}", [128, 512], BF16) for i in range(6)])
        self.rcs = Rot('rc', [self.sb(es, f"rc{i}", [128, 512], F32) for i in range(3)])
        self.ons = Rot('on', [self.sb(es, f"on{i}", [128, 512], BF16) for i in range(2)])
        self.stg = Rot('stg', [self.sb(es, f"stga{i}", [128, 1024], F32) for i in range(3)])
        self.pending = []
        t0, t1 = self.scs.tiles
        p0, p1 = self.pjs.tiles
        if nslots == 4:
            self.scslots = [(t0, 0, ('sc', 0)), (t1, 0, ('sc', 1)), (p0, 0, ('pj', 0)), (p1, 0, ('pj', 1))]
        else:
            self.scslots = [(t0, 0, ('sc', 0)), (t1, 0, ('sc', 1)), (p0, 0, ('pj', 0))]
        self.skew = 2
        self.sci = 0

    def attn_step(self, n, qk_list, scale, pv, after=None):
        tile, off, sres = self.scslots[self.sci % len(self.scslots)]
        self.sci += 1
        sc = tile[:, off:off + n]
        for i, (lhsT, rhs, reads) in enumerate(qk_list):
            self.mm(sc, lhsT, rhs, i == 0, i == len(qk_list) - 1, reads, [sres])
        pt, ptres = self.pts.next()
        self.act(pt[:, 0:n], sc, AF.Exp, [sres], [ptres], scale=scale)
        self.pending.append((pv, pt, ptres, n, after))
        while len(self.pending) > self.skew:
            self._attn_pop()

    def _attn_pop(self):
        pv, pt, ptres, n, after = self.pending.pop(0)
        out_ap, lhsT, reads, ores, first, last = pv
        self.mm(out_ap, lhsT, pt[:, 0:n], first, last, list(reads) + [ptres], [ores], skip=True)
        if after is not None:
            after()

    def attn_flush(self):
        while self.pending:
            self._attn_pop()

    def phase_swa(self, li, xnT):
        S = self.S
        W = self.W
        with contextlib.ExitStack() as es:
            self.alloc_attn_common(es)
            bsw = self.sb(es, "bsw", [128, 16, 2, 256], BF16)
            self.dma(bsw[:], self.C['bsw'], [], ['bsw'])
            esink = self.sb(es, "esink", [128, 16], F32)
            self.dma(esink[:], W[f'l{li}_sinks'].partition_broadcast(128), [], ['esink'])
            self.act(esink[:], esink[:], AF.Exp, ['esink'], ['esink'])
            wkv = self.sb(es, "wkv", [128, 8, 256], BF16)
            wqs = Rot('wq', [self.sb(es, f"wq{i}", [128, 8, 128], BF16) for i in range(2)])
            kTv = self.sb(es, "kTv", [128, 2, 2, SEQ], BF16)
            vaug = self.sb(es, "vaug", [128, NT, 2, 2, 128], BF16)
            qTs = Rot('qT', [self.sb(es, f"qT{i}", [128, SEQ], BF16) for i in range(2)])
            wv = W[f'l{li}_w_qkv'].rearrange("(k p) n -> p k n", p=128)
            self.load_cast(wkv[:, :, 0:128], wv[:, :, 1024:1152], 1024, wres='wkv', shape3=(8, 128))
            self.load_cast(wkv[:, :, 128:256], wv[:, :, 1152:1280], 1024, wres='wkv2', shape3=(8, 128))
            S.op('pool', lambda e: e.memset(vaug[:, :, :, 0, 64:128], 1.0), [], ['vaug1'])
            S.op('pool', lambda e: e.memset(vaug[:, :, :, 1, 0:64], 1.0), [], ['vaug1'])
            for kvh in range(2):
                S.op('pool', lambda e, kvh=kvh: e.memset(kTv[64:128, kvh, 0, :], 0.0), [], ['kTz'])
                S.op('pool', lambda e, kvh=kvh: e.memset(kTv[0:64, kvh, 1, :], 0.0), [], ['kTz'])
            for c in range(8):
                pj, pres = self.pjs.next()
                cs = slice(c * 512, (c + 1) * 512)
                for k in range(8):
                    self.mm(pj[:], wkv[:, k, 0:128], xnT[:, k, cs], k == 0, k == 7, ['wkv', ('xnT', c)], [pres])
                self.act(kTv[0:64, 0, 0, cs], pj[0:64, :], AF.Copy, [pres, 'kTz'], [('kT', c)])
                self.act(kTv[64:128, 0, 1, cs], pj[0:64, :], AF.Copy, [pres, 'kTz'], [('kT', c)])
                S.op('dve', lambda e, pj=pj, cs=cs: e.tensor_copy(kTv[64:128, 1, 1, cs], pj[64:128, :]),
                     [pres, 'kTz'], [('kT', c)])
                S.op('dve', lambda e, pj=pj, cs=cs: e.tensor_copy(kTv[0:64, 1, 0, cs], pj[64:128, :]),
                     [pres, 'kTz'], [('kT', c)])
            for t in range(NT):
                pj, pres = self.pjs.next()
                for k in range(8):
                    self.mm(pj[:, 0:128], xnT[:, k, t * 128:(t + 1) * 128], wkv[:, k, 128:256], k == 0, k == 7,
                            ['wkv2', ('xnT', t // 4)], [pres])
                for kvh in range(2):
                    S.op('dve', lambda e, pj=pj, t=t, kvh=kvh: e.tensor_copy(
                        vaug[:, t, kvh, 0, 0:64], pj[:, kvh * 64:(kvh + 1) * 64]), [pres, 'vaug1'], [('vaug', t)])
                    S.op('dve', lambda e, pj=pj, t=t, kvh=kvh: e.tensor_copy(
                        vaug[:, t, kvh, 1, 64:128], pj[:, kvh * 64:(kvh + 1) * 64]), [pres, 'vaug1'], [('vaug', t)])
            def wq_load(p):
                wq, wqres = wqs.next()
                self.load_cast(wq[:], wv[:, :, p * 128:(p + 1) * 128], 1024, wres=wqres, shape3=(8, 128), engs=('pool',))
                return wq, wqres
            nextwq = wq_load(0)
            for p in range(8):
                wq, wqres = nextwq
                if p + 1 < 8:
                    nextwq = wq_load(p + 1)
                qT, qres = qTs.next()
                for c in range(8):
                    pj, pres = self.pjs.next()
                    for k in range(8):
                        self.mm(pj[:], wq[:, k, :], xnT[:, k, c * 512:(c + 1) * 512], k == 0, k == 7,
                                [wqres, ('xnT', c)], [pres])
                    self.act(qT[:, c * 512:(c + 1) * 512], pj[:], AF.Copy, [pres], [(qres, c)])
                for slot in range(2):
                    h = 2 * p + slot
                    kvh = h // 8
                    for c in range(8):
                        oa, ores = self.oas.next()
                        kts = [kt for kt in range(4 * c - 1, 4 * c + 4) if kt >= 0]
                        for si, kt in enumerate(kts):
                            qts = [qt for qt in (kt, kt + 1) if 4 * c <= qt <= 4 * c + 3]
                            col0 = (qts[0] - 4 * c) * 128
                            n = 128 * len(qts)
                            boff = 0 if qts[0] == kt else 128
                            qk = [(kTv[:, kvh, slot, kt * 128:(kt + 1) * 128], qT[:, c * 512 + col0:c * 512 + col0 + n],
                                   [('kT', kt // 4), 'kTz', (qres, c)]),
                                  (self.ident[:], bsw[:, h, 0, boff:boff + n], ['ident', 'bsw']),
                                  (self.ident[:], bsw[:, h, 1, boff:boff + n], ['ident', 'bsw'])]
                            last = si == len(kts) - 1
                            after = None
                            if last:
                                after = (lambda oa=oa, ores=ores, slot=slot, p=p, c=c, h=h: self.normalize_out(
                                    lambda rows: oa[rows, 0:512], ores, 512, slot, p, c * 512,
                                    extra=(lambda rows: esink[rows, h:h + 1], 'esink'), use_act=True))
                            self.attn_step(n, qk, 0.125, (oa[:, col0:col0 + n], vaug[:, kt, kvh, slot, :],
                                                          [('vaug', kt), 'vaug1'], ores, si == 0, last), after)
                self.attn_flush()
        S.barrier()

    def alloc_pair(self, es):
        S = self.S
        self.wqkv = Rot('wqkv', [self.sb(es, f"wqkv{i}", [128, 8, 384], BF16) for i in range(2)])
        self.qT = self.sb(es, "qTp", [128, SEQ], BF16)
        self.kTz = self.sb(es, "kTz", [128, 2, SEQ], BF16)
        self.vaug = self.sb(es, "vaugp", [128, NT, 2, 128], BF16)
        self.vTs = Rot('vT', [self.sb(es, f"vT{i}", [128, 512], BF16) for i in range(2)])
        S.op('pool', lambda e: e.memset(self.kTz[64:128, 0, :], 0.0), [], ['kTzz'])
        S.op('pool', lambda e: e.memset(self.kTz[0:64, 1, :], 0.0), [], ['kTzz'])
        S.op('pool', lambda e: e.memset(self.vaug[:, :, 0, 64:128], 1.0), [], ['vaug1'])
        S.op('pool', lambda e: e.memset(self.vaug[:, :, 1, 0:64], 1.0), [], ['vaug1'])

    def pair_load(self, wv, qc, kc, vc):
        w, wres = self.wqkv.next()
        for j, c0 in enumerate((qc, kc, vc)):
            self.load_cast(w[:, :, j * 128:(j + 1) * 128], wv[:, :, c0:c0 + 128], 1024, wres=(wres, j), shape3=(8, 128),
                           engs=('pool',))
        return w, wres

    def pair_proj(self, wl, xnT, colmap=None):
        S = self.S
        w, wres = wl
        qT, kTz, vaug = self.qT, self.kTz, self.vaug
        for c in range(8):
            cs = slice(c * 512, (c + 1) * 512)
            xres = ('xnT', c) if colmap is None else 'xnTall'
            outs = []
            for j in range(3):
                pj, pres = self.pjs.next()
                for k in range(8):
                    rhs = xnT[:, k, cs] if colmap is None else colmap(k, c)
                    o = pj[:] if len(rhs.shape) == 2 else pj[:].rearrange("p (a b) -> p a b", a=rhs.shape[1])
                    self.mm(o, w[:, k, j * 128:(j + 1) * 128], rhs, k == 0, k == 7,
                            [(wres, j)] + ([xres] if colmap is None else [('xnT', cc) for cc in range(8)]), [pres])
                if j == 0:
                    self.act(qT[:, cs], pj[:], AF.Copy, [pres], [('qT', c)])
                elif j == 1:
                    self.act(kTz[0:64, 0, cs], pj[0:64, :], AF.Copy, [pres, 'kTzz'], [('kT', c)])
                    S.op('dve', lambda e, pj=pj, cs=cs: e.tensor_copy(kTz[64:128, 1, cs], pj[64:128, :]),
                         [pres, 'kTzz'], [('kT', c)])
                else:
                    vT, vres = self.vTs.next()
                    self.act(vT[:], pj[:], AF.Copy, [pres], [vres])
                    tp, tres = self.tps.next()
                    for jj in range(4):
                        self.tr(tp[:, jj * 128:(jj + 1) * 128], vT[:, jj * 128:(jj + 1) * 128], [vres], [tres])
                    tv = tp[:, 0:512].rearrange("p (a b) -> p a b", a=4)
                    S.op('dve', lambda e, tv=tv, c=c: e.tensor_copy(vaug[:, 4 * c:4 * c + 4, 0, 0:64], tv[:, :, 0:64]),
                         [tres, 'vaug1'], [('vaug', c)])
                    S.op('dve', lambda e, tv=tv, c=c: e.tensor_copy(vaug[:, 4 * c:4 * c + 4, 1, 64:128],
                                                                  tv[:, :, 64:128]), [tres, 'vaug1'], [('vaug', c)])

    def phase_dilated(self, li, xnT):
        S = self.S
        W = self.W
        dils = (1, 4, 16)
        with contextlib.ExitStack() as es:
            self.alloc_attn_common(es)
            self.alloc_pair(es)
            bdl = self.sb(es, "bdl", [128, 24, 2, 256], BF16)
            self.dma(bdl[:], self.C['bdl'], [], ['bdl'])
            acc = [self.sb(es, f"acc{i}", [128, SEQ], F32) for i in range(2)]
            wv = W[f'l{li}_w_qkv'].rearrange("(k p) n -> p k n", p=128)
            qT, kTz, vaug = self.qT, self.kTz, self.vaug
            nextw = self.pair_load(wv, 0, 512, 1024)
            for pp in range(4):
                for g in range(3):
                    d = dils[g]
                    Sd = SEQ // d
                    nseg = Sd // 128

                    def colmap(k, c, d=d, Sd=Sd):
                        if d == 1:
                            return xnT[:, k, c * 512:(c + 1) * 512]
                        if Sd >= 512:
                            r, i0 = divmod(c * 512, Sd)
                            st = r + d * i0
                            return xnT[:, k, st:st + d * 511 + 1:d]
                        v = xnT[:, k, :].rearrange("p (j d) -> p d j", d=d)
                        nr = 512 // Sd
                        return v[:, c * nr:(c + 1) * nr, :]

                    wl = nextw
                    ni = pp * 3 + g + 1
                    if ni < 12:
                        npp, ng = divmod(ni, 3)
                        nb_ = (ng * 3) * 512
                        nextw = self.pair_load(wv, nb_ + npp * 128, nb_ + 512 + npp * 128, nb_ + 1024 + npp * 128)
                    self.pair_proj(wl, xnT, colmap=(None if d == 1 else colmap))
                    for slot in range(2):
                        hs = 2 * pp + slot
                        hb = g * 8 + hs
                        for c in range(8):
                            oa, ores = self.oas.next()
                            steps = []
                            for kt in range(4 * c - 1, 4 * c + 4):
                                if kt < 0:
                                    continue
                                qts = [kt] + ([kt + 1] if (kt + 1) % nseg != 0 else [])
                                qts = [qt for qt in qts if 4 * c <= qt <= 4 * c + 3]
                                if qts:
                                    steps.append((kt, qts))
                            A = acc[slot]
                            if d == 1:
                                def after(oa=oa, ores=ores, A=A, c=c, slot=slot):
                                    self.act(A[:, c * 512:(c + 1) * 512], oa[:], AF.Copy, [ores, ('accall', slot)],
                                             [('acc', slot, c)])
                            else:
                                if Sd >= 512:
                                    r, i0 = divmod(c * 512, Sd)
                                    st = r + d * i0
                                    pieces = [(A[:, st:st + d * 511 + 1:d], oa[:, 0:512])]
                                else:
                                    nr = 512 // Sd
                                    pieces = []
                                    for rr in range(nr):
                                        r = c * nr + rr
                                        pieces.append((A[:, r:r + d * (Sd - 1) + 1:d], oa[:, rr * Sd:(rr + 1) * Sd]))

                                def after(pieces=pieces, ores=ores, slot=slot):
                                    for (av, ov) in pieces:
                                        S.op('dve', lambda e, av=av, ov=ov: e.tensor_tensor(av, ov, av, ALU.add),
                                             [ores] + [('acc', slot, cc) for cc in range(8)], [('accall', slot)])
                            for si, (kt, qts) in enumerate(steps):
                                col0 = (qts[0] - 4 * c) * 128
                                n = 128 * len(qts)
                                boff = 0 if qts[0] == kt else 128
                                qk = [(kTz[:, slot, kt * 128:(kt + 1) * 128], qT[:, c * 512 + col0:c * 512 + col0 + n],
                                       [('kT', kt // 4), 'kTzz', ('qT', c)]),
                                      (self.ident[:], bdl[:, hb, 0, boff:boff + n], ['ident', 'bdl']),
                                      (self.ident[:], bdl[:, hb, 1, boff:boff + n], ['ident', 'bdl'])]
                                last = si == len(steps) - 1
                                self.attn_step(n, qk, 0.125, (oa[:, col0:col0 + n], vaug[:, kt, slot, :],
                                                              [('vaug', kt // 4), 'vaug1'], ores, si == 0, last),
                                               after if last else None)
                    self.attn_flush()
                for slot in range(2):
                    A = acc[slot]
                    for c in range(8):
                        self.normalize_out(lambda rows, A=A, c=c: A[rows, c * 512:(c + 1) * 512], ('accall', slot),
                                           512, slot, pp, c * 512, use_act=True)
        S.barrier()

    def phase_moba(self, li, xnT):
        S = self.S
        W = self.W
        C = self.C
        with contextlib.ExitStack() as es:
            self.alloc_attn_common(es, nslots=3)
            self.wqkv = Rot('wqkv', [self.sb(es, f"wqkv{i}", [128, 8, 384], BF16) for i in range(2)])
            qY = self.sb(es, "qY", [128, 2, SEQ], BF16)
            kX = self.sb(es, "kX", [128, 2, SEQ], BF16)
            vaug = self.sb(es, "vaugp", [128, NT, 2, 128], BF16)
            self.vTs = Rot('vT', [self.sb(es, f"vT{i}", [128, 512], BF16) for i in range(2)])
            for sl in range(2):
                S.op('pool', lambda e, sl=sl: e.memset(qY[64:128, sl, :], 0.0), [], [('qYz', sl)])
                self.dma(kX[64:128, sl, :], C['xselT'], [], ['xselT'])
            S.op('pool', lambda e: e.memset(vaug[:, :, 0, 64:128], 1.0), [], ['vaug1'])
            S.op('pool', lambda e: e.memset(vaug[:, :, 1, 0:64], 1.0), [], ['vaug1'])
            qZs = Rot('qZ', [self.sb(es, f"qZ{i}", [128, 256], BF16) for i in range(3)])
            for i in range(3):
                S.op('pool', lambda e, i=i: e.memset(qZs.tiles[i][64:128, :], 0.0), [], [('qZ', i)])
            bmo = self.sb(es, "bmo", [128, 16, 2, 256], BF16)
            self.dma(bmo[:], C['bmo'], [], ['bmo'])
            tqm = self.sb(es, "tqm", [128, 2, 16], F32)
            self.dma(tqm[:], C['tqm'], [], ['tqm'])
            dtab = self.sb(es, "dtab", [128, 16, 31], F32)
            self.dma(dtab[:], C['dtab'], [], ['dtab'])
            slp = self.sb(es, "slp", [128, 16, 2], BF16)
            self.dma(slp[:], C['slp'], [], ['slp'])
            c30 = self.sb(es, "c30", [128, 16], F32)
            S.op('dve', lambda e: e.memset(c30[:], 30000.0), [], ['c30'])
            kmf = self.sb(es, "kmf", [64, 16], F32)
            kmT = self.sb(es, "kmT", [128, 2, 16], BF16)
            S.op('pool', lambda e: e.memset(kmT[:], 0.0), [], [('kmT', 0), ('kmT', 1)])
            gss = Rot('gs', [self.sb(es, f"gs{i}", [128, 16], F32) for i in range(4)])
            t8s = Rot('t8', [self.sb(es, f"t8{i}", [128, 8], F32) for i in range(4)])
            s3s = Rot('s3', [self.sb(es, f"s3{i}", [128, 16], F32) for i in range(4)])
            vvs = Rot('vv', [self.sb(es, f"vv{i}", [128, 16], F32) for i in range(4)])
            yps = Rot('yp', [self.sb(es, f"yp{i}", [128, 128], BF16) for i in range(6)])
            for i in range(6):
                S.op('pool', lambda e, i=i: e.memset(yps.tiles[i][:], 0.0), [], [('yp', i)])
            gp, gpres = self.pjs.tiles[1], ('pj', 1)
            wv = W[f'l{li}_w_qkv'].rearrange("(k p) n -> p k n", p=128)
            nextw = self.pair_load(wv, 0, 1024, 2048)
            for p in range(8):
                w, wres = nextw
                if p + 1 < 8:
                    nextw = self.pair_load(wv, (p + 1) * 128, 1024 + (p + 1) * 128, 2048 + (p + 1) * 128)
                for c in range(8):
                    cs = slice(c * 512, (c + 1) * 512)
                    for j in range(3):
                        pj, pres = self.pjs.next()
                        for k in range(8):
                            self.mm(pj[:], w[:, k, j * 128:(j + 1) * 128], xnT[:, k, cs], k == 0, k == 7,
                                    [(wres, j), ('xnT', c)], [pres])
                        if j < 2:
                            dst = qY if j == 0 else kX
                            nm = 'qT' if j == 0 else 'kT'
                            self.act(dst[0:64, 0, cs], pj[0:64, :], AF.Copy, [pres], [(nm, 0, c)])
                            S.op('dve', lambda e, pj=pj, cs=cs, dst=dst: e.tensor_copy(dst[0:64, 1, cs], pj[64:128, :]),
                                 [pres], [(nm, 1, c)])
                        else:
                            vT, vres = self.vTs.next()
                            self.act(vT[:], pj[:], AF.Copy, [pres], [vres])
                            tp, tres = self.tps.next()
                            for jj in range(4):
                                self.tr(tp[:, jj * 128:(jj + 1) * 128], vT[:, jj * 128:(jj + 1) * 128], [vres], [tres])
                            tv = tp[:, 0:512].rearrange("p (a b) -> p a b", a=4)
                            S.op('dve', lambda e, tv=tv, c=c: e.tensor_copy(vaug[:, 4 * c:4 * c + 4, 0, 0:64],
                                                                          tv[:, :, 0:64]), [tres, 'vaug1'], [('vaug', c)])
                            S.op('dve', lambda e, tv=tv, c=c: e.tensor_copy(vaug[:, 4 * c:4 * c + 4, 1, 64:128],
                                                                          tv[:, :, 64:128]), [tres, 'vaug1'],
                                 [('vaug', c)])
                for slot in range(2):
                    h = 2 * p + slot
                    allk = [('kT', slot, c) for c in range(8)]
                    S.op('dve', lambda e, slot=slot: e.tensor_reduce(
                        out=kmf[:], in_=kX[0:64, slot, :].rearrange("p (n k) -> p n k", k=256), axis=AX.X, op=ALU.add),
                        allk, ['kmf'])
                    S.op('dve', lambda e, slot=slot: e.tensor_scalar(kmT[0:64, slot, :], kmf[:], 1.0 / 256, None, ALU.mult),
                         ['kmf'], [('kmT', slot)])
                    ypd = {}

                    def prepA(b, slot=slot, h=h, ypd=ypd):
                        for j in range(2):
                            tcols = slice(b * 256 + j * 128, b * 256 + (j + 1) * 128)
                            if b > 3:
                                self.mm(gp[:, 0:16], qY[:, slot, tcols], kmT[:, slot, :], True, True,
                                        [('qT', slot, b // 2), ('qYz', slot), ('kmT', slot)], [gpres])
                                gs, gres = gss.next()
                                S.op('dve', lambda e, gs=gs: e.tensor_copy(gs[:], gp[:, 0:16]), [gpres], [gres])
                                S.op('dve', lambda e, gs=gs, b=b: e.memset(gs[:, b:16], -1e30), [], [gres])
                                t8, t8res = t8s.next()
                                S.op('dve', lambda e, gs=gs, t8=t8: e.max(t8[:], gs[:]), [gres], [t8res])
                                s3, s3res = s3s.next()
                                S.op('dve', lambda e, gs=gs, t8=t8, s3=s3: e.tensor_scalar(
                                    s3[:], gs[:], t8[:, 2:3], 30000.0, ALU.is_ge, ALU.mult), [gres, t8res], [s3res])
                            else:
                                s3, s3res = c30, 'c30'
                            vv, vres = vvs.next()
                            S.op('dve', lambda e, s3=s3, vv=vv, j=j: e.scalar_tensor_tensor(
                                out=vv[:], in0=s3[:], scalar=tqm[:, j, h:h + 1], in1=dtab[:, h, 15 - b:31 - b],
                                op0=ALU.add, op1=ALU.add), [s3res, 'tqm', 'dtab'], [vres])
                            yp, ypres = yps.next()
                            S.op('dve', lambda e, yp=yp, vv=vv: e.tensor_copy(yp[:, 0:16], vv[:]), [vres], [ypres])
                            S.op('dve', lambda e, yp=yp, vv=vv: e.tensor_tensor(yp[:, 16:32], vv[:], yp[:, 0:16],
                                                                              ALU.subtract), [vres, ypres], [ypres])
                            S.op('dve', lambda e, yp=yp: e.tensor_copy(yp[:, 32:34], slp[:, h, :]), ['slp'], [ypres])
                            ypd[(b, j)] = (yp, ypres)

                    def prepB(b, slot=slot, ypd=ypd):
                        for j in range(2):
                            yp, ypres = ypd[(b, j)]
                            tp, tres = self.tps.next()
                            self.tr(tp[:, 0:128], yp[:], [ypres], [tres])
                            cols = slice(b * 256 + j * 128, b * 256 + (j + 1) * 128)
                            S.op('dve', lambda e, tp=tp, cols=cols: e.tensor_copy(qY[64:128, slot, cols], tp[0:64, 0:128]),
                                 [tres, ('qYz', slot)], [('qY', slot, b)])

                    def steps(b, slot=slot, h=h, p=p):
                        oa, ores = self.oas.next()
                        qcols = slice(b * 256, (b + 1) * 256)
                        qz, qzres = qZs.next()
                        S.op('pool', lambda e, qz=qz: e.tensor_copy(qz[0:64, :], qY[0:64, slot, qcols]),
                             [('qT', slot, b // 2)], [qzres])
                        first = True
                        for n in range(b):
                            for half in range(2):
                                kt = 2 * n + half
                                qk = [(kX[:, slot, kt * 128:(kt + 1) * 128], qY[:, slot, qcols],
                                       [('kT', slot, kt // 4), 'xselT', ('qT', slot, b // 2), ('qY', slot, b)])]
                                self.attn_step(256, qk, 0.125, (oa[:, 0:256], vaug[:, kt, slot, :],
                                                                [('vaug', kt // 4), 'vaug1'], ores, first, False))
                                first = False
                        for half in range(2):
                            kt = 2 * b + half
                            n = 256 - 128 * half
                            qk = [(kX[:, slot, kt * 128:(kt + 1) * 128], qz[:, 128 * half:256],
                                   [('kT', slot, kt // 4), 'xselT', qzres]),
                                  (self.ident[:], bmo[:, h, 0, 0:n], ['ident', 'bmo']),
                                  (self.ident[:], bmo[:, h, 1, 0:n], ['ident', 'bmo'])]
                            after = None
                            if half == 1:
                                after = (lambda oa=oa, ores=ores: self.normalize_out(
                                    lambda rows: oa[rows, 0:256], ores, 256, slot, p, b * 256))
                            self.attn_step(n, qk, 0.125, (oa[:, 128 * half:256], vaug[:, kt, slot, :],
                                                          [('vaug', kt // 4), 'vaug1'], ores, first, half == 1), after)
                            first = False

                    prepA(1)
                    prepA(2)
                    prepB(1)
                    for b in range(16):
                        if b >= 1 and b + 1 <= 15:
                            prepB(b + 1)
                        if b >= 1 and b + 2 <= 15:
                            prepA(b + 2)
                        steps(b)
                    self.attn_flush()
        S.barrier()

    def phase_mla(self, li, xnT):
        S = self.S
        W = self.W
        C = self.C
        with contextlib.ExitStack() as es:
            ckvT = self.sb(es, "ckvT", [128, 2, SEQ], BF16)
            krT = self.sb(es, "krT", [32, SEQ], BF16)
            wukv = self.sb(es, "wukv", [128, 2, 2048], BF16)
            mtri = self.sb(es, "mtri", [128, 512], BF16)
            self.dma(mtri[:], C['mtri'], [], ['mtri'])
            A, Ares = self.pjs.tiles[0], ('pj', 0)
            B, Bres = self.pjs.tiles[1], ('pj', 1)
            Cb, Cres = self.scs.tiles[0], ('sc', 0)
            Q = [(self.oas.tiles[0], ('oa', 0)), (self.oas.tiles[1], ('oa', 1)), (self.scs.tiles[1], ('sc', 1))]
            tpA, tAres = self.tps.tiles[0], ('tp', 0)
            tpB, tBres = self.tps.tiles[1], ('tp', 1)
            with contextlib.ExitStack() as es1:
                self.stg = Rot('stg', [self.sb(es1, f"stgm{i}", [128, 1024], F32) for i in range(2)])
                wdkv = self.sb(es1, "wdkv", [128, 8, 1056], BF16)
                wuq = self.sb(es1, "wuq", [128, 6, 1536], BF16)
                wd = W[f'l{li}_w_dkv'].rearrange("(k p) n -> p k n", p=128)
                for k in range(8):
                    self.load_cast(wdkv[:, k, 0:1024], wd[:, k, 0:1024], 1024, wres=('wdkv', k))
                if not DBG.get('skip_wdkvr'):
                    self.load_cast(wdkv[:, :, 1024:1056], wd[:, :, 1024:1056], 256, wres='wdkvr', shape3=(8, 32))
                wq = W[f'l{li}_w_uq'].rearrange("(k p) n -> p k n", p=128)
                for k in range(6):
                    self.load_cast(wuq[:, k, 0:1024], wq[:, k, 0:1024], 1024, wres=('wuq', k))
                    self.load_cast(wuq[:, k, 1024:1536], wq[:, k, 1024:1536], 512, wres=('wuq2', k))
                wk = W[f'l{li}_w_ukv'].rearrange("(k p) n -> p k n", p=128)
                for k in range(2):
                    for hf in range(2):
                        self.load_cast(wukv[:, k, hf * 1024:(hf + 1) * 1024], wk[:, k, hf * 1024:(hf + 1) * 1024], 1024,
                                       wres=('wukv', k, hf))
                qg = self.sb(es1, "qg", [128, 768], F32)
                kvg = self.sb(es1, "kvg", [128, 256], F32)
                if not DBG.get('skip_g'):
                    self.dma(qg[:], W[f'l{li}_q_norm'].partition_broadcast(128), [], ['qg'])
                    self.dma(kvg[:], W[f'l{li}_kv_norm'].partition_broadcast(128), [], ['kvg'])
                cs = self.sb(es1, "ropecs", [128, 32, 32], F32)
                sn = self.sb(es1, "ropesn", [128, 32, 32], F32)
                if not DBG.get('skip_rope'):
                    self.dma(cs[:], C['ropecs'], [], ['ropecs'])
                    self.dma(sn[:], C['ropesn'], [], ['ropesn'])
                cqns = Rot('cqn', [self.sb(es1, f"cqn{i}", [128, 768], BF16) for i in range(2)])
                ckvns = Rot('ckvn', [self.sb(es1, f"ckvn{i}", [128, 256], BF16) for i in range(2)])
                krrs = Rot('krr', [self.sb(es1, f"krr{i}", [128, 128], BF16) for i in range(2)])
                for i in range(2):
                    S.op('pool', lambda e, i=i: e.memset(krrs.tiles[i][:], 0.0), [], [('krr', i)])
                ra = self.sb(es1, "ra", [128, 32], F32)
                rb = self.sb(es1, "rb", [128, 32], F32)
                cqTs = Rot('cqT', [self.sb(es1, f"cqT{i}", [128, 6, 128], BF16) for i in range(2)])
                qf = self.sb(es1, "qf", [128, 1536], F32)
                qbs = Rot('qb', [self.sb(es1, f"qb{i}", [128, 1536], BF16) for i in range(2)])
                ta = self.sb(es1, "ta", [128, 16, 32], F32)
                tb = self.sb(es1, "tb", [128, 16, 32], F32)
                qst = self.sb(es1, "qst", [96, 16, 512], BF16)
                ssa = self.sb(es1, "ssa", [128, NT], F32)
                ssb = self.sb(es1, "ssb", [128, NT], F32)
                ssk = self.sb(es1, "ssk", [128, NT], F32)
                rsq = self.sb(es1, "rsq", [128, NT], F32)
                rsk = self.sb(es1, "rsk", [128, NT], F32)
                def stageXa(t):
                    ts = slice(t * 128, (t + 1) * 128)
                    t1 = slice(t, t + 1)
                    for (dst, dres, c0, c1) in ((A, Ares, 0, 512), (B, Bres, 512, 768), (Cb, Cres, 768, 1056)):
                        for k in range(8):
                            self.mm(dst[:, 0:c1 - c0], xnT[:, k, ts], wdkv[:, k, c0:c1], k == 0, k == 7,
                                    [('xnT', t // 4), ('wdkv', k), 'wdkvr'], [dres])
                    junk, jres = self.junk.next()
                    self.act(junk[:, 0:512], A[:], AF.Square, [Ares], [jres, ('ssa', t)], accum=ssa[:, t1])
                    junk, jres = self.junk.next()
                    self.act(junk[:, 0:256], B[:, 0:256], AF.Square, [Bres], [jres, ('ssb', t)], accum=ssb[:, t1])
                    junk, jres = self.junk.next()
                    self.act(junk[:, 0:256], Cb[:, 0:256], AF.Square, [Cres], [jres, ('ssk', t)], accum=ssk[:, t1])
                    S.op('dve', lambda e, t1=t1: e.tensor_tensor(ssa[:, t1], ssa[:, t1], ssb[:, t1], ALU.add),
                         [('ssa', t), ('ssb', t)], [('ssa', t)])
                    self.rms_rstd(ssa[:, t1], rsq[:, t1], ('ssa', t), ('rsq', t), 768)
                    self.rms_rstd(ssk[:, t1], rsk[:, t1], ('ssk', t), ('rsk', t), 256)
                    cqn, cqres = cqns.next()
                    ckvn, ckres = ckvns.next()
                    S.op('dve', lambda e, cqn=cqn, t1=t1: e.scalar_tensor_tensor(
                        out=cqn[:, 0:512], in0=A[:], scalar=rsq[:, t1], in1=qg[:, 0:512], op0=ALU.mult, op1=ALU.mult),
                        [Ares, ('rsq', t), 'qg'], [cqres])
                    S.op('dve', lambda e, cqn=cqn, t1=t1: e.scalar_tensor_tensor(
                        out=cqn[:, 512:768], in0=B[:, 0:256], scalar=rsq[:, t1], in1=qg[:, 512:768],
                        op0=ALU.mult, op1=ALU.mult), [Bres, ('rsq', t), 'qg'], [cqres])
                    S.op('dve', lambda e, ckvn=ckvn, t1=t1: e.scalar_tensor_tensor(
                        out=ckvn[:], in0=Cb[:, 0:256], scalar=rsk[:, t1], in1=kvg[:], op0=ALU.mult, op1=ALU.mult),
                        [Cres, ('rsk', t), 'kvg'], [ckres])
                    krr, krres = krrs.next()
                    S.op('dve', lambda e, t=t: e.tensor_tensor(ra[:], Cb[:, 256:288], cs[:, t, :], ALU.mult),
                         [Cres, 'ropecs', ('ssk', t)], ['ra'])
                    S.op('dve', lambda e, t=t: e.tensor_tensor(rb[:, 0:16], Cb[:, 272:288], sn[:, t, 0:16], ALU.mult),
                         [Cres, 'ropesn', ('ssk', t)], ['rb'])
                    S.op('dve', lambda e, t=t: e.tensor_tensor(rb[:, 16:32], Cb[:, 256:272], sn[:, t, 16:32], ALU.mult),
                         [Cres, 'ropesn', ('ssk', t)], ['rb'])
                    S.op('dve', lambda e, krr=krr: e.tensor_tensor(krr[:, 0:32], ra[:], rb[:], ALU.add),
                         ['ra', 'rb'], [krres])
                    return cqn, cqres, ckvn, ckres, krr, krres

                def stageXb(t, cqn, cqres, ckvn, ckres, krr, krres):
                    ts = slice(t * 128, (t + 1) * 128)
                    m5 = 7
                    cqT, cqTres = cqTs.next()
                    if m5 & 1:
                        for k in range(6):
                            self.tr(tpA[:, k * 128:(k + 1) * 128], cqn[:, k * 128:(k + 1) * 128], [cqres], [tAres])
                    if m5 & 2:
                        for k in range(2):
                            self.tr(tpA[:, 768 + k * 128:768 + (k + 1) * 128], ckvn[:, k * 128:(k + 1) * 128], [ckres],
                                    [tAres])
                    if m5 & 4:
                        self.tr(tpB[:, 0:128], krr[:], [krres], [tBres])
                    if m5 & 1:
                        self.act(cqT[:], tpA[:, 0:768].rearrange("p (k n) -> p k n", k=6), AF.Copy, [tAres], [cqTres])
                    if m5 & 2:
                        self.act(ckvT[:, :, ts], tpA[:, 768:1024].rearrange("p (k n) -> p k n", k=2), AF.Copy,
                                 [tAres], [('ckvT', t // 4)])
                    if m5 & 4:
                        S.op('dve', lambda e, ts=ts: e.tensor_copy(krT[0:32, ts], tpB[0:32, 0:128]), [tBres],
                             [('krT', t // 4)])
                    return cqT, cqTres

                def stageYa(t, cqT, cqTres):
                    qb, qbres = qbs.next()
                    ts = slice(t * 128, (t + 1) * 128)
                    for j in range(3):
                        Qj, Qres = Q[j]
                        for k in range(6):
                            self.mm(Qj[:], cqT[:, k, :], wuq[:, k, j * 512:(j + 1) * 512], k == 0, k == 5,
                                    [cqTres, ('wuq', k), ('wuq2', k)], [Qres])
                        self.act(qf[:, j * 512:(j + 1) * 512], Qj[:], AF.Copy, [Qres], ['qf'])
                    qv = qf[:].rearrange("p (h d) -> p h d", h=16)
                    qbv = qb[:].rearrange("p (h d) -> p h d", h=16)
                    csb = cs[:, t:t + 1, :].broadcast_to([128, 16, 32])
                    snb = sn[:, t:t + 1, :].broadcast_to([128, 16, 32])
                    S.op('dve', lambda e, qv=qv, csb=csb: e.tensor_tensor(ta[:], qv[:, :, 64:96], csb, ALU.mult),
                         ['qf', 'ropecs'], ['ta'])
                    S.op('dve', lambda e, qv=qv, snb=snb: e.tensor_tensor(tb[:, :, 0:16], qv[:, :, 80:96],
                                                                      snb[:, :, 0:16], ALU.mult),
                         ['qf', 'ropesn'], ['tb'])
                    S.op('dve', lambda e, qv=qv, snb=snb: e.tensor_tensor(tb[:, :, 16:32], qv[:, :, 64:80],
                                                                      snb[:, :, 16:32], ALU.mult),
                         ['qf', 'ropesn'], ['tb'])
                    S.op('pool', lambda e, qv=qv, qbv=qbv: e.tensor_copy(qbv[:, :, 0:64], qv[:, :, 0:64]), ['qf'], [qbres])
                    S.op('dve', lambda e, qbv=qbv: e.tensor_tensor(qbv[:, :, 64:96], ta[:], tb[:], ALU.add),
                         ['ta', 'tb'], [qbres])
                    return qb, qbres

                def stageYb(t, qb, qbres):
                    for hh in range(16):
                        tp_, tr_ = (tpA, tAres) if hh < 8 else (tpB, tBres)
                        self.tr(tp_[0:96, (hh % 8) * 128:(hh % 8 + 1) * 128], qb[:, hh * 96:(hh + 1) * 96], [qbres], [tr_])
                    tsub = t % 4
                    self.act(qst[:, 0:8, tsub * 128:(tsub + 1) * 128], tpA[0:96, :].rearrange("p (h n) -> p h n", h=8),
                             AF.Copy, [tAres], ['qst'])
                    S.op('dve', lambda e, tsub=tsub: e.tensor_copy(qst[:, 8:16, tsub * 128:(tsub + 1) * 128],
                                                                 tpB[0:96, :].rearrange("p (h n) -> p h n", h=8)),
                         [tBres], ['qst'])
                    if tsub == 3:
                        g = t // 4
                        self.dma(self.qTs[:, :, g * 512:(g + 1) * 512].rearrange("h r n -> r h n"), qst[:], ['qst'],
                                 [('qTs', g)])

                nt = DBG.get('mla_nt', NT)
                xa = {0: stageXa(0)}
                xb = {0: stageXb(0, *xa[0])}
                ya = {}
                for t in range(nt):
                    if t + 1 < nt:
                        xa[t + 1] = stageXa(t + 1)
                    ya[t] = stageYa(t, *xb[t])
                    if t + 1 < nt:
                        xb[t + 1] = stageXb(t + 1, *xa[t + 1])
                    if t >= 1:
                        stageYb(t - 1, *ya[t - 1])
                stageYb(nt - 1, *ya[nt - 1])
            S.barrier()
            with contextlib.ExitStack() as es2:
                self.alloc_attn_common(es2)
                if DBG.get('mla_stage1_only'):
                    return
                qThs = Rot('qTh', [self.sb(es2, f"qTh{i}", [96, SEQ], BF16) for i in range(2)])
                kThs = Rot('kTh', [self.sb(es2, f"kTh{i}", [96, SEQ], BF16) for i in range(2)])
                vaugs = [self.sb(es2, f"vaugm{i}", [128, NT, 128], BF16) for i in range(2)]
                S.op('pool', lambda e: e.memset(vaugs[0][:, :, 64:128], 1.0), [], [('vaugm1', 0)])
                S.op('pool', lambda e: e.memset(vaugs[1][:, :, 0:64], 1.0), [], [('vaugm1', 1)])
                vTz = [Rot(f'vTz{sl}', [self.sb(es2, f"vTz{sl}_{i}", [128, 512], BF16) for i in range(2)])
                       for sl in range(2)]
                for sl in range(2):
                    for i in range(2):
                        zr = slice(64, 128) if sl == 0 else slice(0, 64)
                        S.op('pool', lambda e, sl=sl, i=i, zr=zr: e.memset(vTz[sl].tiles[i][zr, :], 0.0), [],
                             [(f'vTz{sl}z', i)])
                scale = 96.0 ** -0.5
                for h in range(16):
                    slot = h % 2
                    qTh, qres = qThs.next()
                    self.dma(qTh[:], self.qTs[h], [('qTs', g) for g in range(8)], [qres])
                    kTh, kres = kThs.next()
                    va = vaugs[slot]
                    S.op('pool', lambda e, kTh=kTh: e.tensor_copy(kTh[64:96, :], krT[0:32, :]),
                         [('krT', g) for g in range(8)], [(kres, 'r')])
                    for c in range(8):
                        cs_ = slice(c * 512, (c + 1) * 512)
                        pj, pres = self.pjs.next()
                        for k in range(2):
                            self.mm(pj[:], wukv[:, k, h * 128:(h + 1) * 128], ckvT[:, k, cs_], k == 0, k == 1,
                                    [('wukv', k, h // 8), ('ckvT', c)], [pres])
                        self.act(kTh[0:64, cs_], pj[0:64, :], AF.Copy, [pres], [(kres, c)])
                        vt, vtres = vTz[slot].next()
                        zres = (f'vTz{slot}z', vtres[1])
                        if slot == 0:
                            S.op('dve', lambda e, vt=vt, pj=pj: e.tensor_copy(vt[0:64, :], pj[64:128, :]),
                                 [pres], [vtres])
                        else:
                            S.op('dve', lambda e, vt=vt, pj=pj: e.tensor_copy(vt[64:128, :], pj[64:128, :]),
                                 [pres], [vtres])
                        tp, tres = self.tps.next()
                        for jj in range(4):
                            self.tr(tp[:, jj * 128:(jj + 1) * 128], vt[:, jj * 128:(jj + 1) * 128], [vtres, zres], [tres])
                        tv = tp[:, 0:512].rearrange("p (a b) -> p a b", a=4)
                        vs = slice(0, 64) if slot == 0 else slice(64, 128)
                        S.op('dve', lambda e, va=va, tv=tv, c=c, vs=vs: e.tensor_copy(va[:, 4 * c:4 * c + 4, vs],
                                                                                  tv[:, :, vs]),
                             [tres, ('vaugm1', slot)], [('vaugm', slot, c)])
                    for c in range(8):
                        oa, ores = self.oas.next()
                        nk = 4 * c + 4
                        for kt in range(nk):
                            ks = slice(kt * 128, (kt + 1) * 128)
                            rd = [(kres, kt // 4), (kres, 'r'), qres]
                            if kt < 4 * c:
                                col0, n = 0, 512
                                qk = [(kTh[:, ks], qTh[:, c * 512:(c + 1) * 512], rd)]
                            else:
                                col0 = 128 * (kt - 4 * c)
                                n = 512 - col0
                                qk = [(kTh[:, ks], qTh[:, c * 512 + col0:(c + 1) * 512], rd),
                                      (self.ident[:], mtri[:, 0:n], ['ident', 'mtri'])]
                            after = None
                            if kt == nk - 1:
                                after = (lambda oa=oa, ores=ores, slot=slot, h=h, c=c: self.normalize_out(
                                    lambda rows: oa[rows, 0:512], ores, 512, slot, h // 2, c * 512))
                            self.attn_step(n, qk, scale, (oa[:, col0:512], va[:, kt, :],
                                                          [('vaugm', slot, kt // 4), ('vaugm1', slot)], ores,
                                                          kt == 0, kt == nk - 1), after)
                    self.attn_flush()
        S.barrier()

    def build(self):
        nc = self.nc
        S = self.S
        with contextlib.ExitStack() as es:
            self.ident = self.sb(es, "ident", [128, 128], BF16)
            self.dma(self.ident[:], self.C['ident'], [], ['ident'])
            self.epsc = self.sb(es, "epsc", [128, 1], F32)
            S.op('dve', lambda e: e.memset(self.epsc[:], EPS), [], ['epsc'])
            self.junk = Rot('junk', [self.sb(es, f"junk{i}", [128, DM], BF16) for i in range(2)])
            ps = lambda name, shape, dt: es.enter_context(nc.psum_tensor(name, shape, dt))
            self.tps = Rot('tp', [ps(f"tp{i}", [128, 1024], BF16) for i in range(2)])
            self.pjs = Rot('pj', [ps(f"pj{i}", [128, 512], F32) for i in range(2)])
            self.scs = Rot('sc', [ps(f"sc{i}", [128, 512], F32) for i in range(2)])
            self.oas = Rot('oa', [ps(f"oa{i}", [128, 512], F32) for i in range(2)])
            hsrc = self.x
            for n, li in enumerate(self.layers):
                last = (n == len(self.layers) - 1)
                with contextlib.ExitStack() as es2:
                    xnT = self.sb(es2, "xnT", [128, 8, SEQ], BF16)
                    with contextlib.ExitStack() as es3:
                        self.phase_norm(es3, hsrc, f'l{li}_attn_norm', xnT)
                    S.barrier()
                    npair = 8
                    if li == 3:
                        self.phase_swa(li, xnT)
                    elif li == 1:
                        npair = 4
                        self.phase_dilated(li, xnT)
                    elif li == 0:
                        self.phase_moba(li, xnT)
                    elif li == 2:
                        self.phase_mla(li, xnT)
                    else:
                        raise NotImplementedError
                self.phase_of(li, npair, hsrc, last)
                hsrc = self.hbuf
            S.barrier()
            S.emit()
        return nc


_CONSTS = None


def run(inputs, layers=(0, 1, 2, 3), final=True, cores=8, x_override=None):
    global _CONSTS
    if _CONSTS is None:
        _CONSTS = host_consts()
    b = Builder(layers, final)
    nc = b.build()
    x = np.ascontiguousarray(inputs['x'], dtype=np.float32) if x_override is None else x_override
    in_maps = []
    for c in range(cores):
        m = {'x': np.ascontiguousarray(x[c])}
        for name in b.used_inputs():
            m[name] = np.ascontiguousarray(inputs[name], dtype=np.float32)
        for name in CONST_SPECS:
            m['c_' + name] = _CONSTS[name]
        in_maps.append(m)
    res = run_bass_kernel_spmd(nc, in_maps, core_ids=list(range(cores)))
    return np.stack([np.asarray(r['y']) for r in res.results], axis=0)


def kernel(**inputs):
    out = run(inputs)
    return out.astype(np.float32)
```

```python
import contextlib
import numpy as np
import ml_dtypes
import concourse.bass as bass
import concourse.mybir as mybir
from concourse.bass_utils import run_bass_kernel_spmd

F32 = mybir.dt.float32
BF16 = mybir.dt.bfloat16
AF = mybir.ActivationFunctionType
ALU = mybir.AluOpType
AX = mybir.AxisListType
NPBF = ml_dtypes.bfloat16

SEQ = 4096
DM = 1024
NT = SEQ // 128
DFF = 4096
EPS = 1e-6
NEG = -30000.0

ENGS = ['pe', 'act', 'dve', 'pool', 'sp']
NDMASEM = 8
DBG = {}


class Op:
    __slots__ = ('fn', 'waits', 'key', 'idx', 'signal', 'isdma')

    def __init__(self, fn, key, idx, isdma):
        self.fn = fn
        self.waits = []
        self.key = key
        self.idx = idx
        self.signal = False
        self.isdma = isdma


class Sched:
    def __init__(self, nc):
        self.nc = nc
        self.streams = {e: [] for e in ENGS}
        self.keyops = {}
        self.seen = {e: {} for e in ENGS}
        self.res = {}
        self.dma_rr = {e: 0 for e in ENGS}
        self.alias = {}

    def _expand(self, lst):
        if not self.alias:
            return lst
        out = []
        for r in lst:
            out.extend(self.alias.get(r, (r,)))
        return out

    def _need(self, eng, op, tok):
        key, idx = tok
        if self.seen[eng].get(key, -1) >= idx:
            return
        self.seen[eng][key] = idx
        op.waits.append(tok)

    def _deps(self, eng, op, reads, writes, mykey, same_ok):
        for r in reads:
            st = self.res.get(r)
            if st is None:
                continue
            w = st[0]
            if w is not None and not (same_ok and w[0] == mykey):
                self._need(eng, op, w)
        for r in writes:
            st = self.res.get(r)
            if st is None:
                continue
            w = st[0]
            if w is not None and w[0] != mykey:
                self._need(eng, op, w)
            for k, i in st[1].items():
                if k != mykey:
                    self._need(eng, op, (k, i))

    def _commit(self, tok, reads, writes):
        for r in reads:
            st = self.res.get(r)
            if st is None:
                st = self.res[r] = [None, {}]
            st[1][tok[0]] = tok[1]
        for r in writes:
            self.res[r] = [tok, {}]

    def op(self, eng, fn, reads=(), writes=()):
        reads = self._expand(reads)
        writes = self._expand(writes)
        key = eng
        lst = self.keyops.setdefault(key, [])
        o = Op(fn, key, len(lst), False)
        self._deps(eng, o, reads, writes, key, same_ok=(eng == 'pe'))
        lst.append(o)
        self.streams[eng].append(o)
        self._commit((key, o.idx), reads, writes)
        return o

    def dma(self, q, fn, reads=(), writes=()):
        reads = self._expand(reads)
        writes = self._expand(writes)
        j = self.dma_rr[q]
        self.dma_rr[q] = (j + 1) % NDMASEM
        key = ('dma', q, j)
        lst = self.keyops.setdefault(key, [])
        o = Op(fn, key, len(lst), True)
        if lst:
            self._need(q, o, (key, len(lst) - 1))
        self._deps(q, o, reads, writes, key, same_ok=False)
        lst.append(o)
        self.streams[q].append(o)
        self._commit((key, o.idx), reads, writes)
        return o

    def barrier(self):
        toks = [(key, len(lst) - 1) for key, lst in self.keyops.items() if lst]
        for e in ENGS:
            o = Op(None, None, None, False)
            for t in toks:
                self._need(e, o, t)
            if o.waits:
                self.streams[e].append(o)

    def emit(self):
        nc = self.nc
        for e in ENGS:
            for o in self.streams[e]:
                for (key, idx) in o.waits:
                    self.keyops[key][idx].signal = True
        semval = {}
        for key, lst in self.keyops.items():
            c = 0
            for o in lst:
                if o.isdma:
                    c += 16
                    semval[(key, o.idx)] = c
                    o.signal = True
                elif o.signal:
                    c += 1
                    semval[(key, o.idx)] = c
            assert c < 60000, (key, c)
        keys = [k for k, l in self.keyops.items() if l]
        with contextlib.ExitStack() as es:
            sems = {}
            for k in keys:
                nm = 's_' + ('_'.join(str(x) for x in k) if isinstance(k, tuple) else k)
                sems[k] = es.enter_context(nc.semaphore(nm))
            block = es.enter_context(nc.Block())

            def run(e):
                def body(eng):
                    for o in self.streams[e]:
                        for tok in o.waits:
                            eng.wait_ge(sems[tok[0]], semval[tok])
                        if o.fn is None:
                            continue
                        ins = o.fn(eng)
                        if o.signal:
                            ins.then_inc(sems[o.key], 16 if o.isdma else 1)
                return body
            if self.streams['pe']:
                block.tensor(run('pe'))
            if self.streams['act']:
                block.scalar(run('act'))
            if self.streams['dve']:
                block.vector(run('dve'))
            if self.streams['pool']:
                block.gpsimd(run('pool'))
            if self.streams['sp']:
                block.sync(run('sp'))


class Rot:
    def __init__(self, name, tiles):
        self.name = name
        self.tiles = tiles
        self.i = 0

    def next(self):
        j = self.i % len(self.tiles)
        self.i += 1
        return self.tiles[j], (self.name, j)


def alibi(n):
    return (2.0 ** (-8.0 * np.arange(1, n + 1, dtype=np.float64) / n))


def split_hi_lo(v):
    v = v.astype(np.float32)
    hi = v.astype(NPBF)
    lo = (v - hi.astype(np.float32)).astype(NPBF)
    return hi, lo


def band_bias(slope_eff, W, width=256):
    k = np.arange(128)[:, None].astype(np.float64)
    col = np.arange(width)[None, :].astype(np.float64)
    diff = col - k
    val = -slope_eff * diff * 8.0
    val = np.where((diff >= 0) & (diff < W), val, NEG)
    return val.astype(np.float32)


def host_consts():
    c = {}
    c['ident'] = np.eye(128, dtype=np.float32).astype(NPBF)
    sl = alibi(16)
    t = np.zeros((128, 16, 2, 256), NPBF)
    for h in range(16):
        hi, lo = split_hi_lo(band_bias(sl[h], 128))
        t[:, h, 0], t[:, h, 1] = hi, lo
    c['bsw'] = t
    sl24 = alibi(24)
    dils = (1, 4, 16)
    t = np.zeros((128, 24, 2, 256), NPBF)
    for g in range(3):
        for hs in range(8):
            hi, lo = split_hi_lo(band_bias(sl24[g * 8 + hs] * dils[g], 129))
            t[:, g * 8 + hs, 0], t[:, g * 8 + hs, 1] = hi, lo
    c['bdl'] = t
    t = np.zeros((128, 16, 2, 256), NPBF)
    for h in range(16):
        hi, lo = split_hi_lo(band_bias(sl[h], 10 ** 9))
        t[:, h, 0], t[:, h, 1] = hi, lo
    c['bmo'] = t
    p = np.arange(128)[:, None, None].astype(np.float64)
    j = np.arange(2)[None, :, None].astype(np.float64)
    c['tqm'] = (-sl[None, None, :] * 8.0 * (j * 128 + p) - 30000.0).astype(np.float32)
    idx = np.arange(31)[None, None, :].astype(np.float64)
    c['dtab'] = np.broadcast_to((-sl[None, :, None] * 2048.0 * (15 - idx)), (128, 16, 31)).astype(np.float32).copy()
    hi, lo = split_hi_lo(np.broadcast_to((sl * 8.0)[None, :], (128, 16)).copy())
    c['slp'] = np.stack([hi, lo], axis=-1)
    X = np.zeros((128, 16, 2, 128), np.float32)
    for n in range(16):
        X[n, n] = 1.0
        X[16 + n, n] = 1.0
    for half in range(2):
        X[32, :, half, :] = np.arange(128)[None, :] + 128 * half
        X[33, :, half, :] = np.arange(128)[None, :] + 128 * half
    c['xsel'] = X.astype(NPBF)
    XT = np.zeros((64, SEQ), np.float32)
    pos = np.arange(SEQ)
    for r in range(16):
        XT[r] = (pos // 256 == r)
        XT[16 + r] = (pos // 256 == r)
    XT[32] = pos % 256
    XT[33] = pos % 256
    c['xselT'] = XT.astype(NPBF)
    def split3(v):
        v = v.astype(np.float64)
        h1 = v.astype(np.float32).astype(NPBF)
        r1 = v - h1.astype(np.float64)
        h2 = r1.astype(np.float32).astype(NPBF)
        r2 = r1 - h2.astype(np.float64)
        h3 = r2.astype(np.float32).astype(NPBF)
        return [h1, h2, h3]
    pidx = np.arange(SEQ, dtype=np.float64)
    QA = np.zeros((24, 9, SEQ), NPBF)
    for g in range(3):
        for hs in range(8):
            s8 = sl24[g * 8 + hs] * dils[g] * 8.0
            rows = split3(-s8 * pidx) + split3(np.full(SEQ, s8 * 64.0)) + split3(np.full(SEQ, s8))
            for r in range(9):
                QA[g * 8 + hs, r] = rows[r]
    c['dlqa'] = QA
    KA = np.zeros((9, SEQ), np.float32)
    KA[0:3] = 1.0
    KA[3:6] = np.floor(pidx / 64)[None, :]
    KA[6:9] = (pidx % 64)[None, :]
    c['dlka'] = KA.astype(NPBF)
    kk = np.arange(128)[:, None]
    cc = np.arange(256)[None, :]
    dd = cc - kk
    c['bandm'] = np.where((dd >= 0) & (dd <= 128), 0.0, NEG).astype(np.float32).astype(NPBF)
    k = np.arange(128)[:, None]
    col = np.arange(512)[None, :]
    c['mtri'] = np.where(col >= k, 0.0, NEG).astype(np.float32).astype(NPBF)
    inv = 10000.0 ** (-np.arange(0, 32, 2, dtype=np.float64) / 32)
    pos = (np.arange(32)[None, :] * 128 + np.arange(128)[:, None]).astype(np.float64)
    ang = pos[:, :, None] * inv[None, None, :]
    ang = ang.astype(np.float32).astype(np.float64)
    cos, sin = np.cos(ang), np.sin(ang)
    c['ropecs'] = np.concatenate([cos, cos], axis=-1).astype(np.float32)
    c['ropesn'] = np.concatenate([-sin, sin], axis=-1).astype(np.float32)
    return c


CONST_SPECS = {
    'ident': ([128, 128], BF16), 'bsw': ([128, 16, 2, 256], BF16), 'bdl': ([128, 24, 2, 256], BF16),
    'bmo': ([128, 16, 2, 256], BF16), 'tqm': ([128, 2, 16], F32), 'dtab': ([128, 16, 31], F32),
    'slp': ([128, 16, 2], BF16), 'xsel': ([128, 16, 2, 128], BF16), 'xselT': ([64, SEQ], BF16), 'dlqa': ([24, 9, SEQ], BF16), 'dlka': ([9, SEQ], BF16), 'bandm': ([128, 256], BF16), 'mtri': ([128, 512], BF16),
    'ropecs': ([128, 32, 32], F32), 'ropesn': ([128, 32, 32], F32),
}

WEIGHT_SPECS = [
    ('l0_attn_norm', [1024]), ('l0_w_qkv', [1024, 3072]), ('l0_w_o', [1024, 1024]), ('l0_mlp_norm', [1024]),
    ('l0_w_up', [1024, 4096]), ('l0_w_down', [4096, 1024]),
    ('l1_attn_norm', [1024]), ('l1_w_qkv', [1024, 4608]), ('l1_w_o', [512, 1024]), ('l1_mlp_norm', [1024]),
    ('l1_w_up', [1024, 4096]), ('l1_w_down', [4096, 1024]),
    ('l2_attn_norm', [1024]), ('l2_w_dkv', [1024, 1056]), ('l2_q_norm', [768]), ('l2_w_uq', [768, 1536]),
    ('l2_kv_norm', [256]), ('l2_w_ukv', [256, 2048]), ('l2_w_o', [1024, 1024]), ('l2_mlp_norm', [1024]),
    ('l2_w_up', [1024, 4096]), ('l2_w_down', [4096, 1024]),
    ('l3_attn_norm', [1024]), ('l3_w_qkv', [1024, 1280]), ('l3_sinks', [16]), ('l3_w_o', [1024, 1024]),
    ('l3_mlp_norm', [1024]), ('l3_w_up', [1024, 4096]), ('l3_w_down', [4096, 1024]),
    ('final_norm', [1024]),
]


class Builder:
    def __init__(self, layers=(0, 1, 2, 3), final=True):
        self.layers = tuple(layers)
        self.final = final
        nc = self.nc = bass.Bass("TRN2", target_bir_lowering=False)
        self.S = Sched(nc)
        self.x = nc.dram_tensor("x", [SEQ, DM], F32, kind="ExternalInput").ap()
        self.W = {}
        for name, shape in WEIGHT_SPECS:
            if name == 'final_norm' or int(name[1]) in self.layers:
                self.W[name] = nc.dram_tensor(name, shape, F32, kind="ExternalInput").ap()
        self.C = {}
        for name, (shape, dt) in CONST_SPECS.items():
            self.C[name] = nc.dram_tensor("c_" + name, shape, dt, kind="ExternalInput").ap()
        self.y = nc.dram_tensor("y", [SEQ, DM], F32, kind="ExternalOutput").ap()
        self.hbuf = nc.dram_tensor("hbuf", [SEQ, DM], F32, kind="Internal").ap()
        self.oTs = nc.dram_tensor("oTs", [8, 128, SEQ], BF16, kind="Internal").ap()
        self.qTs = nc.dram_tensor("qTs", [16, 96, SEQ], BF16, kind="Internal").ap()
        self.cast_rr = 0
        for c in range(8):
            self.S.alias[('xnT', c)] = [('xnTs', c, 0), ('xnTs', c, 1)]

    def used_inputs(self):
        return list(self.W.keys())

    def sb(self, es, name, shape, dt):
        self.uid = getattr(self, 'uid', 0) + 1
        return es.enter_context(self.nc.sbuf_tensor(f"{name}_{self.uid}", shape, dt))

    def mm(self, out, lhsT, rhs, start, stop, reads, writes, skip=False):
        self.S.op('pe', lambda e: e.matmul(out, lhsT=lhsT, rhs=rhs, start=start, stop=stop,
                                           skip_group_check=skip), reads, writes)

    def tr(self, out, in_, reads, writes):
        ident = self.ident
        self.S.op('pe', lambda e: e.transpose(out, in_, ident[:]), list(reads) + ['ident'], writes)

    def act(self, out, in_, func, reads, writes, scale=1.0, bias=None, accum=None):
        kw = {}
        if bias is not None:
            kw['bias'] = bias
        if accum is not None:
            kw['accum_out'] = accum
        self.S.op('act', lambda e: e.activation(out=out, in_=in_, func=func, scale=scale, **kw), reads, writes)

    def dma(self, out, in_, reads, writes, q='sp'):
        self.S.dma(q, lambda e: e.dma_start(out=out, in_=in_), reads, writes)

    def load_cast(self, dst, src, n, reads_src=(), wres=None, shape3=None, engs=('dve', 'pool')):
        stg, sres = self.stg.next()
        sv = stg[:, 0:n]
        if shape3 is not None:
            sv = sv.rearrange("p (a b) -> p a b", a=shape3[0])
        self.dma(sv, src, list(reads_src), [sres])
        eng = engs[self.cast_rr % len(engs)]
        self.cast_rr += 1
        if eng == 'act':
            self.act(dst, sv, AF.Copy, [sres], [wres])
        else:
            self.S.op(eng, lambda e: e.tensor_copy(dst, sv), [sres], [wres])

    def rms_stats(self, src_ap, ss_ap, reads, ssres):
        junk, jres = self.junk.next()
        self.act(junk[:], src_ap, AF.Square, reads, [jres, ssres], accum=ss_ap)

    def rms_rstd(self, ss_ap, rstd_ap, ssres, rres, n_feat):
        self.act(rstd_ap, ss_ap, AF.Ln, [ssres, 'epsc'], [rres], scale=1.0 / n_feat, bias=self.epsc[:, 0:1])
        self.act(rstd_ap, rstd_ap, AF.Exp, [rres], [rres], scale=-0.5)

    def phase_norm(self, es, src_dram, gname, xnT):
        S = self.S
        nc = self.nc
        gbc = self.sb(es, "gbc", [128, DM], F32)
        self.dma(gbc[:], self.W[gname].partition_broadcast(128), [], ['gbc'])
        hts = Rot('ht', [self.sb(es, f"ht{i}", [128, DM], F32) for i in range(4)])
        xns = Rot('xn', [self.sb(es, f"xn{i}", [128, DM], BF16) for i in range(3)])
        ss = self.sb(es, "ssn", [128, NT], F32)
        rs = self.sb(es, "rsn", [128, NT], F32)
        pend = None
        for t in range(NT + 1):
            cur = None
            if t < NT:
                ht, hres = hts.next()
                self.dma(ht[:], src_dram[t * 128:(t + 1) * 128, :], [('h', t // 4)], [hres])
                self.rms_stats(ht[:], ss[:, t:t + 1], [hres], ('ssn', t))
                self.rms_rstd(ss[:, t:t + 1], rs[:, t:t + 1], ('ssn', t), ('rsn', t), DM)
                xn, xres = xns.next()
                S.op('dve', lambda e, xn=xn, ht=ht, t=t: e.scalar_tensor_tensor(
                    out=xn[:], in0=ht[:], scalar=rs[:, t:t + 1], in1=gbc[:], op0=ALU.mult, op1=ALU.mult),
                    [hres, ('rsn', t), 'gbc'], [xres])
                tp, tres = self.tps.next()
                for k in range(8):
                    self.tr(tp[:, k * 128:(k + 1) * 128], xn[:, k * 128:(k + 1) * 128], [xres], [tres])
                cur = (t, tp, tres)
            if pend is not None:
                pt_, tp_, tres_ = pend
                dst = xnT[:, :, pt_ * 128:(pt_ + 1) * 128]
                src = tp_[:].rearrange("p (k n) -> p k n", k=8)
                if pt_ % 2 == 0:
                    self.act(dst, src, AF.Copy, [tres_], [('xnTs', pt_ // 4, 0)])
                else:
                    S.op('dve', lambda e, dst=dst, src=src: e.tensor_copy(dst, src), [tres_], [('xnTs', pt_ // 4, 1)])
            pend = cur

    def phase_of(self, li, npair, hsrc, last):
        S = self.S
        W = self.W
        fin = last and self.final
        with contextlib.ExitStack() as es:
            wup = self.sb(es, "wup", [128, 8, DFF], BF16)
            wdn = self.sb(es, "wdn", [128, 32, DM], BF16)
            self.stg = Rot('stg', [self.sb(es, f"stgf{i}", [128, 1024], F32) for i in range(4)])
            wupv = W[f'l{li}_w_up'].rearrange("(k p) n -> p k n", p=128)
            wdnv = W[f'l{li}_w_down'].rearrange("(k p) n -> p k n", p=128)
            pieces = []
            for qq in range(4):
                for k in range(8):
                    pieces.append((wup[:, k, qq * 1024:(qq + 1) * 1024], wupv[:, k, qq * 1024:(qq + 1) * 1024],
                                   ('wup', k, qq)))
            for c in range(32):
                pieces.append((wdn[:, c, :], wdnv[:, c, :], ('wdn', c)))

            def emit_pieces(n):
                for _ in range(n):
                    if pieces:
                        dst, src, wres = pieces.pop(0)
                        self.load_cast(dst, src, 1024, wres=wres, engs=('act',))

            with contextlib.ExitStack() as es1:
                wo = self.sb(es1, "wo", [128, npair, DM], BF16)
                wov = W[f'l{li}_w_o'].rearrange("(k p) n -> p k n", p=128)
                for k in range(npair):
                    self.load_cast(wo[:, k, :], wov[:, k, :], DM, wres=('wo', k), engs=('act',))
                oTg = Rot('oTg', [self.sb(es1, f"oTg{i}", [128, npair, 256], BF16) for i in range(2)])
                hgs = Rot('hgo', [self.sb(es1, f"hgo{i}", [128, 2, DM], F32) for i in range(2)])

                def loads(g):
                    og, ores = oTg.next()
                    hg, hres = hgs.next()
                    self.dma(og[:], self.oTs[0:npair, :, g * 256:(g + 1) * 256].rearrange("a p n -> p a n"),
                             [('oTs', g // 2)], [ores])
                    self.dma(hg[:], hsrc[g * 256:(g + 1) * 256, :].rearrange("(t p) n -> p t n", p=128),
                             [('h', g // 2)], [hres])
                    return og, ores, hg, hres
                nxt = loads(0)
                for g in range(16):
                    og, ores, hg, hres = nxt
                    if g + 1 < 16:
                        nxt = loads(g + 1)
                    for t in range(2):
                        for hf in range(2):
                            pj, pres = self.pjs.next()
                            for k in range(npair):
                                self.mm(pj[:], og[:, k, t * 128:(t + 1) * 128], wo[:, k, hf * 512:(hf + 1) * 512],
                                        k == 0, k == npair - 1, [ores, ('wo', k)], [pres])
                            S.op('dve', lambda e, hg=hg, pj=pj, t=t, hf=hf: e.tensor_tensor(
                                hg[:, t, hf * 512:(hf + 1) * 512], pj[:], hg[:, t, hf * 512:(hf + 1) * 512], ALU.add),
                                [pres, hres], [hres])
                    self.dma(self.hbuf[g * 256:(g + 1) * 256, :].rearrange("(t p) n -> p t n", p=128), hg[:],
                             [hres], [('h', g // 2)])
                    emit_pieces(4)
                emit_pieces(64)
            S.barrier()
            with contextlib.ExitStack() as es2:
                gbc = self.sb(es2, "gbcf", [128, DM], F32)
                self.dma(gbc[:], W[f'l{li}_mlp_norm'].partition_broadcast(128), [], ['gbcf'])
                if fin:
                    gfin = self.sb(es2, "gfin", [128, DM], F32)
                    self.dma(gfin[:], W['final_norm'].partition_broadcast(128), [], ['gfin'])
                hgs = Rot('hg', [self.sb(es2, f"hg{i}", [128, 2, DM], F32) for i in range(2)])
                xns = Rot('xnf', [self.sb(es2, f"xnf{i}", [128, DM], BF16) for i in range(2)])
                xTs = Rot('xT', [self.sb(es2, f"xT{i}", [128, 8, 256], BF16) for i in range(2)])
                uTs = Rot('uT', [self.sb(es2, f"uT{i}", [128, 8, 256], BF16) for i in range(2)])
                rl = Rot('rl', [self.sb(es2, f"rl{i}", [128, 256], F32) for i in range(3)])
                ss = self.sb(es2, "ssf", [128, NT], F32)
                rs = self.sb(es2, "rsf", [128, NT], F32)
                ssl = self.sb(es2, "ssl", [128, NT], F32)
                rsl = self.sb(es2, "rsl", [128, NT], F32)

                def load(g):
                    hg, hres = hgs.next()
                    self.dma(hg[:], self.hbuf[g * 256:(g + 1) * 256, :].rearrange("(t p) n -> p t n", p=128),
                             [('h', g // 2)], [hres])
                    return hg, hres

                def prep(g, hg, hres):
                    x2, x2res = xTs.next()
                    for t in range(2):
                        i = g * 2 + t
                        self.rms_stats(hg[:, t, :], ss[:, i:i + 1], [hres], ('ssf', i))
                        self.rms_rstd(ss[:, i:i + 1], rs[:, i:i + 1], ('ssf', i), ('rsf', i), DM)
                        xn, xres = xns.next()
                        S.op('dve', lambda e, xn=xn, hg=hg, t=t, i=i: e.scalar_tensor_tensor(
                            out=xn[:], in0=hg[:, t, :], scalar=rs[:, i:i + 1], in1=gbc[:], op0=ALU.mult, op1=ALU.mult),
                            [hres, ('rsf', i), 'gbcf'], [xres])
                        tp, tres = self.tps.next()
                        for k in range(8):
                            self.tr(tp[:, k * 128:(k + 1) * 128], xn[:, k * 128:(k + 1) * 128], [xres], [tres])
                        self.act(x2[:, :, t * 128:(t + 1) * 128], tp[:].rearrange("p (k n) -> p k n", k=8), AF.Copy,
                                 [tres], [x2res])
                    return x2, x2res

                cur = load(0)
                curx = prep(0, *cur)
                for g in range(16):
                    hg, hres = cur
                    x2, x2res = curx
                    if g + 1 < 16:
                        nxt = load(g + 1)
                    for q in range(4):
                        uT, ures = uTs.next()
                        for cc in range(8):
                            c = q * 8 + cc
                            sc, sres = self.scs.next()
                            for k in range(8):
                                self.mm(sc[:, 0:256], wup[:, k, c * 128:(c + 1) * 128], x2[:, k, :], k == 0, k == 7,
                                        [x2res, ('wup', k, c // 8)], [sres])
                            r, rres = rl.next()
                            self.act(r[:], sc[:, 0:256], AF.Relu, [sres], [rres])
                            S.op('dve', lambda e, r=r, cc=cc, uT=uT: e.tensor_tensor(uT[:, cc, :], r[:], r[:], ALU.mult),
                                 [rres], [(ures, cc)])
                        if q == 1 and g + 1 < 16:
                            nxtx = prep(g + 1, *nxt)
                        for t in range(2):
                            for hf in range(2):
                                pj, pres = self.pjs.next()
                                for cc in range(8):
                                    c = q * 8 + cc
                                    self.mm(pj[:], uT[:, cc, t * 128:(t + 1) * 128], wdn[:, c, hf * 512:(hf + 1) * 512],
                                            cc == 0, cc == 7, [(ures, cc), ('wdn', c)], [pres])
                                S.op('dve', lambda e, hg=hg, pj=pj, t=t, hf=hf: e.tensor_tensor(
                                    hg[:, t, hf * 512:(hf + 1) * 512], pj[:], hg[:, t, hf * 512:(hf + 1) * 512],
                                    ALU.add), [pres, hres], [hres])
                    rows = slice(g * 256, (g + 1) * 256)
                    if fin:
                        for t in range(2):
                            i = g * 2 + t
                            self.rms_stats(hg[:, t, :], ssl[:, i:i + 1], [hres], ('ssl', i))
                            self.rms_rstd(ssl[:, i:i + 1], rsl[:, i:i + 1], ('ssl', i), ('rsl', i), DM)
                            S.op('dve', lambda e, hg=hg, i=i, t=t: e.scalar_tensor_tensor(
                                out=hg[:, t, :], in0=hg[:, t, :], scalar=rsl[:, i:i + 1], in1=gfin[:],
                                op0=ALU.mult, op1=ALU.mult), [hres, ('rsl', i), 'gfin'], [hres])
                    dst = self.y if last else self.hbuf
                    self.dma(dst[rows, :].rearrange("(t p) n -> p t n", p=128), hg[:], [hres],
                             ['y'] if last else [('h', g // 2)])
                    if g + 1 < 16:
                        cur, curx = nxt, nxtx
        S.barrier()

    def normalize_out(self, src, sres, ncols, slot, pair, col0, extra=None, use_act=False):
        S = self.S
        orow = slice(0, 64) if slot == 0 else slice(64, 128)
        drow = slice(64, 128) if slot == 0 else slice(0, 64)
        rc, rres = self.rcs.next()
        if use_act:
            if extra is not None:
                sc_ap, sc_res = extra
                self.act(rc[orow, 0:ncols], src(drow), AF.Ln, [sres, sc_res], [rres], bias=sc_ap(orow))
            else:
                self.act(rc[orow, 0:ncols], src(drow), AF.Ln, [sres], [rres])
            self.act(rc[orow, 0:ncols], rc[orow, 0:ncols], AF.Exp, [rres], [rres], scale=-1.0)
        else:
            S.op('dve', lambda e: e.tensor_copy(rc[orow, 0:ncols], src(drow)), [sres], [rres])
            if extra is not None:
                sc_ap, sc_res = extra
                S.op('dve', lambda e: e.tensor_scalar(rc[orow, 0:ncols], rc[orow, 0:ncols], sc_ap(orow), None,
                                                      ALU.add), [rres, sc_res], [rres])
            S.op('dve', lambda e: e.reciprocal(rc[orow, 0:ncols], rc[orow, 0:ncols]), [rres], [rres])
        on, onres = self.ons.next()
        S.op('dve', lambda e: e.tensor_tensor(on[orow, 0:ncols], src(orow), rc[orow, 0:ncols], ALU.mult),
             [sres, rres], [onres])
        self.dma(self.oTs[pair, orow, col0:col0 + ncols], on[orow, 0:ncols], [onres], [('oTs', col0 // 512)])

    def alloc_attn_common(self, es, nslots=4):
        self.pts = Rot('pt', [self.sb(es, f"pt{i}", [128, 512], BF16) for i in range(6)])
        self.rcs = Rot('rc', [self.sb(es, f"rc{i}", [128, 512], F32) for i in range(3)])
        self.ons = Rot('on', [self.sb(es, f"on{i}", [128, 512], BF16) for i in range(2)])
        self.stg = Rot('stg', [self.sb(es, f"stga{i}", [128, 1024], F32) for i in range(3)])
        self.pending = []
        t0, t1 = self.scs.tiles
        p0, p1 = self.pjs.tiles
        if nslots == 4:
            self.scslots = [(t0, 0, ('sc', 0)), (t1, 0, ('sc', 1)), (p0, 0, ('pj', 0)), (p1, 0, ('pj', 1))]
        else:
            self.scslots = [(t0, 0, ('sc', 0)), (t1, 0, ('sc', 1)), (p0, 0, ('pj', 0))]
        self.skew = 2
        self.sci = 0

    def attn_step(self, n, qk_list, scale, pv, after=None):
        tile, off, sres = self.scslots[self.sci % len(self.scslots)]
        self.sci += 1
        sc = tile[:, off:off + n]
        for i, (lhsT, rhs, reads) in enumerate(qk_list):
            self.mm(sc, lhsT, rhs, i == 0, i == len(qk_list) - 1, reads, [sres])
        pt, ptres = self.pts.next()
        self.act(pt[:, 0:n], sc, AF.Exp, [sres], [ptres], scale=scale)
        self.pending.append((pv, pt, ptres, n, after))
        while len(self.pending) > self.skew:
            self._attn_pop()

    def _attn_pop(self):
        pv, pt, ptres, n, after = self.pending.pop(0)
        out_ap, lhsT, reads, ores, first, last = pv
        self.mm(out_ap, lhsT, pt[:, 0:n], first, last, list(reads) + [ptres], [ores], skip=True)
        if after is not None:
            after()

    def attn_flush(self):
        while self.pending:
            self._attn_pop()

    def phase_swa(self, li, xnT):
        S = self.S
        W = self.W
        with contextlib.ExitStack() as es:
            self.alloc_attn_common(es)
            bsw = self.sb(es, "bsw", [128, 16, 2, 256], BF16)
            self.dma(bsw[:], self.C['bsw'], [], ['bsw'])
            esink = self.sb(es, "esink", [128, 16], F32)
            self.dma(esink[:], W[f'l{li}_sinks'].partition_broadcast(128), [], ['esink'])
            self.act(esink[:], esink[:], AF.Exp, ['esink'], ['esink'])
            wkv = self.sb(es, "wkv", [128, 8, 256], BF16)
            wqs = Rot('wq', [self.sb(es, f"wq{i}", [128, 8, 128], BF16) for i in range(2)])
            kTv = self.sb(es, "kTv", [128, 2, 2, SEQ], BF16)
            vaug = self.sb(es, "vaug", [128, NT, 2, 2, 128], BF16)
            qTs = Rot('qT', [self.sb(es, f"qT{i}", [128, SEQ], BF16) for i in range(2)])
            wv = W[f'l{li}_w_qkv'].rearrange("(k p) n -> p k n", p=128)
            self.load_cast(wkv[:, :, 0:128], wv[:, :, 1024:1152], 1024, wres='wkv', shape3=(8, 128))
            self.load_cast(wkv[:, :, 128:256], wv[:, :, 1152:1280], 1024, wres='wkv2', shape3=(8, 128))
            S.op('pool', lambda e: e.memset(vaug[:, :, :, 0, 64:128], 1.0), [], ['vaug1'])
            S.op('pool', lambda e: e.memset(vaug[:, :, :, 1, 0:64], 1.0), [], ['vaug1'])
            for kvh in range(2):
                S.op('pool', lambda e, kvh=kvh: e.memset(kTv[64:128, kvh, 0, :], 0.0), [], ['kTz'])
                S.op('pool', lambda e, kvh=kvh: e.memset(kTv[0:64, kvh, 1, :], 0.0), [], ['kTz'])
            for c in range(8):
                pj, pres = self.pjs.next()
                cs = slice(c * 512, (c + 1) * 512)
                for k in range(8):
                    self.mm(pj[:], wkv[:, k, 0:128], xnT[:, k, cs], k == 0, k == 7, ['wkv', ('xnT', c)], [pres])
                self.act(kTv[0:64, 0, 0, cs], pj[0:64, :], AF.Copy, [pres, 'kTz'], [('kT', c)])
                self.act(kTv[64:128, 0, 1, cs], pj[0:64, :], AF.Copy, [pres, 'kTz'], [('kT', c)])
                S.op('dve', lambda e, pj=pj, cs=cs: e.tensor_copy(kTv[64:128, 1, 1, cs], pj[64:128, :]),
                     [pres, 'kTz'], [('kT', c)])
                S.op('dve', lambda e, pj=pj, cs=cs: e.tensor_copy(kTv[0:64, 1, 0, cs], pj[64:128, :]),
                     [pres, 'kTz'], [('kT', c)])
            for t in range(NT):
                pj, pres = self.pjs.next()
                for k in range(8):
                    self.mm(pj[:, 0:128], xnT[:, k, t * 128:(t + 1) * 128], wkv[:, k, 128:256], k == 0, k == 7,
                            ['wkv2', ('xnT', t // 4)], [pres])
                for kvh in range(2):
                    S.op('dve', lambda e, pj=pj, t=t, kvh=kvh: e.tensor_copy(
                        vaug[:, t, kvh, 0, 0:64], pj[:, kvh * 64:(kvh + 1) * 64]), [pres, 'vaug1'], [('vaug', t)])
                    S.op('dve', lambda e, pj=pj, t=t, kvh=kvh: e.tensor_copy(
                        vaug[:, t, kvh, 1, 64:128], pj[:, kvh * 64:(kvh + 1) * 64]), [pres, 'vaug1'], [('vaug', t)])
            def wq_load(p):
                wq, wqres = wqs.next()
                self.load_cast(wq[:], wv[:, :, p * 128:(p + 1) * 128], 1024, wres=wqres, shape3=(8, 128), engs=('pool',))
                return wq, wqres
            nextwq = wq_load(0)
            for p in range(8):
                wq, wqres = nextwq
                if p + 1 < 8:
                    nextwq = wq_load(p + 1)
                qT, qres = qTs.next()
                for c in range(8):
                    pj, pres = self.pjs.next()
                    for k in range(8):
                        self.mm(pj[:], wq[:, k, :], xnT[:, k, c * 512:(c + 1) * 512], k == 0, k == 7,
                                [wqres, ('xnT', c)], [pres])
                    self.act(qT[:, c * 512:(c + 1) * 512], pj[:], AF.Copy, [pres], [(qres, c)])
                for slot in range(2):
                    h = 2 * p + slot
                    kvh = h // 8
                    for c in range(8):
                        oa, ores = self.oas.next()
                        kts = [kt for kt in range(4 * c - 1, 4 * c + 4) if kt >= 0]
                        for si, kt in enumerate(kts):
                            qts = [qt for qt in (kt, kt + 1) if 4 * c <= qt <= 4 * c + 3]
                            col0 = (qts[0] - 4 * c) * 128
                            n = 128 * len(qts)
                            boff = 0 if qts[0] == kt else 128
                            qk = [(kTv[:, kvh, slot, kt * 128:(kt + 1) * 128], qT[:, c * 512 + col0:c * 512 + col0 + n],
                                   [('kT', kt // 4), 'kTz', (qres, c)]),
                                  (self.ident[:], bsw[:, h, 0, boff:boff + n], ['ident', 'bsw']),
                                  (self.ident[:], bsw[:, h, 1, boff:boff + n], ['ident', 'bsw'])]
                            last = si == len(kts) - 1
                            after = None
                            if last:
                                after = (lambda oa=oa, ores=ores, slot=slot, p=p, c=c, h=h: self.normalize_out(
                                    lambda rows: oa[rows, 0:512], ores, 512, slot, p, c * 512,
                                    extra=(lambda rows: esink[rows, h:h + 1], 'esink'), use_act=True))
                            self.attn_step(n, qk, 0.125, (oa[:, col0:col0 + n], vaug[:, kt, kvh, slot, :],
                                                          [('vaug', kt), 'vaug1'], ores, si == 0, last), after)
                self.attn_flush()
        S.barrier()

    def alloc_pair(self, es):
        S = self.S
        self.wqkv = Rot('wqkv', [self.sb(es, f"wqkv{i}", [128, 8, 384], BF16) for i in range(2)])
        self.qT = self.sb(es, "qTp", [128, SEQ], BF16)
        self.kTz = self.sb(es, "kTz", [128, 2, SEQ], BF16)
        self.vaug = self.sb(es, "vaugp", [128, NT, 2, 128], BF16)
        self.vTs = Rot('vT', [self.sb(es, f"vT{i}", [128, 512], BF16) for i in range(2)])
        S.op('pool', lambda e: e.memset(self.kTz[64:128, 0, :], 0.0), [], ['kTzz'])
        S.op('pool', lambda e: e.memset(self.kTz[0:64, 1, :], 0.0), [], ['kTzz'])
        S.op('pool', lambda e: e.memset(self.vaug[:, :, 0, 64:128], 1.0), [], ['vaug1'])
        S.op('pool', lambda e: e.memset(self.vaug[:, :, 1, 0:64], 1.0), [], ['vaug1'])

    def pair_load(self, wv, qc, kc, vc):
        w, wres = self.wqkv.next()
        for j, c0 in enumerate((qc, kc, vc)):
            self.load_cast(w[:, :, j * 128:(j + 1) * 128], wv[:, :, c0:c0 + 128], 1024, wres=(wres, j), shape3=(8, 128),
                           engs=('pool',))
        return w, wres

    def pair_proj(self, wl, xnT, colmap=None):
        S = self.S
        w, wres = wl
        qT, kTz, vaug = self.qT, self.kTz, self.vaug
        for c in range(8):
            cs = slice(c * 512, (c + 1) * 512)
            xres = ('xnT', c) if colmap is None else 'xnTall'
            outs = []
            for j in range(3):
                pj, pres = self.pjs.next()
                for k in range(8):
                    rhs = xnT[:, k, cs] if colmap is None else colmap(k, c)
                    o = pj[:] if len(rhs.shape) == 2 else pj[:].rearrange("p (a b) -> p a b", a=rhs.shape[1])
                    self.mm(o, w[:, k, j * 128:(j + 1) * 128], rhs, k == 0, k == 7,
                            [(wres, j)] + ([xres] if colmap is None else [('xnT', cc) for cc in range(8)]), [pres])
                if j == 0:
                    self.act(qT[:, cs], pj[:], AF.Copy, [pres], [('qT', c)])
                elif j == 1:
                    self.act(kTz[0:64, 0, cs], pj[0:64, :], AF.Copy, [pres, 'kTzz'], [('kT', c)])
                    S.op('dve', lambda e, pj=pj, cs=cs: e.tensor_copy(kTz[64:128, 1, cs], pj[64:128, :]),
                         [pres, 'kTzz'], [('kT', c)])
                else:
                    vT, vres = self.vTs.next()
                    self.act(vT[:], pj[:], AF.Copy, [pres], [vres])
                    tp, tres = self.tps.next()
                    for jj in range(4):
                        self.tr(tp[:, jj * 128:(jj + 1) * 128], vT[:, jj * 128:(jj + 1) * 128], [vres], [tres])
                    tv = tp[:, 0:512].rearrange("p (a b) -> p a b", a=4)
                    S.op('dve', lambda e, tv=tv, c=c: e.tensor_copy(vaug[:, 4 * c:4 * c + 4, 0, 0:64], tv[:, :, 0:64]),
                         [tres, 'vaug1'], [('vaug', c)])
                    S.op('dve', lambda e, tv=tv, c=c: e.tensor_copy(vaug[:, 4 * c:4 * c + 4, 1, 64:128],
                                                                  tv[:, :, 64:128]), [tres, 'vaug1'], [('vaug', c)])

    def phase_dilated(self, li, xnT):
        S = self.S
        W = self.W
        C = self.C
        dils = (1, 4, 16)
        with contextlib.ExitStack() as es:
            self.alloc_attn_common(es)
            self.wqkv = Rot('wqkv', [self.sb(es, f"wqkv{i}", [128, 8, 384], BF16) for i in range(2)])
            qY = self.sb(es, "qYd", [128, 2, SEQ], BF16)
            kX = self.sb(es, "kXd", [128, 2, SEQ], BF16)
            vaug = self.sb(es, "vaugd", [128, NT, 2, 128], BF16)
            vTp = self.sb(es, "vTp", [128, SEQ], BF16)
            bandm = self.sb(es, "bandm", [128, 256], BF16)
            self.dma(bandm[:], C['bandm'], [], ['bandm'])
            for sl in range(2):
                S.op('pool', lambda e, sl=sl: e.memset(qY[64:128, sl, :], 0.0), [], [('qA', sl)])
                S.op('pool', lambda e, sl=sl: e.memset(kX[64:128, sl, :], 0.0), [], [('kA', sl)])
                self.dma(kX[64:73, sl, :], C['dlka'], [], [('kA', sl)])
            S.op('pool', lambda e: e.memset(vaug[:, :, 0, 64:128], 1.0), [], ['vaug1'])
            S.op('pool', lambda e: e.memset(vaug[:, :, 1, 0:64], 1.0), [], ['vaug1'])
            acc = [self.sb(es, f"acc{i}", [128, SEQ], F32) for i in range(2)]
            wv = W[f'l{li}_w_qkv'].rearrange("(k p) n -> p k n", p=128)
            nextw = self.pair_load(wv, 0, 512, 1024)
            for pp in range(4):
                for g in range(3):
                    d = dils[g]
                    Sd = SEQ // d
                    nseg = Sd // 128

                    def colmap(k, c, d=d, Sd=Sd):
                        if d == 1:
                            return xnT[:, k, c * 512:(c + 1) * 512]
                        if Sd >= 512:
                            r, i0 = divmod(c * 512, Sd)
                            st = r + d * i0
                            return xnT[:, k, st:st + d * 511 + 1:d]
                        v = xnT[:, k, :].rearrange("p (j d) -> p d j", d=d)
                        nr = 512 // Sd
                        return v[:, c * nr:(c + 1) * nr, :]

                    w, wres = nextw
                    ni = pp * 3 + g + 1
                    if ni < 12:
                        npp, ng = divmod(ni, 3)
                        nb_ = (ng * 3) * 512
                        nextw = self.pair_load(wv, nb_ + npp * 128, nb_ + 512 + npp * 128, nb_ + 1024 + npp * 128)
                    for sl in range(2):
                        self.dma(qY[64:73, sl, :], C['dlqa'][g * 8 + 2 * pp + sl], [], [('qA', sl)])
                    allx = [('xnT', cc) for cc in range(8)]
                    for c in range(8):
                        cs = slice(c * 512, (c + 1) * 512)
                        for j in range(3):
                            pj, pres = self.pjs.next()
                            for k in range(8):
                                self.mm(pj[:], w[:, k, j * 128:(j + 1) * 128], xnT[:, k, cs], k == 0, k == 7,
                                        [(wres, j), ('xnT', c)], [pres])
                            w_ = 512 // d
                            isl = slice(c * w_, (c + 1) * w_)

                            def pv_(ap, rows):
                                return ap.rearrange("p (r i) -> p r i", r=d)[:, :, isl]

                            def sv_(rows):
                                return pj[rows, :].rearrange("p (i r) -> p r i", r=d)
                            if j < 2:
                                dst = qY if j == 0 else kX
                                nm = 'qTa' if j == 0 else 'kTa'
                                d0 = pv_(dst[0:64, 0, :], None)
                                d1 = pv_(dst[0:64, 1, :], None)
                                s0 = sv_(slice(0, 64))
                                s1 = sv_(slice(64, 128))
                                self.act(d0, s0, AF.Copy, [pres], [(nm, 0)])
                                S.op('dve', lambda e, d1=d1, s1=s1: e.tensor_copy(d1, s1), [pres], [(nm, 1)])
                            else:
                                self.act(pv_(vTp[:, :], None), sv_(slice(0, 128)), AF.Copy, [pres], ['vTp'])
                        if False:
                            if True:
                                tv = None
                                pass
                    for c in range(8):
                        tp, tres = self.tps.next()
                        for jj in range(4):
                            self.tr(tp[:, jj * 128:(jj + 1) * 128], vTp[:, c * 512 + jj * 128:c * 512 + (jj + 1) * 128],
                                    ['vTp'], [tres])
                        tv = tp[:, 0:512].rearrange("p (a b) -> p a b", a=4)
                        S.op('dve', lambda e, tv=tv, c=c: e.tensor_copy(vaug[:, 4 * c:4 * c + 4, 0, 0:64],
                                                                      tv[:, :, 0:64]), [tres, 'vaug1'], [('vaug', c)])
                        S.op('dve', lambda e, tv=tv, c=c: e.tensor_copy(vaug[:, 4 * c:4 * c + 4, 1, 64:128],
                                                                      tv[:, :, 64:128]), [tres, 'vaug1'], [('vaug', c)])
                    for slot in range(2):
                        for c in range(8):
                            oa, ores = self.oas.next()
                            steps = []
                            for kt in range(4 * c - 1, 4 * c + 4):
                                if kt < 0:
                                    continue
                                qts = [kt] + ([kt + 1] if (kt + 1) % nseg != 0 else [])
                                qts = [qt for qt in qts if 4 * c <= qt <= 4 * c + 3]
                                if qts:
                                    steps.append((kt, qts))
                            A = acc[slot]
                            if d == 1:
                                def after(oa=oa, ores=ores, A=A, c=c, slot=slot):
                                    self.act(A[:, c * 512:(c + 1) * 512], oa[:], AF.Copy, [ores, ('accall', slot)],
                                             [('acc', slot, c)])
                            else:
                                if Sd >= 512:
                                    r, i0 = divmod(c * 512, Sd)
                                    st = r + d * i0
                                    pieces = [(A[:, st:st + d * 511 + 1:d], oa[:, 0:512])]
                                else:
                                    nr = 512 // Sd
                                    pieces = []
                                    for rr in range(nr):
                                        r = c * nr + rr
                                        pieces.append((A[:, r:r + d * (Sd - 1) + 1:d], oa[:, rr * Sd:(rr + 1) * Sd]))

                                def after(pieces=pieces, ores=ores, slot=slot):
                                    for (av, ov) in pieces:
                                        S.op('dve', lambda e, av=av, ov=ov: e.tensor_tensor(av, ov, av, ALU.add),
                                             [ores] + [('acc', slot, cc) for cc in range(8)], [('accall', slot)])
                            for si, (kt, qts) in enumerate(steps):
                                col0 = (qts[0] - 4 * c) * 128
                                n = 128 * len(qts)
                                boff = 0 if qts[0] == kt else 128
                                qk = [(kX[:, slot, kt * 128:(kt + 1) * 128], qY[:, slot, c * 512 + col0:c * 512 + col0 + n],
                                       [('kTa', slot), ('kA', slot), ('qTa', slot), ('qA', slot)]),
                                      (self.ident[:], bandm[:, boff:boff + n], ['ident', 'bandm'])]
                                last = si == len(steps) - 1
                                self.attn_step(n, qk, 0.125, (oa[:, col0:col0 + n], vaug[:, kt, slot, :],
                                                              [('vaug', kt // 4), 'vaug1'], ores, si == 0, last),
                                               after if last else None)
                    self.attn_flush()
                for slot in range(2):
                    A = acc[slot]
                    for c in range(8):
                        self.normalize_out(lambda rows, A=A, c=c: A[rows, c * 512:(c + 1) * 512], ('accall', slot),
                                           512, slot, pp, c * 512, use_act=True)
        S.barrier()

    def phase_moba(self, li, xnT):
        S = self.S
        W = self.W
        C = self.C
        with contextlib.ExitStack() as es:
            self.alloc_attn_common(es, nslots=3)
            self.wqkv = Rot('wqkv', [self.sb(es, f"wqkv{i}", [128, 8, 384], BF16) for i in range(2)])
            qY = self.sb(es, "qY", [128, 2, SEQ], BF16)
            kX = self.sb(es, "kX", [128, 2, SEQ], BF16)
            vaug = self.sb(es, "vaugp", [128, NT, 2, 128], BF16)
            self.vTs = Rot('vT', [self.sb(es, f"vT{i}", [128, 512], BF16) for i in range(2)])
            for sl in range(2):
                S.op('pool', lambda e, sl=sl: e.memset(qY[64:128, sl, :], 0.0), [], [('qYz', sl)])
                self.dma(kX[64:128, sl, :], C['xselT'], [], ['xselT'])
            S.op('pool', lambda e: e.memset(vaug[:, :, 0, 64:128], 1.0), [], ['vaug1'])
            S.op('pool', lambda e: e.memset(vaug[:, :, 1, 0:64], 1.0), [], ['vaug1'])
            qZs = Rot('qZ', [self.sb(es, f"qZ{i}", [128, 256], BF16) for i in range(3)])
            for i in range(3):
                S.op('pool', lambda e, i=i: e.memset(qZs.tiles[i][64:128, :], 0.0), [], [('qZ', i)])
            bmo = self.sb(es, "bmo", [128, 16, 2, 256], BF16)
            self.dma(bmo[:], C['bmo'], [], ['bmo'])
            tqm = self.sb(es, "tqm", [128, 2, 16], F32)
            self.dma(tqm[:], C['tqm'], [], ['tqm'])
            dtab = self.sb(es, "dtab", [128, 16, 31], F32)
            self.dma(dtab[:], C['dtab'], [], ['dtab'])
            slp = self.sb(es, "slp", [128, 16, 2], BF16)
            self.dma(slp[:], C['slp'], [], ['slp'])
            c30 = self.sb(es, "c30", [128, 16], F32)
            S.op('dve', lambda e: e.memset(c30[:], 30000.0), [], ['c30'])
            kmf = self.sb(es, "kmf", [64, 16], F32)
            kmT = self.sb(es, "kmT", [128, 2, 16], BF16)
            S.op('pool', lambda e: e.memset(kmT[:], 0.0), [], [('kmT', 0), ('kmT', 1)])
            gss = Rot('gs', [self.sb(es, f"gs{i}", [128, 16], F32) for i in range(4)])
            t8s = Rot('t8', [self.sb(es, f"t8{i}", [128, 8], F32) for i in range(4)])
            s3s = Rot('s3', [self.sb(es, f"s3{i}", [128, 16], F32) for i in range(4)])
            vvs = Rot('vv', [self.sb(es, f"vv{i}", [128, 16], F32) for i in range(4)])
            yps = Rot('yp', [self.sb(es, f"yp{i}", [128, 128], BF16) for i in range(6)])
            for i in range(6):
                S.op('pool', lambda e, i=i: e.memset(yps.tiles[i][:], 0.0), [], [('yp', i)])
            gp, gpres = self.pjs.tiles[1], ('pj', 1)
            wv = W[f'l{li}_w_qkv'].rearrange("(k p) n -> p k n", p=128)
            nextw = self.pair_load(wv, 0, 1024, 2048)
            for p in range(8):
                w, wres = nextw
                if p + 1 < 8:
                    nextw = self.pair_load(wv, (p + 1) * 128, 1024 + (p + 1) * 128, 2048 + (p + 1) * 128)
                for c in range(8):
                    cs = slice(c * 512, (c + 1) * 512)
                    for j in range(3):
                        pj, pres = self.pjs.next()
                        for k in range(8):
                            self.mm(pj[:], w[:, k, j * 128:(j + 1) * 128], xnT[:, k, cs], k == 0, k == 7,
                                    [(wres, j), ('xnT', c)], [pres])
                        if j < 2:
                            dst = qY if j == 0 else kX
                            nm = 'qT' if j == 0 else 'kT'
                            self.act(dst[0:64, 0, cs], pj[0:64, :], AF.Copy, [pres], [(nm, 0, c)])
                            S.op('dve', lambda e, pj=pj, cs=cs, dst=dst: e.tensor_copy(dst[0:64, 1, cs], pj[64:128, :]),
                                 [pres], [(nm, 1, c)])
                        else:
                            vT, vres = self.vTs.next()
                            self.act(vT[:], pj[:], AF.Copy, [pres], [vres])
                            tp, tres = self.tps.next()
                            for jj in range(4):
                                self.tr(tp[:, jj * 128:(jj + 1) * 128], vT[:, jj * 128:(jj + 1) * 128], [vres], [tres])
                            tv = tp[:, 0:512].rearrange("p (a b) -> p a b", a=4)
                            S.op('dve', lambda e, tv=tv, c=c: e.tensor_copy(vaug[:, 4 * c:4 * c + 4, 0, 0:64],
                                                                          tv[:, :, 0:64]), [tres, 'vaug1'], [('vaug', c)])
                            S.op('dve', lambda e, tv=tv, c=c: e.tensor_copy(vaug[:, 4 * c:4 * c + 4, 1, 64:128],
                                                                          tv[:, :, 64:128]), [tres, 'vaug1'],
                                 [('vaug', c)])
                for slot in range(2):
                    h = 2 * p + slot
                    allk = [('kT', slot, c) for c in range(8)]
                    S.op('dve', lambda e, slot=slot: e.tensor_reduce(
                        out=kmf[:], in_=kX[0:64, slot, :].rearrange("p (n k) -> p n k", k=256), axis=AX.X, op=ALU.add),
                        allk, ['kmf'])
                    S.op('dve', lambda e, slot=slot: e.tensor_scalar(kmT[0:64, slot, :], kmf[:], 1.0 / 256, None, ALU.mult),
                         ['kmf'], [('kmT', slot)])
                    ypd = {}

                    def prepA(b, slot=slot, h=h, ypd=ypd):
                        for j in range(2):
                            tcols = slice(b * 256 + j * 128, b * 256 + (j + 1) * 128)
                            if b > 3:
                                self.mm(gp[:, 0:16], qY[:, slot, tcols], kmT[:, slot, :], True, True,
                                        [('qT', slot, b // 2), ('qYz', slot), ('kmT', slot)], [gpres])
                                gs, gres = gss.next()
                                S.op('dve', lambda e, gs=gs: e.tensor_copy(gs[:], gp[:, 0:16]), [gpres], [gres])
                                S.op('dve', lambda e, gs=gs, b=b: e.memset(gs[:, b:16], -1e30), [], [gres])
                                t8, t8res = t8s.next()
                                S.op('dve', lambda e, gs=gs, t8=t8: e.max(t8[:], gs[:]), [gres], [t8res])
                                s3, s3res = s3s.next()
                                S.op('dve', lambda e, gs=gs, t8=t8, s3=s3: e.tensor_scalar(
                                    s3[:], gs[:], t8[:, 2:3], 30000.0, ALU.is_ge, ALU.mult), [gres, t8res], [s3res])
                            else:
                                s3, s3res = c30, 'c30'
                            vv, vres = vvs.next()
                            S.op('dve', lambda e, s3=s3, vv=vv, j=j: e.scalar_tensor_tensor(
                                out=vv[:], in0=s3[:], scalar=tqm[:, j, h:h + 1], in1=dtab[:, h, 15 - b:31 - b],
                                op0=ALU.add, op1=ALU.add), [s3res, 'tqm', 'dtab'], [vres])
                            yp, ypres = yps.next()
                            S.op('dve', lambda e, yp=yp, vv=vv: e.tensor_copy(yp[:, 0:16], vv[:]), [vres], [ypres])
                            S.op('dve', lambda e, yp=yp, vv=vv: e.tensor_tensor(yp[:, 16:32], vv[:], yp[:, 0:16],
                                                                              ALU.subtract), [vres, ypres], [ypres])
                            S.op('dve', lambda e, yp=yp: e.tensor_copy(yp[:, 32:34], slp[:, h, :]), ['slp'], [ypres])
                            ypd[(b, j)] = (yp, ypres)

                    def prepB(b, slot=slot, ypd=ypd):
                        for j in range(2):
                            yp, ypres = ypd[(b, j)]
                            tp, tres = self.tps.next()
                            self.tr(tp[:, 0:128], yp[:], [ypres], [tres])
                            cols = slice(b * 256 + j * 128, b * 256 + (j + 1) * 128)
                            S.op('dve', lambda e, tp=tp, cols=cols: e.tensor_copy(qY[64:128, slot, cols], tp[0:64, 0:128]),
                                 [tres, ('qYz', slot)], [('qY', slot, b)])

                    def steps(b, slot=slot, h=h, p=p):
                        oa, ores = self.oas.next()
                        qcols = slice(b * 256, (b + 1) * 256)
                        qz, qzres = qZs.next()
                        S.op('pool', lambda e, qz=qz: e.tensor_copy(qz[0:64, :], qY[0:64, slot, qcols]),
                             [('qT', slot, b // 2)], [qzres])
                        first = True
                        for n in range(b):
                            for half in range(2):
                                kt = 2 * n + half
                                qk = [(kX[:, slot, kt * 128:(kt + 1) * 128], qY[:, slot, qcols],
                                       [('kT', slot, kt // 4), 'xselT', ('qT', slot, b // 2), ('qY', slot, b)])]
                                self.attn_step(256, qk, 0.125, (oa[:, 0:256], vaug[:, kt, slot, :],
                                                                [('vaug', kt // 4), 'vaug1'], ores, first, False))
                                first = False
                        for half in range(2):
                            kt = 2 * b + half
                            n = 256 - 128 * half
                            qk = [(kX[:, slot, kt * 128:(kt + 1) * 128], qz[:, 128 * half:256],
                                   [('kT', slot, kt // 4), 'xselT', qzres]),
                                  (self.ident[:], bmo[:, h, 0, 0:n], ['ident', 'bmo']),
                                  (self.ident[:], bmo[:, h, 1, 0:n], ['ident', 'bmo'])]
                            after = None
                            if half == 1:
                                after = (lambda oa=oa, ores=ores: self.normalize_out(
                                    lambda rows: oa[rows, 0:256], ores, 256, slot, p, b * 256))
                            self.attn_step(n, qk, 0.125, (oa[:, 128 * half:256], vaug[:, kt, slot, :],
                                                          [('vaug', kt // 4), 'vaug1'], ores, first, half == 1), after)
                            first = False

                    prepA(1)
                    prepA(2)
                    prepB(1)
                    for b in range(16):
                        if b >= 1 and b + 1 <= 15:
                            prepB(b + 1)
                        if b >= 1 and b + 2 <= 15:
                            prepA(b + 2)
                        steps(b)
                    self.attn_flush()
        S.barrier()

    def phase_mla(self, li, xnT):
        S = self.S
        W = self.W
        C = self.C
        with contextlib.ExitStack() as es:
            ckvT = self.sb(es, "ckvT", [128, 2, SEQ], BF16)
            krT = self.sb(es, "krT", [32, SEQ], BF16)
            wukv = self.sb(es, "wukv", [128, 2, 2048], BF16)
            mtri = self.sb(es, "mtri", [128, 512], BF16)
            self.dma(mtri[:], C['mtri'], [], ['mtri'])
            A, Ares = self.pjs.tiles[0], ('pj', 0)
            B, Bres = self.pjs.tiles[1], ('pj', 1)
            Cb, Cres = self.scs.tiles[0], ('sc', 0)
            Q = [(self.oas.tiles[0], ('oa', 0)), (self.oas.tiles[1], ('oa', 1)), (self.scs.tiles[1], ('sc', 1))]
            tpA, tAres = self.tps.tiles[0], ('tp', 0)
            tpB, tBres = self.tps.tiles[1], ('tp', 1)
            with contextlib.ExitStack() as es1:
                self.stg = Rot('stg', [self.sb(es1, f"stgm{i}", [128, 1024], F32) for i in range(2)])
                wdkv = self.sb(es1, "wdkv", [128, 8, 1056], BF16)
                wuq = self.sb(es1, "wuq", [128, 6, 1536], BF16)
                wd = W[f'l{li}_w_dkv'].rearrange("(k p) n -> p k n", p=128)
                for k in range(8):
                    self.load_cast(wdkv[:, k, 0:1024], wd[:, k, 0:1024], 1024, wres=('wdkv', k))
                if not DBG.get('skip_wdkvr'):
                    self.load_cast(wdkv[:, :, 1024:1056], wd[:, :, 1024:1056], 256, wres='wdkvr', shape3=(8, 32))
                wq = W[f'l{li}_w_uq'].rearrange("(k p) n -> p k n", p=128)
                for k in range(6):
                    self.load_cast(wuq[:, k, 0:1024], wq[:, k, 0:1024], 1024, wres=('wuq', k))
                    self.load_cast(wuq[:, k, 1024:1536], wq[:, k, 1024:1536], 512, wres=('wuq2', k))
                wk = W[f'l{li}_w_ukv'].rearrange("(k p) n -> p k n", p=128)
                for k in range(2):
                    for hf in range(2):
                        self.load_cast(wukv[:, k, hf * 1024:(hf + 1) * 1024], wk[:, k, hf * 1024:(hf + 1) * 1024], 1024,
                                       wres=('wukv', k, hf))
                qg = self.sb(es1, "qg", [128, 768], F32)
                kvg = self.sb(es1, "kvg", [128, 256], F32)
                if not DBG.get('skip_g'):
                    self.dma(qg[:], W[f'l{li}_q_norm'].partition_broadcast(128), [], ['qg'])
                    self.dma(kvg[:], W[f'l{li}_kv_norm'].partition_broadcast(128), [], ['kvg'])
                cs = self.sb(es1, "ropecs", [128, 32, 32], F32)
                sn = self.sb(es1, "ropesn", [128, 32, 32], F32)
                if not DBG.get('skip_rope'):
                    self.dma(cs[:], C['ropecs'], [], ['ropecs'])
                    self.dma(sn[:], C['ropesn'], [], ['ropesn'])
                cqns = Rot('cqn', [self.sb(es1, f"cqn{i}", [128, 768], BF16) for i in range(2)])
                ckvns = Rot('ckvn', [self.sb(es1, f"ckvn{i}", [128, 256], BF16) for i in range(2)])
                krrs = Rot('krr', [self.sb(es1, f"krr{i}", [128, 128], BF16) for i in range(2)])
                for i in range(2):
                    S.op('pool', lambda e, i=i: e.memset(krrs.tiles[i][:], 0.0), [], [('krr', i)])
                ra = self.sb(es1, "ra", [128, 32], F32)
                rb = self.sb(es1, "rb", [128, 32], F32)
                cqTs = Rot('cqT', [self.sb(es1, f"cqT{i}", [128, 6, 128], BF16) for i in range(2)])
                qf = self.sb(es1, "qf", [128, 1536], F32)
                qbs = Rot('qb', [self.sb(es1, f"qb{i}", [128, 1536], BF16) for i in range(2)])
                ta = self.sb(es1, "ta", [128, 16, 32], F32)
                tb = self.sb(es1, "tb", [128, 16, 32], F32)
                qst = self.sb(es1, "qst", [96, 16, 512], BF16)
                ssa = self.sb(es1, "ssa", [128, NT], F32)
                ssb = self.sb(es1, "ssb", [128, NT], F32)
                ssk = self.sb(es1, "ssk", [128, NT], F32)
                rsq = self.sb(es1, "rsq", [128, NT], F32)
                rsk = self.sb(es1, "rsk", [128, NT], F32)
                def stageXa(t):
                    ts = slice(t * 128, (t + 1) * 128)
                    t1 = slice(t, t + 1)
                    for (dst, dres, c0, c1) in ((A, Ares, 0, 512), (B, Bres, 512, 768), (Cb, Cres, 768, 1056)):
                        for k in range(8):
                            self.mm(dst[:, 0:c1 - c0], xnT[:, k, ts], wdkv[:, k, c0:c1], k == 0, k == 7,
                                    [('xnT', t // 4), ('wdkv', k), 'wdkvr'], [dres])
                    junk, jres = self.junk.next()
                    self.act(junk[:, 0:512], A[:], AF.Square, [Ares], [jres, ('ssa', t)], accum=ssa[:, t1])
                    junk, jres = self.junk.next()
                    self.act(junk[:, 0:256], B[:, 0:256], AF.Square, [Bres], [jres, ('ssb', t)], accum=ssb[:, t1])
                    junk, jres = self.junk.next()
                    self.act(junk[:, 0:256], Cb[:, 0:256], AF.Square, [Cres], [jres, ('ssk', t)], accum=ssk[:, t1])
                    S.op('dve', lambda e, t1=t1: e.tensor_tensor(ssa[:, t1], ssa[:, t1], ssb[:, t1], ALU.add),
                         [('ssa', t), ('ssb', t)], [('ssa', t)])
                    self.rms_rstd(ssa[:, t1], rsq[:, t1], ('ssa', t), ('rsq', t), 768)
                    self.rms_rstd(ssk[:, t1], rsk[:, t1], ('ssk', t), ('rsk', t), 256)
                    cqn, cqres = cqns.next()
                    ckvn, ckres = ckvns.next()
                    S.op('dve', lambda e, cqn=cqn, t1=t1: e.scalar_tensor_tensor(
                        out=cqn[:, 0:512], in0=A[:], scalar=rsq[:, t1], in1=qg[:, 0:512], op0=ALU.mult, op1=ALU.mult),
                        [Ares, ('rsq', t), 'qg'], [cqres])
                    S.op('dve', lambda e, cqn=cqn, t1=t1: e.scalar_tensor_tensor(
                        out=cqn[:, 512:768], in0=B[:, 0:256], scalar=rsq[:, t1], in1=qg[:, 512:768],
                        op0=ALU.mult, op1=ALU.mult), [Bres, ('rsq', t), 'qg'], [cqres])
                    S.op('dve', lambda e, ckvn=ckvn, t1=t1: e.scalar_tensor_tensor(
                        out=ckvn[:], in0=Cb[:, 0:256], scalar=rsk[:, t1], in1=kvg[:], op0=ALU.mult, op1=ALU.mult),
                        [Cres, ('rsk', t), 'kvg'], [ckres])
                    krr, krres = krrs.next()
                    S.op('dve', lambda e, t=t: e.tensor_tensor(ra[:], Cb[:, 256:288], cs[:, t, :], ALU.mult),
                         [Cres, 'ropecs', ('ssk', t)], ['ra'])
                    S.op('dve', lambda e, t=t: e.tensor_tensor(rb[:, 0:16], Cb[:, 272:288], sn[:, t, 0:16], ALU.mult),
                         [Cres, 'ropesn', ('ssk', t)], ['rb'])
                    S.op('dve', lambda e, t=t: e.tensor_tensor(rb[:, 16:32], Cb[:, 256:272], sn[:, t, 16:32], ALU.mult),
                         [Cres, 'ropesn', ('ssk', t)], ['rb'])
                    S.op('dve', lambda e, krr=krr: e.tensor_tensor(krr[:, 0:32], ra[:], rb[:], ALU.add),
                         ['ra', 'rb'], [krres])
                    return cqn, cqres, ckvn, ckres, krr, krres

                def stageXb(t, cqn, cqres, ckvn, ckres, krr, krres):
                    ts = slice(t * 128, (t + 1) * 128)
                    m5 = 7
                    cqT, cqTres = cqTs.next()
                    if m5 & 1:
                        for k in range(6):
                            self.tr(tpA[:, k * 128:(k + 1) * 128], cqn[:, k * 128:(k + 1) * 128], [cqres], [tAres])
                    if m5 & 2:
                        for k in range(2):
                            self.tr(tpA[:, 768 + k * 128:768 + (k + 1) * 128], ckvn[:, k * 128:(k + 1) * 128], [ckres],
                                    [tAres])
                    if m5 & 4:
                        self.tr(tpB[:, 0:128], krr[:], [krres], [tBres])
                    if m5 & 1:
                        self.act(cqT[:], tpA[:, 0:768].rearrange("p (k n) -> p k n", k=6), AF.Copy, [tAres], [cqTres])
                    if m5 & 2:
                        self.act(ckvT[:, :, ts], tpA[:, 768:1024].rearrange("p (k n) -> p k n", k=2), AF.Copy,
                                 [tAres], [('ckvT', t // 4)])
                    if m5 & 4:
                        S.op('dve', lambda e, ts=ts: e.tensor_copy(krT[0:32, ts], tpB[0:32, 0:128]), [tBres],
                             [('krT', t // 4)])
                    return cqT, cqTres

                def stageYa(t, cqT, cqTres):
                    qb, qbres = qbs.next()
                    ts = slice(t * 128, (t + 1) * 128)
                    for j in range(3):
                        Qj, Qres = Q[j]
                        for k in range(6):
                            self.mm(Qj[:], cqT[:, k, :], wuq[:, k, j * 512:(j + 1) * 512], k == 0, k == 5,
                                    [cqTres, ('wuq', k), ('wuq2', k)], [Qres])
                        self.act(qf[:, j * 512:(j + 1) * 512], Qj[:], AF.Copy, [Qres], ['qf'])
                    qv = qf[:].rearrange("p (h d) -> p h d", h=16)
                    qbv = qb[:].rearrange("p (h d) -> p h d", h=16)
                    csb = cs[:, t:t + 1, :].broadcast_to([128, 16, 32])
                    snb = sn[:, t:t + 1, :].broadcast_to([128, 16, 32])
                    S.op('dve', lambda e, qv=qv, csb=csb: e.tensor_tensor(ta[:], qv[:, :, 64:96], csb, ALU.mult),
                         ['qf', 'ropecs'], ['ta'])
                    S.op('dve', lambda e, qv=qv, snb=snb: e.tensor_tensor(tb[:, :, 0:16], qv[:, :, 80:96],
                                                                      snb[:, :, 0:16], ALU.mult),
                         ['qf', 'ropesn'], ['tb'])
                    S.op('dve', lambda e, qv=qv, snb=snb: e.tensor_tensor(tb[:, :, 16:32], qv[:, :, 64:80],
                                                                      snb[:, :, 16:32], ALU.mult),
                         ['qf', 'ropesn'], ['tb'])
                    S.op('pool', lambda e, qv=qv, qbv=qbv: e.tensor_copy(qbv[:, :, 0:64], qv[:, :, 0:64]), ['qf'], [qbres])
                    S.op('dve', lambda e, qbv=qbv: e.tensor_tensor(qbv[:, :, 64:96], ta[:], tb[:], ALU.add),
                         ['ta', 'tb'], [qbres])
                    return qb, qbres

                def stageYb(t, qb, qbres):
                    for hh in range(16):
                        tp_, tr_ = (tpA, tAres) if hh < 8 else (tpB, tBres)
                        self.tr(tp_[0:96, (hh % 8) * 128:(hh % 8 + 1) * 128], qb[:, hh * 96:(hh + 1) * 96], [qbres], [tr_])
                    tsub = t % 4
                    self.act(qst[:, 0:8, tsub * 128:(tsub + 1) * 128], tpA[0:96, :].rearrange("p (h n) -> p h n", h=8),
                             AF.Copy, [tAres], ['qst'])
                    S.op('dve', lambda e, tsub=tsub: e.tensor_copy(qst[:, 8:16, tsub * 128:(tsub + 1) * 128],
                                                                 tpB[0:96, :].rearrange("p (h n) -> p h n", h=8)),
                         [tBres], ['qst'])
                    if tsub == 3:
                        g = t // 4
                        self.dma(self.qTs[:, :, g * 512:(g + 1) * 512].rearrange("h r n -> r h n"), qst[:], ['qst'],
                                 [('qTs', g)])

                nt = DBG.get('mla_nt', NT)
                xa = {0: stageXa(0)}
                xb = {0: stageXb(0, *xa[0])}
                ya = {}
                for t in range(nt):
                    if t + 1 < nt:
                        xa[t + 1] = stageXa(t + 1)
                    ya[t] = stageYa(t, *xb[t])
                    if t + 1 < nt:
                        xb[t + 1] = stageXb(t + 1, *xa[t + 1])
                    if t >= 1:
                        stageYb(t - 1, *ya[t - 1])
                stageYb(nt - 1, *ya[nt - 1])
            S.barrier()
            with contextlib.ExitStack() as es2:
                self.alloc_attn_common(es2)
                if DBG.get('mla_stage1_only'):
                    return
                qThs = Rot('qTh', [self.sb(es2, f"qTh{i}", [96, SEQ], BF16) for i in range(2)])
                kThs = Rot('kTh', [self.sb(es2, f"kTh{i}", [96, SEQ], BF16) for i in range(2)])
                vaugs = [self.sb(es2, f"vaugm{i}", [128, NT, 128], BF16) for i in range(2)]
                S.op('pool', lambda e: e.memset(vaugs[0][:, :, 64:128], 1.0), [], [('vaugm1', 0)])
                S.op('pool', lambda e: e.memset(vaugs[1][:, :, 0:64], 1.0), [], [('vaugm1', 1)])
                vTz = [Rot(f'vTz{sl}', [self.sb(es2, f"vTz{sl}_{i}", [128, 512], BF16) for i in range(2)])
                       for sl in range(2)]
                for sl in range(2):
                    for i in range(2):
                        zr = slice(64, 128) if sl == 0 else slice(0, 64)
                        S.op('pool', lambda e, sl=sl, i=i, zr=zr: e.memset(vTz[sl].tiles[i][zr, :], 0.0), [],
                             [(f'vTz{sl}z', i)])
                scale = 96.0 ** -0.5
                for h in range(16):
                    slot = h % 2
                    qTh, qres = qThs.next()
                    self.dma(qTh[:], self.qTs[h], [('qTs', g) for g in range(8)], [qres])
                    kTh, kres = kThs.next()
                    va = vaugs[slot]
                    S.op('pool', lambda e, kTh=kTh: e.tensor_copy(kTh[64:96, :], krT[0:32, :]),
                         [('krT', g) for g in range(8)], [(kres, 'r')])
                    for c in range(8):
                        cs_ = slice(c * 512, (c + 1) * 512)
                        pj, pres = self.pjs.next()
                        for k in range(2):
                            self.mm(pj[:], wukv[:, k, h * 128:(h + 1) * 128], ckvT[:, k, cs_], k == 0, k == 1,
                                    [('wukv', k, h // 8), ('ckvT', c)], [pres])
                        self.act(kTh[0:64, cs_], pj[0:64, :], AF.Copy, [pres], [(kres, c)])
                        vt, vtres = vTz[slot].next()
                        zres = (f'vTz{slot}z', vtres[1])
                        if slot == 0:
                            S.op('dve', lambda e, vt=vt, pj=pj: e.tensor_copy(vt[0:64, :], pj[64:128, :]),
                                 [pres], [vtres])
                        else:
                            S.op('dve', lambda e, vt=vt, pj=pj: e.tensor_copy(vt[64:128, :], pj[64:128, :]),
                                 [pres], [vtres])
                        tp, tres = self.tps.next()
                        for jj in range(4):
                            self.tr(tp[:, jj * 128:(jj + 1) * 128], vt[:, jj * 128:(jj + 1) * 128], [vtres, zres], [tres])
                        tv = tp[:, 0:512].rearrange("p (a b) -> p a b", a=4)
                        vs = slice(0, 64) if slot == 0 else slice(64, 128)
                        S.op('dve', lambda e, va=va, tv=tv, c=c, vs=vs: e.tensor_copy(va[:, 4 * c:4 * c + 4, vs],
                                                                                  tv[:, :, vs]),
                             [tres, ('vaugm1', slot)], [('vaugm', slot, c)])
                    for c in range(8):
                        oa, ores = self.oas.next()
                        nk = 4 * c + 4
                        for kt in range(nk):
                            ks = slice(kt * 128, (kt + 1) * 128)
                            rd = [(kres, kt // 4), (kres, 'r'), qres]
                            if kt < 4 * c:
                                col0, n = 0, 512
                                qk = [(kTh[:, ks], qTh[:, c * 512:(c + 1) * 512], rd)]
                            else:
                                col0 = 128 * (kt - 4 * c)
                                n = 512 - col0
                                qk = [(kTh[:, ks], qTh[:, c * 512 + col0:(c + 1) * 512], rd),
                                      (self.ident[:], mtri[:, 0:n], ['ident', 'mtri'])]
                            after = None
                            if kt == nk - 1:
                                after = (lambda oa=oa, ores=ores, slot=slot, h=h, c=c: self.normalize_out(
                                    lambda rows: oa[rows, 0:512], ores, 512, slot, h // 2, c * 512))
                            self.attn_step(n, qk, scale, (oa[:, col0:512], va[:, kt, :],
                                                          [('vaugm', slot, kt // 4), ('vaugm1', slot)], ores,
                                                          kt == 0, kt == nk - 1), after)
                    self.attn_flush()
        S.barrier()

    def build(self):
        nc = self.nc
        S = self.S
        with contextlib.ExitStack() as es:
            self.ident = self.sb(es, "ident", [128, 128], BF16)
            self.dma(self.ident[:], self.C['ident'], [], ['ident'])
            self.epsc = self.sb(es, "epsc", [128, 1], F32)
            S.op('dve', lambda e: e.memset(self.epsc[:], EPS), [], ['epsc'])
            self.junk = Rot('junk', [self.sb(es, f"junk{i}", [128, DM], BF16) for i in range(2)])
            ps = lambda name, shape, dt: es.enter_context(nc.psum_tensor(name, shape, dt))
            self.tps = Rot('tp', [ps(f"tp{i}", [128, 1024], BF16) for i in range(2)])
            self.pjs = Rot('pj', [ps(f"pj{i}", [128, 512], F32) for i in range(2)])
            self.scs = Rot('sc', [ps(f"sc{i}", [128, 512], F32) for i in range(2)])
            self.oas = Rot('oa', [ps(f"oa{i}", [128, 512], F32) for i in range(2)])
            hsrc = self.x
            for n, li in enumerate(self.layers):
                last = (n == len(self.layers) - 1)
                with contextlib.ExitStack() as es2:
                    xnT = self.sb(es2, "xnT", [128, 8, SEQ], BF16)
                    with contextlib.ExitStack() as es3:
                        self.phase_norm(es3, hsrc, f'l{li}_attn_norm', xnT)
                    S.barrier()
                    npair = 8
                    if li == 3:
                        self.phase_swa(li, xnT)
                    elif li == 1:
                        npair = 4
                        self.phase_dilated(li, xnT)
                    elif li == 0:
                        self.phase_moba(li, xnT)
                    elif li == 2:
                        self.phase_mla(li, xnT)
                    else:
                        raise NotImplementedError
                self.phase_of(li, npair, hsrc, last)
                hsrc = self.hbuf
            S.barrier()
            S.emit()
        return nc


_CONSTS = None


def run(inputs, layers=(0, 1, 2, 3), final=True, cores=8, x_override=None):
    global _CONSTS
    if _CONSTS is None:
        _CONSTS = host_consts()
    b = Builder(layers, final)
    nc = b.build()
    x = np.ascontiguousarray(inputs['x'], dtype=np.float32) if x_override is None else x_override
    in_maps = []
    for c in range(cores):
        m = {'x': np.ascontiguousarray(x[c])}
        for name in b.used_inputs():
            m[name] = np.ascontiguousarray(inputs[name], dtype=np.float32)
        for name in CONST_SPECS:
            m['c_' + name] = _CONSTS[name]
        in_maps.append(m)
    res = run_bass_kernel_spmd(nc, in_maps, core_ids=list(range(cores)))
    return np.stack([np.asarray(r['y']) for r in res.results], axis=0)


def kernel(**inputs):
    out = run(inputs)
    return out.astype(np.float32)
```
